# Optimizing a Trainium2 kernel written in Bass

```python
import math
import jax
import jax.numpy as jnp
from jax import lax
import numpy as np

D_MODEL = 2048
BATCH = 2
SEQ = 4096
DEPTH = 2

ROPE_THETA = 10000.0
ROPE_DIM = 64
Q_BLOCK = 128
NEG_INF = -1e30
LN_EPS = 1e-5
RMS_EPS = 1e-6

DIFF_HEADS = 8
DIFF_HEAD_DIM = 64
DIFF_V_DIM = 2 * DIFF_HEAD_DIM
MLA_HEADS = 8
MLA_Q_RANK = 512
MLA_KV_RANK = 256
MLA_NOPE_DIM = 128
MLA_ROPE_DIM = ROPE_DIM
MLA_V_DIM = 128
SWA_HEADS = 16
SWA_KV_HEADS = 4
SWA_GROUP = SWA_HEADS // SWA_KV_HEADS
SWA_HEAD_DIM = 64
WINDOW = 128

BRANCH_A_WIDTH = DIFF_HEADS * DIFF_V_DIM
BRANCH_B_WIDTH = MLA_HEADS * MLA_V_DIM
BRANCH_C_WIDTH = SWA_HEADS * SWA_HEAD_DIM

IN_SIZES = (
    2 * DIFF_HEADS * DIFF_HEAD_DIM,
    2 * DIFF_HEADS * DIFF_HEAD_DIM,
    DIFF_HEADS * DIFF_V_DIM,
    MLA_Q_RANK,
    MLA_KV_RANK,
    MLA_ROPE_DIM,
    SWA_HEADS * SWA_HEAD_DIM,
    SWA_KV_HEADS * SWA_HEAD_DIM,
    SWA_KV_HEADS * SWA_HEAD_DIM,
    3 * D_MODEL,
)
IN_WIDTH = sum(IN_SIZES)
IN_OFFSETS = tuple(int(o) for o in np.cumsum(IN_SIZES)[:-1])

N_EXPERTS = 16
N_GROUPS = 4
EXPERTS_PER_GROUP = N_EXPERTS // N_GROUPS
TOP_K = 2
EXPERT_HIDDEN = 512

DEEPNORM_ALPHA = (2 * DEPTH) ** 0.25
DEEPNORM_BETA = (8 * DEPTH) ** -0.25

kernel_name = 'hybrid_gated_diffattn_mla_swa_grouped_moe'


def layer_norm(x, g=None, b=None):
    xf = x.astype(jnp.float32)
    mu = xf.mean(-1, keepdims=True)
    var = jnp.square(xf - mu).mean(-1, keepdims=True)
    y = (xf - mu) * lax.rsqrt(var + LN_EPS)
    if g is not None:
        y = y * g.astype(jnp.float32) + b.astype(jnp.float32)
    return y.astype(x.dtype)


def rms_norm(x, g):
    xf = x.astype(jnp.float32)
    y = xf * lax.rsqrt(jnp.mean(xf * xf, -1, keepdims=True) + RMS_EPS) * g.astype(jnp.float32)
    return y.astype(x.dtype)


def modulate(xn, shift, scale):
    return xn * (1.0 + scale) + shift


def rope_tables(positions, dim):
    inv = ROPE_THETA ** (-jnp.arange(0, dim, 2, dtype=jnp.float32) / dim)
    ang = positions.astype(jnp.float32)[..., None] * inv
    return jnp.cos(ang), jnp.sin(ang)


def apply_rope(x, cos, sin):
    extra = x.ndim - 3
    cos = cos.reshape(cos.shape[:2] + (1,) * extra + cos.shape[2:])
    sin = sin.reshape(sin.shape[:2] + (1,) * extra + sin.shape[2:])
    x1, x2 = jnp.split(x.astype(jnp.float32), 2, axis=-1)
    out = jnp.concatenate([x1 * cos - x2 * sin, x2 * cos + x1 * sin], axis=-1)
    return out.astype(x.dtype)


def dense_attention(q, k, v, scale):
    B, M, H, S, dk = q.shape
    dv = v.shape[-1]
    nq = S // Q_BLOCK
    qb = q.reshape(B, M, H, nq, Q_BLOCK, dk).transpose(3, 0, 1, 2, 4, 5)

    def one_block(qi):
        s = jnp.einsum('bmhqd,bmhkd->bmhqk', qi, k).astype(jnp.float32) * scale
        p = jax.nn.softmax(s, axis=-1).astype(v.dtype)
        return jnp.einsum('bmhqk,bhkv->bmhqv', p, v)

    out = lax.map(one_block, qb)
    return out.transpose(1, 2, 3, 0, 4, 5).reshape(B, M, H, S, dv)


def diff_attention(q, k, v, cos, sin, lam_q, lam_k, subln_g, lam_init):
    B, S, _ = q.shape
    q = apply_rope(q.reshape(B, S, 2, DIFF_HEADS, DIFF_HEAD_DIM), cos, sin).transpose(0, 2, 3, 1, 4)
    k = apply_rope(k.reshape(B, S, 2, DIFF_HEADS, DIFF_HEAD_DIM), cos, sin).transpose(0, 2, 3, 1, 4)
    v = v.reshape(B, S, DIFF_HEADS, DIFF_V_DIM).transpose(0, 2, 1, 3)
    o = dense_attention(q, k, v, DIFF_HEAD_DIM ** -0.5)
    e = jnp.exp(jnp.sum(lam_q.astype(jnp.float32) * lam_k.astype(jnp.float32), axis=-1))
    lam = e[0] - e[1] + lam_init
    o = o[:, 0] - lam.astype(o.dtype) * o[:, 1]
    o = rms_norm(o, subln_g) * (1.0 - lam_init)
    return o.transpose(0, 2, 1, 3).reshape(B, S, BRANCH_A_WIDTH)


def latent_attention(cq, ckv, kr, cos, sin, q_norm, kv_norm, w_uq, w_ukv):
    B, S, _ = cq.shape
    q = (rms_norm(cq, q_norm) @ w_uq).reshape(B, S, MLA_HEADS, MLA_NOPE_DIM + MLA_ROPE_DIM)
    q = jnp.concatenate([q[..., :MLA_NOPE_DIM], apply_rope(q[..., MLA_NOPE_DIM:], cos, sin)], axis=-1)
    kv = (rms_norm(ckv, kv_norm) @ w_ukv).reshape(B, S, MLA_HEADS, MLA_NOPE_DIM + MLA_V_DIM)
    k_nope, v = kv[..., :MLA_NOPE_DIM], kv[..., MLA_NOPE_DIM:]
    k_rope = apply_rope(kr, cos, sin)
    k = jnp.concatenate(
        [k_nope, jnp.broadcast_to(k_rope[:, :, None, :], (B, S, MLA_HEADS, MLA_ROPE_DIM))], axis=-1)
    q = q.transpose(0, 2, 1, 3)[:, None]
    k = k.transpose(0, 2, 1, 3)[:, None]
    v = v.transpose(0, 2, 1, 3)
    o = dense_attention(q, k, v, (MLA_NOPE_DIM + MLA_ROPE_DIM) ** -0.5)[:, 0]
    return o.transpose(0, 2, 1, 3).reshape(B, S, BRANCH_B_WIDTH)


def window_attention(q, k, v, cos, sin, sink):
    B, S, _ = q.shape
    nb = S // WINDOW
    q = apply_rope(q.reshape(B, S, SWA_KV_HEADS, SWA_GROUP, SWA_HEAD_DIM), cos, sin)
    k = apply_rope(k.reshape(B, S, SWA_KV_HEADS, SWA_HEAD_DIM), cos, sin)
    v = v.reshape(B, S, SWA_KV_HEADS, SWA_HEAD_DIM)
    qb = q.reshape(B, nb, WINDOW, SWA_KV_HEADS, SWA_GROUP, SWA_HEAD_DIM)

    def band(t):
        tp = jnp.pad(t, ((0, 0), (WINDOW, WINDOW), (0, 0), (0, 0)))
        tp = tp.reshape(B, nb + 2, WINDOW, SWA_KV_HEADS, SWA_HEAD_DIM)
        return jnp.concatenate([tp[:, :-2], tp[:, 1:-1], tp[:, 2:]], axis=2)

    kb, vb = band(k), band(v)
    s = jnp.einsum('bnqgrd,bnkgd->bgrnqk', qb, kb).astype(jnp.float32) * (SWA_HEAD_DIM ** -0.5)
    blk = jnp.arange(nb)[:, None, None]
    qpos = blk * WINDOW + jnp.arange(WINDOW)[None, :, None]
    kpos = (blk - 1) * WINDOW + jnp.arange(3 * WINDOW)[None, None, :]
    valid = (jnp.abs(kpos - qpos) <= WINDOW) & (kpos >= 0) & (kpos < S)
    s = jnp.where(valid, s, NEG_INF)
    sink_logit = jnp.broadcast_to(
        sink.astype(jnp.float32).reshape(1, SWA_KV_HEADS, SWA_GROUP, 1, 1, 1), s.shape[:-1] + (1,))
    p = jax.nn.softmax(jnp.concatenate([s, sink_logit], axis=-1), axis=-1)[..., :-1].astype(v.dtype)
    o = jnp.einsum('bgrnqk,bnkgd->bnqgrd', p, vb)
    return o.reshape(B, S, BRANCH_C_WIDTH)


def hybrid_mixer(h, cos, sin, lam_init, w_in, lam_q, lam_k, subln_g, q_norm, kv_norm, w_uq, w_ukv,
                 sink, w_a, w_b, w_c, w_o):
    proj = h @ w_in
    a_q, a_k, a_v, b_cq, b_ckv, b_kr, c_q, c_k, c_v, gates = jnp.split(proj, IN_OFFSETS, axis=-1)
    ya = diff_attention(a_q, a_k, a_v, cos, sin, lam_q, lam_k, subln_g, lam_init)
    yb = latent_attention(b_cq, b_ckv, b_kr, cos, sin, q_norm, kv_norm, w_uq, w_ukv)
    yc = window_attention(c_q, c_k, c_v, cos, sin, sink)
    g_a, g_b, g_c = jnp.split(jax.nn.sigmoid(gates), 3, axis=-1)
    y = g_a * (ya @ w_a) + g_b * (yb @ w_b) + g_c * (yc @ w_c)
    return y @ w_o


def grouped_moe(h, router_w, router_bias, w1, w3, w2):
    B, S, D = h.shape
    t = h.reshape(B * S, D)
    T = t.shape[0]
    scores = jax.nn.sigmoid((t @ router_w).astype(jnp.float32))
    biased = scores + router_bias.astype(jnp.float32)
    grouped = biased.reshape(T, N_GROUPS, EXPERTS_PER_GROUP)
    group_score = lax.top_k(grouped, TOP_K)[0].sum(-1)
    best = jnp.argmax(group_score, axis=-1)
    in_group = jnp.arange(N_GROUPS)[None, :] == best[:, None]
    masked = jnp.where(in_group[:, :, None], grouped, NEG_INF).reshape(T, N_EXPERTS)
    _, idx = lax.top_k(masked, TOP_K)
    w = jnp.take_along_axis(scores, idx, axis=-1)
    w = w / jnp.sum(w, axis=-1, keepdims=True)
    gate = jnp.einsum('tk,tke->te', w, jax.nn.one_hot(idx, N_EXPERTS, dtype=jnp.float32))
    hidden = jax.nn.silu(jnp.einsum('td,edf->tef', t, w1)) * jnp.einsum('td,edf->tef', t, w3)
    hidden = hidden * gate[:, :, None].astype(hidden.dtype)
    out = jnp.einsum('tef,efd->td', hidden, w2)
    return out.reshape(B, S, D)


def setup_inputs(seed: int = 0) -> dict:
    key = jax.random.key(seed)
    ks = iter(jax.random.split(key, 40))
    L, D = DEPTH, D_MODEL

    def nrm(shape, std):
        return std * jax.random.normal(next(ks), shape, jnp.float32)

    return {
        'x': nrm((BATCH, SEQ, D), 1.0),
        'c': nrm((BATCH, D), 1.0),
        'positions': jnp.broadcast_to(jnp.arange(SEQ, dtype=jnp.int32), (BATCH, SEQ)),
        'w_in': nrm((L, D, IN_WIDTH), D ** -0.5),
        'diff_lambda_q': nrm((L, 2, DIFF_HEAD_DIM), 0.1),
        'diff_lambda_k': nrm((L, 2, DIFF_HEAD_DIM), 0.1),
        'diff_subln': 1.0 + nrm((L, DIFF_V_DIM), 0.02),
        'mla_q_norm': 1.0 + nrm((L, MLA_Q_RANK), 0.02),
        'mla_kv_norm': 1.0 + nrm((L, MLA_KV_RANK), 0.02),
        'mla_w_uq': nrm((L, MLA_Q_RANK, MLA_HEADS * (MLA_NOPE_DIM + MLA_ROPE_DIM)), MLA_Q_RANK ** -0.5),
        'mla_w_ukv': nrm((L, MLA_KV_RANK, MLA_HEADS * (MLA_NOPE_DIM + MLA_V_DIM)), MLA_KV_RANK ** -0.5),
        'swa_sink': nrm((L, SWA_HEADS), 1.0),
        'w_branch_a': nrm((L, BRANCH_A_WIDTH, D), DEEPNORM_BETA * BRANCH_A_WIDTH ** -0.5),
        'w_branch_b': nrm((L, BRANCH_B_WIDTH, D), DEEPNORM_BETA * BRANCH_B_WIDTH ** -0.5),
        'w_branch_c': nrm((L, BRANCH_C_WIDTH, D), DEEPNORM_BETA * BRANCH_C_WIDTH ** -0.5),
        'w_out': nrm((L, D, D), DEEPNORM_BETA * D ** -0.5),
        'w_ada': nrm((L, D, 6 * D), 0.5 * D ** -0.5),
        'b_ada': nrm((L, 6 * D), 0.1),
        'ln_mix_g': 1.0 + nrm((L, D), 0.02),
        'ln_mix_b': nrm((L, D), 0.02),
        'ln_ffn_g': 1.0 + nrm((L, D), 0.02),
        'ln_ffn_b': nrm((L, D), 0.02),
        'router_w': nrm((D, N_EXPERTS), D ** -0.5),
        'router_bias': nrm((N_EXPERTS,), 0.01),
        'expert_w1': nrm((L, N_EXPERTS, D, EXPERT_HIDDEN), D ** -0.5),
        'expert_w3': nrm((L, N_EXPERTS, D, EXPERT_HIDDEN), D ** -0.5),
        'expert_w2': nrm((L, N_EXPERTS, EXPERT_HIDDEN, D), DEEPNORM_BETA * EXPERT_HIDDEN ** -0.5),
    }


def reference(x, c, positions, w_in, diff_lambda_q, diff_lambda_k, diff_subln, mla_q_norm, mla_kv_norm,
              mla_w_uq, mla_w_ukv, swa_sink, w_branch_a, w_branch_b, w_branch_c, w_out, w_ada, b_ada,
              ln_mix_g, ln_mix_b, ln_ffn_g, ln_ffn_b, router_w, router_bias, expert_w1, expert_w3,
              expert_w2):
    cos, sin = rope_tables(positions, ROPE_DIM)
    c_act = jax.nn.silu(c)
    for l in range(DEPTH):
        mod = (c_act @ w_ada[l] + b_ada[l])[:, None, :]
        sh1, sc1, g1, sh2, sc2, g2 = jnp.split(mod, 6, axis=-1)
        lam_init = 0.8 - 0.6 * math.exp(-0.3 * l)
        h = modulate(layer_norm(x), sh1, sc1)
        y = hybrid_mixer(h, cos, sin, lam_init, w_in[l], diff_lambda_q[l], diff_lambda_k[l], diff_subln[l],
                         mla_q_norm[l], mla_kv_norm[l], mla_w_uq[l], mla_w_ukv[l], swa_sink[l],
                         w_branch_a[l], w_branch_b[l], w_branch_c[l], w_out[l])
        x = layer_norm(DEEPNORM_ALPHA * x + g1 * y, ln_mix_g[l], ln_mix_b[l])
        h = modulate(layer_norm(x), sh2, sc2)
        y = grouped_moe(h, router_w, router_bias, expert_w1[l], expert_w3[l], expert_w2[l])
        x = layer_norm(DEEPNORM_ALPHA * x + g2 * y, ln_ffn_g[l], ln_ffn_b[l])
    return x
```

```python
import math
import numpy as np
import concourse.bass as bass
import concourse.mybir as mybir
from concourse.bass_utils import run_bass_kernel_spmd

F32 = mybir.dt.float32
BF16 = mybir.dt.bfloat16
I32 = mybir.dt.int32
AF = mybir.ActivationFunctionType
ALU = mybir.AluOpType
AX = mybir.AxisListType

ENGS = ("pe", "act", "dve", "pool", "sp")
SB_BASE = 16512
SB_TOP = 229344


class Ref:
    __slots__ = ("ap", "keys")

    def __init__(self, ap, keys):
        self.ap = ap
        self.keys = tuple(keys)


class T:
    def __init__(self, t, key):
        self.t = t
        self.key = key

    def __getitem__(self, idx):
        return Ref(self.t[idx], (self.key,))

    def ref(self, ap):
        return Ref(ap, (self.key,))


class Op:
    __slots__ = ("eng", "fn", "deps", "flag", "kind", "ev", "idx")


class Prog:
    def __init__(self, nc, dma_k=None, same_engine_sync=True):
        self.nc = nc
        self.ops = {e: [] for e in ENGS}
        self.wstate = {}
        self.rstate = {}
        self.dma_k = dma_k or {"sp": 8, "pool": 8, "act": 4}
        self.ndma = {e: 0 for e in ENGS}
        self.ncc = 0
        self.same_engine_sync = same_engine_sync
        self.excl = set()
        self.sb_ptr = SB_BASE
        self.sb_stack = []
        self.sb_ranges = {}
        self.overlaps = {}
        self.sb_peak = SB_BASE
        self.nalloc = 0

    def push(self):
        self.sb_stack.append(self.sb_ptr)

    def pop(self):
        self.sb_ptr = self.sb_stack.pop()

    def sbuf(self, name, shape, dtype):
        esz = {F32: 4, BF16: 2, I32: 4}[dtype]
        n = 1
        for d_ in shape[1:]:
            n *= d_
        size = (n * esz + 31) // 32 * 32
        off = self.sb_ptr
        self.sb_ptr += size
        assert self.sb_ptr <= SB_TOP, "SBUF overflow at %s: %d" % (name, self.sb_ptr)
        self.sb_peak = max(self.sb_peak, self.sb_ptr)
        self.nalloc += 1
        key = "%s#%d" % (name, self.nalloc)
        t = self.nc.alloc_sbuf_tensor_at(key, list(shape), dtype, offset=off)
        ov = [key]
        for k2, (o2, e2) in self.sb_ranges.items():
            if o2 < off + size and off < e2:
                ov.append(k2)
                self.overlaps[k2].append(key)
        self.sb_ranges[key] = (off, off + size)
        self.overlaps[key] = ov
        return T(t, key)

    def psum(self, name, shape, dtype=F32):
        self.excl.add(name)
        return T(self.nc.alloc_psum_tensor(name, list(shape), dtype), name)

    def dram(self, name, shape, dtype, kind="Internal", **kw):
        return T(self.nc.dram_tensor(name, list(shape), dtype, kind=kind, **kw), name)

    def op(self, eng, fn, reads=(), writes=(), kind="c"):
        o = Op()
        o.eng = eng
        o.fn = fn
        o.kind = kind
        o.flag = False
        o.idx = len(self.ops[eng])
        deps = []
        rk = [k for r in reads for k in r.keys]
        wk = [k for w in writes for k in w.keys]
        for k0 in rk:
            for k in self.overlaps.get(k0, (k0,)):
                ev = self.wstate.get(k)
                if ev is not None:
                    deps.append(ev)
                if k in self.excl:
                    for ev in self.rstate.get(k, {}).values():
                        if not (ev[0] == "c" and ev[1] == eng):
                            deps.append(ev)
        for k0 in wk:
            for k in self.overlaps.get(k0, (k0,)):
                ev = self.wstate.get(k)
                if ev is not None:
                    deps.append(ev)
                for ev in self.rstate.get(k, {}).values():
                    deps.append(ev)
        if kind == "d":
            q = eng
            i = self.ndma[q]
            self.ndma[q] += 1
            K = self.dma_k[q]
            o.ev = ("d", q, i % K, 16 * (i // K + 1))
            if i >= K:
                deps.append(("d", q, i % K, 16 * (i // K)))
        elif kind == "cc":
            o.ev = ("cc", self.ncc)
            self.ncc += 1
        else:
            o.ev = ("c", eng, o.idx)
        fd = []
        for ev in deps:
            if ev[0] == "c":
                if ev[1] == eng:
                    if eng == "pe" or eng == "sp" or not self.same_engine_sync:
                        continue
                self.ops[ev[1]][ev[2]].flag = True
            fd.append(ev)
        o.deps = fd
        self.ops[eng].append(o)
        for k in wk:
            self.wstate[k] = o.ev
            self.rstate[k] = {}
        for k in rk:
            d = self.rstate.setdefault(k, {})
            if o.ev[0] == "c":
                d[eng] = o.ev
            elif o.ev[0] == "d":
                d[o.ev[:3]] = o.ev
            else:
                d[o.ev] = o.ev
        return o

    def emit(self, final_wait_eng="sp"):
        nc = self.nc
        sems = {e: nc.alloc_semaphore("s_" + e) for e in ENGS if e != "sp"}
        dsems = {
            q: [nc.alloc_semaphore("d_%s%d" % (q, j)) for j in range(self.dma_k[q])]
            for q in self.dma_k
        }
        ccsems = [nc.alloc_semaphore("cc%d" % i) for i in range(self.ncc)]
        semval = {}
        for e in ENGS:
            c = 0
            for o in self.ops[e]:
                if o.flag and o.kind == "c":
                    c += 1
                    semval[(e, o.idx)] = c
        final = []
        for q in self.dma_k:
            n = self.ndma[q]
            K = self.dma_k[q]
            for j in range(K):
                cnt = (n - j + K - 1) // K if n > j else 0
                if cnt > 0:
                    final.append((q, j, 16 * cnt))

        def run(e, h):
            seen = {}
            for o in self.ops[e]:
                for ev in o.deps:
                    if ev[0] == "c":
                        key = ("c", ev[1])
                        val = semval[(ev[1], ev[2])]
                        s = sems[ev[1]]
                    elif ev[0] == "d":
                        key = ("d", ev[1], ev[2])
                        val = ev[3]
                        s = dsems[ev[1]][ev[2]]
                    else:
                        key = ev
                        val = 1
                        s = ccsems[ev[1]]
                    if seen.get(key, 0) >= val:
                        continue
                    seen[key] = val
                    h.wait_ge(s, val)
                ins = o.fn(h)
                if o.kind == "d":
                    ins.then_inc(dsems[o.ev[1]][o.ev[2]], 16)
                elif o.kind == "cc":
                    ins.then_inc(ccsems[o.ev[1]], 1)
                elif o.flag:
                    ins.then_inc(sems[e], 1)
            if e == final_wait_eng:
                for q, j, v in final:
                    if seen.get(("d", q, j), 0) < v:
                        h.wait_ge(dsems[q][j], v)

        with nc.Block() as block:

            @block.tensor
            def _(h):
                run("pe", h)

            @block.scalar
            def _(h):
                run("act", h)

            @block.vector
            def _(h):
                run("dve", h)

            @block.gpsimd
            def _(h):
                run("pool", h)

            @block.sync
            def _(h):
                run("sp", h)

    def dma(self, out, in_, q="sp", **kw):
        return self.op(q, lambda h: h.dma_start(out.ap, in_.ap, **kw), [in_], [out], kind="d")

    def mm(self, out, lhsT, rhs, start=True, stop=True, **kw):
        return self.op(
            "pe",
            lambda h: h.matmul(out.ap, lhsT.ap, rhs.ap, start=start, stop=stop, **kw),
            [lhsT, rhs],
            [out],
        )

    def tr(self, out, in_, ident):
        return self.op("pe", lambda h: h.transpose(out.ap, in_.ap, ident.ap), [in_, ident], [out])

    def act(self, out, in_, func, bias=None, scale=None, accum_out=None):
        kw = {}
        rd = [in_]
        wr = [out]
        if bias is not None:
            if isinstance(bias, Ref):
                kw["bias"] = bias.ap
                rd.append(bias)
            else:
                kw["bias"] = bias
        if scale is not None:
            if isinstance(scale, Ref):
                kw["scale"] = scale.ap
                rd.append(scale)
            else:
                kw["scale"] = scale
        if accum_out is not None:
            kw["accum_out"] = accum_out.ap
            wr.append(accum_out)
        return self.op("act", lambda h: h.activation(out.ap, in_.ap, func, **kw), rd, wr)

    def copy(self, out, in_, eng="dve"):
        if eng == "act":
            return self.op(eng, lambda h: h.copy(out.ap, in_.ap), [in_], [out])
        return self.op(eng, lambda h: h.tensor_copy(out.ap, in_.ap), [in_], [out])

    def tt(self, out, in0, in1, op, eng="dve"):
        return self.op(eng, lambda h: h.tensor_tensor(out.ap, in0.ap, in1.ap, op), [in0, in1], [out])

    def ts(self, out, in0, s1, op0, s2=None, op1=None, accum_out=None, eng="dve"):
        rd = [in0]
        wr = [out]
        a1 = s1
        a2 = s2
        if isinstance(s1, Ref):
            rd.append(s1)
            a1 = s1.ap
        if isinstance(s2, Ref):
            rd.append(s2)
            a2 = s2.ap
        kw = {}
        if op1 is not None:
            kw["op1"] = op1
        if accum_out is not None:
            kw["accum_out"] = accum_out.ap
            wr.append(accum_out)
        return self.op(eng, lambda h: h.tensor_scalar(out.ap, in0.ap, a1, a2, op0, **kw), rd, wr)

    def stt(self, out, in0, scalar, in1, op0, op1, eng="dve"):
        rd = [in0, in1]
        a = scalar
        if isinstance(scalar, Ref):
            rd.append(scalar)
            a = scalar.ap
        return self.op(
            eng, lambda h: h.scalar_tensor_tensor(out.ap, in0.ap, a, in1.ap, op0, op1), rd, [out]
        )

    def memset(self, out, val, eng="dve"):
        return self.op(eng, lambda h: h.memset(out.ap, val), [], [out])

    def reduce(self, out, in_, op, axis=AX.X, eng="dve"):
        return self.op(eng, lambda h: h.tensor_reduce(out.ap, in_.ap, axis, op), [in_], [out])


D = 2048
S = 4096
NB = 2
DEPTH = 2
TOK = 1024
NT = TOK // 128
KC = D // 128
IN_W = 11584
O_AQ, O_AK, O_AV, O_CQ, O_CKV, O_KR, O_SQ, O_SK, O_SV, O_G = (
    0, 1024, 2048, 3072, 3584, 3840, 3904, 4928, 5184, 5440)
KR_A, KR_BN, KR_BR, KR_C, FK = 0, 1024, 2048, 2112, 2368
VC_A, VC_B, VC_C, FV = 0, 1024, 2048, 2304
ALPHA = (2 * DEPTH) ** 0.25
LN_EPS = 1e-5
RMS_EPS = 1e-6
NEXP = 16
EH = 512
TWO_PI = 2.0 * math.pi


def build(dbg=None, nlayers=DEPTH, moe=True, dbg_layer=0):
    nc = bass.Bass("TRN2", target_bir_lowering=False)
    P = Prog(nc)
    LW = nlayers
    din = lambda n, s, dt=F32: P.dram(n, [LW] + list(s[1:]) if (len(s) >= 3 and s[0] == DEPTH) else s, dt,
                                      kind="ExternalInput")
    x_in = din("x", [TOK, D])
    c_in = din("c", [1, D])
    pos_in = din("positions", [TOK, 1], I32)
    sel_in = din("sel", [1, 8])
    w_in = din("w_in", [DEPTH, D, IN_W])
    lamq_in = din("diff_lambda_q", [DEPTH, 1, 128])
    lamk_in = din("diff_lambda_k", [DEPTH, 1, 128])
    subln_in = din("diff_subln", [DEPTH, 1, 128])
    qn_in = din("mla_q_norm", [DEPTH, 1, 512])
    kvn_in = din("mla_kv_norm", [DEPTH, 1, 256])
    wuq_in = din("mla_w_uq", [DEPTH, 512, 1536])
    wukv_in = din("mla_w_ukv", [DEPTH, 256, 2048])
    sink_in = din("swa_sink", [DEPTH, 1, 16])
    wa_in = din("w_branch_a", [DEPTH, 1024, D])
    wb_in = din("w_branch_b", [DEPTH, 1024, D])
    wc_in = din("w_branch_c", [DEPTH, 1024, D])
    wo_in = din("w_out", [DEPTH, D, D])
    wada_in = din("w_ada", [DEPTH, D, 6 * D])
    bada_in = din("b_ada", [DEPTH, 1, 6 * D])
    lnmg_in = din("ln_mix_g", [DEPTH, 1, D])
    lnmb_in = din("ln_mix_b", [DEPTH, 1, D])
    lnfg_in = din("ln_ffn_g", [DEPTH, 1, D])
    lnfb_in = din("ln_ffn_b", [DEPTH, 1, D])
    rw_in = din("router_w", [D, NEXP])
    rb_in = din("router_bias", [1, NEXP])
    if moe:
        w1_in = din("expert_w1", [DEPTH, NEXP, D, EH])
        w3_in = din("expert_w3", [DEPTH, NEXP, D, EH])
        w2_in = din("expert_w2", [DEPTH, NEXP, EH, D])
    out_d = P.dram("out", [TOK, D], F32, kind="ExternalOutput")

    mod_d = P.dram("mod_d", [DEPTH, 1, 6 * D], F32)
    xs_d = P.dram("xs_d", [TOK, D], F32)
    x1p_d = P.dram("x1p_d", [TOK, D], F32)
    qTA_d = P.dram("qTA_d", [1024, TOK], BF16)
    qTBn_d = P.dram("qTBn_d", [1024, TOK], BF16)
    qTBr_d = P.dram("qTBr_d", [512, TOK], BF16)
    qTC_d = P.dram("qTC_d", [1024, TOK], BF16)
    def xch(name, shape):
        src = [P.dram("%s_s%d" % (name, l), shape, BF16) for l in range(DEPTH)]
        dst = [P.dram("%s_a%d" % (name, l), [4 * shape[0], shape[1]], BF16) for l in range(DEPTH)]
        return src, dst
    kA_s, kA_a = zip(*[xch("kA%d" % m, [512, TOK]) for m in range(2)])
    kB_s, kB_a = zip(*[xch("kB%d" % g, [512, TOK]) for g in range(2)])
    kRC_s, kRC_a = xch("kRC", [320, TOK])
    vA_s, vA_a = zip(*[xch("vA%d" % g, [TOK, 512]) for g in range(2)])
    vB_s, vB_a = zip(*[xch("vB%d" % g, [TOK, 512]) for g in range(2)])
    vC_s, vC_a = xch("vC", [TOK, 256])

    def bc_rows(t, row_ap_offset, n):
        return t.ref(bass.AP(t.t, row_ap_offset, [[0, 128], [1, n]]))

    identf = P.sbuf("identf", [128, 128], F32)
    ident = P.sbuf("ident", [128, 128], BF16)
    onesf = P.sbuf("onesf", [128, 128], F32)
    P.memset(identf[:], 1.0, eng="pool")
    P.op("pool", lambda h: h.affine_select(identf.t[:], identf.t[:], [[-1, 128]], ALU.is_equal, 0.0,
                                           base=0, channel_multiplier=1), [identf[:]], [identf[:]])
    P.copy(ident[:], identf[:], eng="pool")
    P.memset(onesf[:], 1.0, eng="pool")

    ps = [P.psum("ps%d" % i, [128, 512], F32) for i in range(8)]

    def psbf(i):
        return ps[i].t[:].bitcast(BF16)

    hT = P.sbuf("hT", [128, KC, TOK], BF16)
    stats = P.sbuf("stats", [128, 4, 6], F32)
    mv = P.sbuf("mv", [128, 8], F32)
    cosT = P.sbuf("cosT", [128, NT, 32], F32)
    sinT = P.sbuf("sinT", [128, NT, 32], F32)

    caT = P.sbuf("caT", [128, KC, 1], F32)
    P.push()
    cT = P.sbuf("cT", [128, KC, 1], F32)
    P.dma(cT[:], c_in.ref(bass.AP(c_in.t, 0, [[1, 128], [128, KC], [1, 1]])), q="sp", allow_slow_non_contiguous=True)
    P.act(caT[:], cT[:], AF.Silu)
    def ada_ln(l, eng, q, CW, defer=None):
        wada_b = [P.sbuf("wada_b%d" % i, [128, CW], F32) for i in range(3)]
        accs = [P.sbuf("ada_acc%d" % i, [128, CW], F32) for i in range(2)]
        modrow = P.sbuf("modrow", [128, CW], F32)
        badab = P.sbuf("badab", [128, CW], F32)
        ncb = 6 * D // CW
        seq = [(cb, kc) for cb in range(ncb) for kc in range(KC)]

        def load(i):
            cb, kc = seq[i]
            P.dma(wada_b[i % 3][:], wada_in[l, kc * 128:(kc + 1) * 128, cb * CW:(cb + 1) * CW], q=q)

        def colsum(cb, acc):
            P.dma(badab[:], bc_rows(bada_in, l * 6 * D + cb * CW, CW), q="sp")
            for n4 in range(CW // 512):
                P.mm(ps[7][:], onesf[:], acc[:, n4 * 512:(n4 + 1) * 512])
                P.tt(modrow[:, n4 * 512:(n4 + 1) * 512], ps[7][:], badab[:, n4 * 512:(n4 + 1) * 512], ALU.add)
            P.dma(mod_d[l, :, cb * CW:(cb + 1) * CW], modrow[0:1, :], q="sp")

        def fma_block(cb):
            for kc in range(KC):
                i = cb * KC + kc
                if i == 0:
                    load(0)
                    load(1)
                if i + 2 < len(seq):
                    load(i + 2)
                wb = wada_b[i % 3]
                acc = accs[cb % 2]
                if kc == 0:
                    P.ts(acc[:], wb[:], caT[:, kc, :], ALU.mult, eng=eng)
                elif eng == "dve":
                    P.stt(acc[:], wb[:], caT[:, kc, :], acc[:], ALU.mult, ALU.add, eng=eng)
                else:
                    P.ts(wb[:], wb[:], caT[:, kc, :], ALU.mult, eng=eng)
                    P.tt(acc[:], acc[:], wb[:], ALU.add, eng=eng)

        if defer is None:
            for cb in range(ncb):
                fma_block(cb)
                colsum(cb, accs[cb % 2])
        else:
            fma_block(0)
            fma_block(1)
            for cb in range(ncb):
                def step(cb=cb):
                    colsum(cb, accs[cb % 2])
                    if cb + 2 < ncb:
                        fma_block(cb + 2)
                defer.append(step)

    posi = P.sbuf("posi", [128, NT, 1], I32)
    posf = P.sbuf("posf", [128, NT, 1], F32)
    P.dma(posi[:], pos_in.ref(bass.AP(pos_in.t, 0, [[1, 128], [128, NT], [1, 1]])), q="sp", allow_slow_non_contiguous=True)
    P.copy(posf[:], posi[:])
    ii = P.sbuf("iota_i", [128, 32], I32)
    iif = P.sbuf("iota_f", [128, 32], F32)
    invf = P.sbuf("invf", [128, 32], F32)
    for i_ in range(32):
        P.memset(invf[:, i_:i_ + 1], float(np.float32(10000.0) ** np.float32(-(2.0 * i_) / 64.0)), eng="pool")
    ang = P.sbuf("ang", [128, NT, 32], F32)
    ang2 = P.sbuf("ang2", [128, NT, 32], F32)
    P.tt(ang[:], Ref(posf.t[:].broadcast_to([128, NT, 32]), posf[:].keys),
         Ref(invf.t[:].unsqueeze(1).broadcast_to([128, NT, 32]), invf[:].keys), ALU.mult)
    ki = P.sbuf("ki", [128, NT, 32], I32)
    kf = P.sbuf("kf", [128, NT, 32], F32)
    msk = P.sbuf("msk", [128, NT, 32], F32)

    def sin_of(out, a_in):
        P.ts(kf[:], a_in, 1.0 / TWO_PI, ALU.mult)
        P.copy(ki[:], kf[:])
        P.copy(kf[:], ki[:])
        P.stt(ang2[:], kf[:], -TWO_PI, a_in, ALU.mult, ALU.add)
        P.ts(msk[:], ang2[:], math.pi, ALU.is_gt)
        P.stt(ang2[:], msk[:], -TWO_PI, ang2[:], ALU.mult, ALU.add)
        P.ts(msk[:], ang2[:], -math.pi, ALU.is_lt)
        P.stt(ang2[:], msk[:], TWO_PI, ang2[:], ALU.mult, ALU.add)
        P.ts(ang2[:], ang2[:], math.pi, ALU.min, -math.pi, ALU.max)
        P.act(out, ang2[:], AF.Sin)

    sin_of(sinT[:], ang[:])
    P.ts(ang[:], ang[:], math.pi / 2, ALU.add)
    sin_of(cosT[:], ang[:])
    P.pop()

    if dbg == "p0":
        P.push()
        o = P.dram("dbg_mod", [DEPTH, 1, 6 * D], F32, kind="ExternalOutput")
        for l in range(DEPTH):
            t_ = P.sbuf("dbg_t%d" % l, [1, 6 * D], F32)
            P.dma(t_[:], mod_d[l, :, :], q="sp")
            P.dma(o[l, :, :], t_[:], q="sp")
        o2 = P.dram("dbg_cs", [128, 2 * NT * 32], F32, kind="ExternalOutput")
        P.dma(o2[:, 0:NT * 32], cosT.ref(cosT.t[:].rearrange("p a b -> p (a b)")), q="sp")
        P.dma(o2[:, NT * 32:], sinT.ref(sinT.t[:].rearrange("p a b -> p (a b)")), q="sp")
        P.emit()
        return nc


    ADA_CW = 512
    ADA_NCB = 6 * D // ADA_CW
    ada_w = [P.sbuf("ada_w%d" % i, [128, ADA_CW], F32) for i in range(4)]
    ada_acc = [P.sbuf("ada_acc%d" % i, [128, ADA_CW], F32) for i in range(2)]
    ada_row = P.sbuf("ada_row", [128, ADA_CW], F32)
    ada_b = P.sbuf("ada_b", [128, ADA_CW], F32)
    ada_seq = [(l_, cb, kc) for l_ in range(nlayers) for cb in range(ADA_NCB) for kc in range(KC)]
    ada_pos = [0]

    def ada_load(i):
        l_, cb, kc = ada_seq[i]
        P.dma(ada_w[i % 4][:], wada_in[l_, kc * 128:(kc + 1) * 128, cb * ADA_CW:(cb + 1) * ADA_CW], q="sp")

    for i_ in range(3):
        ada_load(i_)

    def ada_tick(n=1):
        for _ in range(n):
            i = ada_pos[0]
            if i >= len(ada_seq):
                return
            ada_pos[0] += 1
            if i + 3 < len(ada_seq):
                ada_load(i + 3)
            l_, cb, kc = ada_seq[i]
            acc = ada_acc[(l_ * ADA_NCB + cb) % 2]
            wb = ada_w[i % 4]
            if kc == 0:
                P.ts(acc[:], wb[:], caT[:, kc, :], ALU.mult)
            else:
                P.stt(acc[:], wb[:], caT[:, kc, :], acc[:], ALU.mult, ALU.add)
            if kc == KC - 1:
                P.dma(ada_b[:], bc_rows(bada_in, l_ * 6 * D + cb * ADA_CW, ADA_CW), q="sp")
                P.mm(ps[7][:, 0:ADA_CW], onesf[:], acc[:])
                P.tt(ada_row[:], ps[7][:, 0:ADA_CW], ada_b[:], ALU.add)
                P.dma(mod_d[l_, :, cb * ADA_CW:(cb + 1) * ADA_CW], ada_row[0:1, :], q="sp")

    def ada_flush(upto=None):
        upto = len(ada_seq) if upto is None else min(upto, len(ada_seq))
        while ada_pos[0] < upto:
            ada_tick()

    ada_tick((2 * D // ADA_CW) * KC)

    def R(tobj, ap):
        return Ref(ap, (tobj.key,))

    gates = P.sbuf("gates", [128, NT, NEXP], F32)
    selb = P.sbuf("selb", [128, 8], F32)
    P.dma(selb[:], bc_rows(sel_in, 0, 8), q="sp")
    validp = P.sbuf("validp", [128, 1], F32)
    validn = P.sbuf("validn", [128, 1], F32)
    P.reduce(validp[:], selb[:, 0:3], ALU.add)
    P.reduce(validn[:], selb[:, 4:7], ALU.add)
    maskP = P.sbuf("maskP", [128, 128], BF16)
    maskN = P.sbuf("maskN", [128, 128], BF16)
    mtmp = P.sbuf("mtmp", [128, 128], F32)
    for mk, sg in ((maskP, 1), (maskN, -1)):
        P.memset(mtmp[:], 1.0, eng="pool")
        P.op("pool", lambda h, sg=sg: h.affine_select(mtmp.t[:], mtmp.t[:], [[-sg, 128]], ALU.is_ge, 0.0,
                                                      base=0, channel_multiplier=sg), [mtmp[:]], [mtmp[:]])
        P.copy(mk[:], mtmp[:], eng="pool")
    s12_ = [P.sbuf("s12_%d" % i, [128, 4], F32) for i in range(2)]
    sm_ = [P.sbuf("sm_%d" % i, [128, 16], F32) for i in range(2)]
    lnst_ = [P.sbuf("lnst_%d" % i, [128, 4, 6], F32) for i in range(2)]
    lncnt = [0]

    def rstd_from(out, var_ref, eps):
        P.ts(out, var_ref, eps, ALU.add)
        P.act(out, out, AF.Ln)
        P.act(out, out, AF.Exp, scale=-0.5)

    def layer_norm(xt_, out_ref, A, B, junk, xn):
        i_ = lncnt[0] % 2
        lncnt[0] += 1
        sm, xn, st = sm_[i_], xn[i_], lnst_[i_]
        for c4 in range(4):
            P.op("dve", lambda h, c4=c4: h.bn_stats(st.t[:, c4, :], xt_.t[:, c4 * 512:(c4 + 1) * 512]),
                 [xt_[:]], [st[:]])
        P.op("dve", lambda h: h.bn_aggr(sm.t[:, 0:2], st.t[:]), [st[:]], [sm[:]])
        rstd_from(sm[:, 3:4], sm[:, 1:2], LN_EPS)
        P.stt(xn[:], xt_[:], sm[:, 0:1], A[:], ALU.subtract, ALU.mult)
        P.stt(out_ref, xn[:], sm[:, 3:4], B[:], ALU.mult, ALU.add)

    def transpose_to_hT(hb, t, evac_i):
        for g8 in range(2):
            bank = 2 + g8
            for c in range(8):
                kc = g8 * 8 + c
                P.tr(R(ps[bank], psbf(bank)[:, c * 128:(c + 1) * 128]), hb[:, kc * 128:(kc + 1) * 128], ident[:])
            src = R(ps[bank], psbf(bank)[:, 0:1024].rearrange("p (c n) -> p c n", n=128))
            dst = R(hT, hT.t[:, g8 * 8:(g8 + 1) * 8, t * 128:(t + 1) * 128])
            P.copy(dst, src, eng="act")

    def rope(src_ap, src_t, dst_ap, dst_t, t, nh, tmps):
        X = src_ap.rearrange("p (h two d) -> p h two d", two=2, d=32)
        Y = dst_ap.rearrange("p (h two d) -> p h two d", two=2, d=32)
        x1, x2 = R(src_t, X[:, :, 0, :]), R(src_t, X[:, :, 1, :])
        y1, y2 = R(dst_t, Y[:, :, 0, :]), R(dst_t, Y[:, :, 1, :])
        cb_ = R(cosT, cosT.t[:, t, :].unsqueeze(1).broadcast_to([128, nh, 32]))
        sb_ = R(sinT, sinT.t[:, t, :].unsqueeze(1).broadcast_to([128, nh, 32]))
        ta, tb_, tc_, td_ = tmps
        a_ = R(ta, ta.t[:, 0:nh, :])
        b_ = R(tb_, tb_.t[:, 0:nh, :])
        c_ = R(tc_, tc_.t[:, 0:nh, :])
        d_ = R(td_, td_.t[:, 0:nh, :])
        P.tt(a_, x1, cb_, ALU.mult)
        P.tt(b_, x2, sb_, ALU.mult)
        P.tt(c_, x2, cb_, ALU.mult)
        P.tt(d_, x1, sb_, ALU.mult)
        P.tt(y1, a_, b_, ALU.subtract)
        P.tt(y2, c_, d_, ALU.add)

    def rms_norm_tile(src, n, gain, out, junk):
        i_ = lncnt[0] % 2
        lncnt[0] += 1
        s12, sm = s12_[i_], sm_[i_]
        P.memset(s12[:, 2:3], 0.0)
        P.act(junk, src, AF.Square, accum_out=s12[:, 2:3])
        P.ts(sm[:, 8:9], s12[:, 2:3], 1.0 / n, ALU.mult)
        rstd_from(sm[:, 9:10], sm[:, 8:9], RMS_EPS)
        P.stt(out, src, sm[:, 9:10], gain, ALU.mult, ALU.mult)

    for l in range(nlayers):
        lam_init = 0.8 - 0.6 * math.exp(-0.3 * l)
        x_src = x_in if l == 0 else xs_d
        P.push()
        wblk = [P.sbuf("wblk%d" % i, [128, KC, 512], BF16) for i in range(2)]
        xt = [P.sbuf("xt%d" % i, [128, D], F32) for i in range(2)]
        modA = P.sbuf("modA", [128, D], F32)
        modB = P.sbuf("modB", [128, D], F32)
        junk = [P.sbuf("junk%d" % i, [128, D], BF16) for i in range(2)]
        xn = [P.sbuf("xn%d" % i, [128, D], F32) for i in range(2)]
        hb = [P.sbuf("hb%d" % i, [128, D], BF16) for i in range(2)]
        P.dma(modB[:], bc_rows(mod_d, l * 6 * D + 0 * D, D), q="sp")
        P.dma(modA[:], bc_rows(mod_d, l * 6 * D + 1 * D, D), q="sp")
        P.ts(modA[:], modA[:], 1.0, ALU.add)
        for t in range(NT):
            P.dma(xt[t % 2][:], x_src[t * 128:(t + 1) * 128, :], q="sp")
            layer_norm(xt[t % 2], hb[t % 2][:], modA, modB, junk, xn)
            transpose_to_hT(hb[t % 2], t, t)
        P.pop()
        if dbg == "ln1" and l == 0:
            o = P.dram("dbg_hT", [D, TOK], BF16, kind="ExternalOutput")
            P.dma(R(o, o.t[:].rearrange("(c p) n -> p c n", p=128)), hT[:], q="sp")
            P.emit()
            return nc

        P.push()
        wblk = [P.sbuf("wblk%d" % i, [128, KC, 512], BF16) for i in range(2)]
        stageT = [P.sbuf("stageT%d" % i, [128, 4, TOK], BF16) for i in range(2)]
        stageV = [P.sbuf("stageV%d" % i, [128, NT, 512], BF16) for i in range(2)]
        cqT = P.sbuf("cqT", [128, 4, TOK], BF16)
        ckvT = P.sbuf("ckvT", [128, 2, TOK], BF16)
        wuq = P.sbuf("wuq", [128, 4, 1536], BF16)
        wukv = P.sbuf("wukv", [128, 2, 2048], BF16)
        tok = [P.sbuf("tok%d" % i, [128, 512], BF16) for i in range(2)]
        rt = [P.sbuf("rt%d" % i, [128, 8, 32], F32) for i in range(4)]
        qnb = P.sbuf("qnb", [128, 512], F32)
        kvnb = P.sbuf("kvnb", [128, 256], F32)
        junk2_ = [P.sbuf("junk2_%d" % i, [128, 512], BF16) for i in range(2)]
        P.dma(wuq[:], R(wuq_in, wuq_in.t[l].rearrange("(c p) n -> p c n", p=128)), q="pool")
        P.dma(wukv[:], R(wukv_in, wukv_in.t[l].rearrange("(c p) n -> p c n", p=128)), q="pool")
        P.dma(qnb[:], bc_rows(qn_in, l * 512, 512), q="sp")
        P.dma(kvnb[:], bc_rows(kvn_in, l * 256, 256), q="sp")

        nst = [0, 0]

        def flushT(st, nchunk, dst_t, row0, parts=128):
            if parts == 128:
                P.dma(R(dst_t, dst_t.t[row0:row0 + nchunk * 128, :].rearrange("(c p) n -> p c n", p=128)),
                      R(st, st.t[:, 0:nchunk, :]), q="sp")
            else:
                P.dma(R(dst_t, dst_t.t[row0:row0 + parts, :]), R(st, st.t[0:parts, 0, :]), q="sp")

        def flushV(sv, ncol, dst_t):
            P.dma(R(dst_t, dst_t.t[:, 0:ncol].rearrange("(t p) c -> p t c", p=128)),
                  R(sv, sv.t[:, :, 0:ncol]), q="sp")

        def tok_transpose(tk, ncol, st, t, bank, c_off=0, eng="act"):
            nchunk = (ncol + 127) // 128
            for c in range(nchunk):
                w_ = min(128, ncol - c * 128)
                P.tr(R(ps[bank], psbf(bank)[0:w_, c * 128:(c + 1) * 128]), R(tk, tk.t[:, c * 128:c * 128 + w_]),
                     ident[:])
            if ncol % 128 == 0:
                src = R(ps[bank], psbf(bank)[:, 0:nchunk * 128].rearrange("p (c n) -> p c n", n=128))
                dst = R(st, st.t[:, c_off:c_off + nchunk, t * 128:(t + 1) * 128])
            else:
                src = R(ps[bank], psbf(bank)[0:ncol, 0:128])
                dst = R(st, st.t[0:ncol, c_off, t * 128:(t + 1) * 128])
            P.copy(dst, src, eng=eng)

        def tok_transpose_view(tk, c0_, ncol, st, t, bank):
            P.tr(R(ps[bank], psbf(bank)[0:ncol, 0:128]), R(tk, tk.t[:, c0_:c0_ + ncol]), ident[:])
            P.copy(R(st, st.t[0:ncol, 0, t * 128:(t + 1) * 128]), R(ps[bank], psbf(bank)[0:ncol, 0:128]), eng="act")

        blocks = [
            (O_AQ, 512, "ropeT", (qTA_d, 0)), (O_AQ + 512, 512, "ropeT", (qTA_d, 512)),
            (O_AK, 512, "ropeT", (kA_s[0][l], 0)), (O_AK + 512, 512, "ropeT", (kA_s[1][l], 0)),
            (O_AV, 512, "v", vA_s[0][l]), (O_AV + 512, 512, "v", vA_s[1][l]),
            (O_CQ, 512, "cq", None), (O_CKV, 320, "ckvkr", None),
            (O_SQ, 512, "ropeT", (qTC_d, 0)), (O_SQ + 512, 512, "ropeT", (qTC_d, 512)),
            (O_SK, 512, "ckcv", None),
        ]
        rg = [[0, 1, 2, 3], [4, 5, 6, 7]]

        def gather(src_t, dst_t):
            P.op("pool", lambda h, src_t=src_t, dst_t=dst_t: h.collective_compute(
                "AllGather", ALU.bypass, replica_groups=rg, ins=[src_t.t[:]], outs=[dst_t.t[:]]),
                [src_t[:]], [dst_t[:]], kind="cc")

        cc_after = {4: [(kA_s[0][l], kA_a[0][l])], 5: [(kA_s[1][l], kA_a[1][l])],
                    6: [(vA_s[0][l], vA_a[0][l])], 7: [(vA_s[1][l], vA_a[1][l])]}
        for bi, (c0, ncol, kind, dest) in enumerate(blocks):
            for pr in cc_after.get(bi, ()):
                gather(*pr)
            wb = wblk[bi % 2]
            P.dma(R(wb, wb.t[:, :, 0:ncol]),
                  R(w_in, w_in.t[l, :, c0:c0 + ncol].rearrange("(c p) n -> p c n", p=128)), q="pool")
            if kind in ("ropeT", "ckvkr", "ckcv"):
                st = stageT[nst[0] % 2]
                nst[0] += 1
            if kind in ("v", "ckcv"):
                sv = stageV[nst[1] % 2]
                nst[1] += 1
            pend_tr = []

            def flush_tr():
                while pend_tr:
                    pend_tr.pop(0)()

            for t in range(NT):
                pb = t % 2
                pst = ps[pb]
                for kc in range(KC):
                    P.mm(pst[:, 0:ncol], hT[:, kc, t * 128:(t + 1) * 128], wb[:, kc, 0:ncol],
                         start=(kc == 0), stop=(kc == KC - 1))
                flush_tr()
                tk = tok[t % 2]
                if kind == "ropeT":
                    rope(pst.t[:, 0:512], pst, tk.t[:, 0:512], tk, t, 8, rt)
                    pend_tr.append(lambda tk=tk, st=st, t=t, pb=pb: tok_transpose(tk, 512, st, t, 2 + pb))
                elif kind == "v":
                    P.copy(R(sv, sv.t[:, t, 0:ncol]), pst[:, 0:ncol], eng="act")
                elif kind == "cq":
                    rms_norm_tile(pst[:, 0:512], 512, qnb[:], tk[:, 0:512], junk2_[t % 2][:, 0:512])
                    pend_tr.append(lambda tk=tk, t=t, pb=pb: tok_transpose(tk, 512, cqT, t, 2 + pb))
                elif kind == "ckvkr":
                    rms_norm_tile(pst[:, 0:256], 256, kvnb[:], tk[:, 0:256], junk2_[t % 2][:, 0:256])
                    rope(pst.t[:, 256:320], pst, tk.t[:, 256:320], tk, t, 1, rt)
                    pend_tr.append(lambda tk=tk, t=t, pb=pb: tok_transpose(tk, 256, ckvT, t, 2 + pb))
                    pend_tr.append(lambda tk=tk, st=st, t=t, pb=pb: tok_transpose_view(tk, 256, 64, st, t, 4 + pb))
                elif kind == "ckcv":
                    rope(pst.t[:, 0:256], pst, tk.t[:, 0:256], tk, t, 4, rt)
                    P.copy(R(sv, sv.t[:, t, 0:256]), pst[:, 256:512], eng="act")
                    pend_tr.append(lambda tk=tk, st=st, t=t, pb=pb: tok_transpose(tk, 256, st, t, 2 + pb))
                ada_tick(1)
            flush_tr()
            if kind == "ropeT":
                flushT(st, 4, dest[0], dest[1])
            elif kind == "v":
                flushV(sv, ncol, dest)
            elif kind == "ckvkr":
                flushT(st, 1, kRC_s[l], 0, parts=64)
            elif kind == "ckcv":
                flushT(st, 2, kRC_s[l], 64)
                flushV(sv, 256, vC_s[l])

        for (wsrc, nkc, cstride, srcT, dsts) in ((wuq, 4, 192, cqT, None),
                                                   (wukv, 2, 256, ckvT, (kB_s[0][l], kB_s[1][l]))):
            for hg in range(2):
                st = stageT[nst[0] % 2]
                nst[0] += 1
                for hh in range(4):
                    h_ = hg * 4 + hh
                    for tb in range(2):
                        pst = ps[(hh * 2 + tb) % 2]
                        for kc in range(nkc):
                            P.mm(pst[:], R(wsrc, wsrc.t[:, kc, h_ * cstride:h_ * cstride + 128]),
                                 R(srcT, srcT.t[:, kc, tb * 512:(tb + 1) * 512]),
                                 start=(kc == 0), stop=(kc == nkc - 1))
                        P.copy(R(st, st.t[:, hh, tb * 512:(tb + 1) * 512]), pst[:],
                               eng="act" if tb == 0 else "dve")
                        ada_tick(1)
                if dsts is None:
                    flushT(st, 4, qTBn_d, hg * 512)
                else:
                    flushT(st, 4, dsts[hg], 0)
        st = stageT[nst[0] % 2]
        nst[0] += 1
        for t in range(NT):
            pb = t % 2
            pst = ps[pb]
            for kc in range(4):
                rhs = R(wuq, wuq.t[:, kc, :].rearrange("p (h c) -> p h c", c=192)[:, :, 128:192])
                P.mm(pst[:], R(cqT, cqT.t[:, kc, t * 128:(t + 1) * 128]), rhs, start=(kc == 0), stop=(kc == 3))
            flush_tr()
            tk = tok[t % 2]
            rope(pst.t[:, 0:512], pst, tk.t[:, 0:512], tk, t, 8, rt)
            pend_tr.append(lambda tk=tk, st=st, t=t, pb=pb: tok_transpose(tk, 512, st, t, 2 + pb))
        flush_tr()
        flushT(st, 4, qTBr_d, 0)
        for hg in range(2):
            sv = stageV[nst[1] % 2]
            nst[1] += 1
            for t in range(NT):
                pst = ps[t % 2]
                for kc in range(2):
                    rhs = R(wukv, wukv.t[:, kc, :].rearrange("p (h c) -> p h c", c=256)[:, hg * 4:(hg + 1) * 4, 128:256])
                    P.mm(pst[:], R(ckvT, ckvT.t[:, kc, t * 128:(t + 1) * 128]), rhs, start=(kc == 0), stop=(kc == 1))
                P.copy(R(sv, sv.t[:, t, :]), pst[:], eng="act" if t % 2 == 0 else "dve")
            flushV(sv, 512, vB_s[hg][l])
        P.pop()

        for pr in [(kB_s[0][l], kB_a[0][l]), (kB_s[1][l], kB_a[1][l]),
                   (kRC_s[l], kRC_a[l]), (vB_s[0][l], vB_a[0][l]), (vB_s[1][l], vB_a[1][l]),
                   (vC_s[l], vC_a[l])]:
            gather(*pr)

        if dbg == "p1" and l == 0:
            for nm, tt_ in (("qTA", qTA_d), ("qTBn", qTBn_d), ("qTBr", qTBr_d), ("qTC", qTC_d),
                            ("kA0", kA_a[0][l]), ("kA1", kA_a[1][l]), ("kB0", kB_a[0][l]), ("kB1", kB_a[1][l]),
                            ("kRC", kRC_a[l]), ("vA0", vA_a[0][l]), ("vA1", vA_a[1][l]), ("vB0", vB_a[0][l]),
                            ("vB1", vB_a[1][l]), ("vC", vC_a[l])):
                shp = list(tt_.t.shape)
                o = P.dram("dbg_" + nm, shp, BF16, kind="ExternalOutput")
                P.dma(o[:], tt_[:], q="sp")
            P.emit()
            return nc


        P.push()
        yT = P.sbuf("yT", [128, 24, TOK], BF16)
        P.push()
        KT = [P.sbuf("KT%d" % i, [128, S], BF16) for i in range(2)]
        Vt = [P.sbuf("Vt%d" % i, [128, 32, 132], BF16) for i in range(2)]
        QT = [P.sbuf("QT%d" % i, [128, TOK], BF16) for i in range(2)]
        Kr = P.sbuf("Kr", [64, S], BF16)
        QTr = [P.sbuf("QTr%d" % i, [64, TOK], BF16) for i in range(2)]
        Pt = [P.sbuf("Pt%d" % i, [128, 512], BF16) for i in range(4)]
        for v_ in Vt:
            P.memset(R(v_, v_.t[:, :, 128:129]), 1.0, eng="pool")
        lq = P.sbuf("lq", [128, 128], F32)
        lk = P.sbuf("lk", [128, 128], F32)
        lam2 = P.sbuf("lam2", [128, 2], F32)
        neglam = P.sbuf("neglam", [128, 1], F32)
        sublnS = P.sbuf("sublnS", [128, 128], F32)
        esink = P.sbuf("esink", [128, 16], F32)
        P.dma(lq[:], bc_rows(lamq_in, l * 128, 128), q="sp")
        P.dma(lk[:], bc_rows(lamk_in, l * 128, 128), q="sp")
        P.dma(sublnS[:], bc_rows(subln_in, l * 128, 128), q="sp")
        P.dma(esink[:], bc_rows(sink_in, l * 16, 16), q="sp")
        P.tt(lq[:], lq[:], lk[:], ALU.mult)
        P.reduce(lam2[:], R(lq, lq.t[:].rearrange("p (m d) -> p m d", m=2)), ALU.add)
        P.act(lam2[:], lam2[:], AF.Exp)
        P.act(esink[:], esink[:], AF.Exp)
        P.tt(neglam[:], lam2[:, 1:2], lam2[:, 0:1], ALU.subtract)
        P.ts(neglam[:], neglam[:], -lam_init, ALU.add)
        P.ts(sublnS[:], sublnS[:], 1.0 - lam_init, ALU.mult)
        fo = P.sbuf("fo", [128, 128], F32)
        fsq = P.sbuf("fsq", [128, 128], F32)
        fr = P.sbuf("fr", [128, 8], F32)
        Oc = P.sbuf("Oc", [128, 3, 396], F32)
        yblk = [P.sbuf("yblk%d" % i, [128, 512], BF16) for i in range(2)]
        nblk = [0]
        pending = []
        ada_pending = []

        def flush_pending():
            while pending:
                pending.pop(0)()

        def oslot(a):
            return ps[4 + a // 3], (a % 3) * 132

        def ocslot(a, c0, c1):
            return R(Oc, Oc.t[:, a // 3, (a % 3) * 132 + c0:(a % 3) * 132 + c1])

        def defer_transpose(yb, ncol, chunk0, nchunk_out, tok0, ntok):
            def f():
                nsub = ntok // 128
                nch = ncol // 128
                for sub in range(nsub):
                    for c in range(nch):
                        P.tr(R(ps[7], psbf(7)[:, (c * nsub + sub) * 128:(c * nsub + sub + 1) * 128]),
                             R(yb, yb.t[:, sub * ncol + c * 128:sub * ncol + (c + 1) * 128]), ident[:])
                P.copy(R(yT, yT.t[:, chunk0:chunk0 + nch, tok0:tok0 + ntok]),
                       R(ps[7], psbf(7)[:, 0:nch * ntok].rearrange("p (c n) -> p c n", n=ntok)))
            pending.append(f)

        def loads_A(h_):
            kt_, v_, q_ = KT[h_ % 2], Vt[h_ % 2], QT[h_ % 2]
            for m in range(2):
                P.dma(R(kt_, kt_.t[m * 64:(m + 1) * 64, :].rearrange("p (r n) -> p r n", r=4)),
                      R(kA_a[m][l], kA_a[m][l].t[:].rearrange("(r f) n -> f r n", r=4)[h_ * 64:(h_ + 1) * 64, :, :]),
                      q="sp")
                P.dma(R(q_, q_.t[m * 64:(m + 1) * 64, :]), qTA_d[m * 512 + h_ * 64:m * 512 + (h_ + 1) * 64, :], q="sp")
            P.dma(R(v_, v_.t[:, :, 0:128]),
                  R(vA_a[h_ // 4][l], vA_a[h_ // 4][l].t[:, (h_ % 4) * 128:(h_ % 4 + 1) * 128].rearrange(
                      "(k p) c -> p k c", p=128)), q="sp")

        def S_A(blk, kt, m):
            h_, qb = blk
            kt_, q_ = KT[h_ % 2], QT[h_ % 2]
            par = kt % 2
            P.mm(ps[par * 2 + m][:], R(kt_, kt_.t[m * 64:(m + 1) * 64, kt * 128:(kt + 1) * 128]),
                 R(q_, q_.t[m * 64:(m + 1) * 64, qb * 512:(qb + 1) * 512]))
            P.act(Pt[par * 2 + m][:], ps[par * 2 + m][:], AF.Exp, scale=0.125)

        def PV_A(blk, kt, started, m):
            h_, qb = blk
            v_ = Vt[h_ % 2]
            par = kt % 2
            for qs in range(4):
                if True:
                    bank, off = oslot(m * 4 + qs)
                    st_ = kt == 0 and bank.key not in started
                    started.add(bank.key)
                    P.mm(R(bank, bank.t[:, off:off + 129]),
                         R(Pt[par * 2 + m], Pt[par * 2 + m].t[:, qs * 128:(qs + 1) * 128]),
                         R(v_, v_.t[:, kt, 0:129]), start=st_, stop=(kt == 31))

        def fin_A(blk):
            h_, qb = blk
            for bnk in range(3):
                P.copy(Oc[:, bnk, :], R(ps[4 + bnk], ps[4 + bnk].t[:, 0:396]))
            yb = yblk[nblk[0] % 2]
            nblk[0] += 1
            for qs in range(4):
                P.op("dve", lambda h, qs=qs: h.reciprocal(fr.t[:, 0:1], ocslot(qs, 128, 129).ap), [Oc[:]], [fr[:]])
                P.op("dve", lambda h, qs=qs: h.reciprocal(fr.t[:, 1:2], ocslot(4 + qs, 128, 129).ap), [Oc[:]], [fr[:]])
                P.tt(fr[:, 2:3], fr[:, 1:2], neglam[:], ALU.mult)
                P.ts(fo[:], ocslot(qs, 0, 128), fr[:, 0:1], ALU.mult)
                P.stt(fo[:], ocslot(4 + qs, 0, 128), fr[:, 2:3], fo[:], ALU.mult, ALU.add)
                P.tt(fsq[:], fo[:], fo[:], ALU.mult)
                P.reduce(fr[:, 3:4], fsq[:], ALU.add)
                P.ts(fr[:, 3:4], fr[:, 3:4], 1.0 / 128, ALU.mult)
                rstd_from(fr[:, 4:5], fr[:, 3:4], RMS_EPS)
                P.stt(R(yb, yb.t[:, qs * 128:(qs + 1) * 128]), fo[:], fr[:, 4:5], sublnS[:], ALU.mult, ALU.mult)
            defer_transpose(yb, 128, h_, 1, qb * 512, 512)

        def run_dense(blocks_, loads, S_, PV_, fin_, nm, ticks):
            loads(blocks_[0][0])
            for m in range(nm):
                S_(blocks_[0], 0, m)
            for bi, blk in enumerate(blocks_):
                if blk[1] == 0 and blk[0] + 1 < 8:
                    loads(blk[0] + 1)
                started = set()
                for kt in range(32):
                    nxt = (blk, kt + 1) if kt + 1 < 32 else ((blocks_[bi + 1], 0) if bi + 1 < len(blocks_) else None)
                    if nxt is not None:
                        for m in range(nm):
                            S_(nxt[0], nxt[1], m)
                    for m in reversed(range(nm)):
                        PV_(blk, kt, started, m)
                    if ticks:
                        ada_tick(ticks)
                    if kt == 6:
                        flush_pending()
                    if kt == 12 and ada_pending and (bi % 2 == 1):
                        ada_pending.pop(0)()
                fin_(blk)
            flush_pending()

        blocks_hq = [(h_, qb) for h_ in range(8) for qb in range(2)]
        run_dense(blocks_hq, loads_A, S_A, PV_A, fin_A, 2, 0)

        P.dma(R(Kr, Kr.t[:, :].rearrange("p (r n) -> p r n", r=4)),
              R(kRC_a[l], kRC_a[l].t[:].rearrange("(r f) n -> f r n", r=4)[0:64, :, :]), q="sp")
        sB = 192.0 ** -0.5

        def loads_B(h_):
            kt_, v_, q_, qr_ = KT[h_ % 2], Vt[h_ % 2], QT[h_ % 2], QTr[h_ % 2]
            P.dma(R(kt_, kt_.t[:, :].rearrange("p (r n) -> p r n", r=4)),
                  R(kB_a[h_ // 4][l], kB_a[h_ // 4][l].t[:].rearrange("(r f) n -> f r n", r=4)[
                      (h_ % 4) * 128:(h_ % 4 + 1) * 128, :, :]), q="sp")
            P.dma(q_[:], qTBn_d[h_ * 128:(h_ + 1) * 128, :], q="sp")
            P.dma(qr_[:], qTBr_d[h_ * 64:(h_ + 1) * 64, :], q="sp")
            P.dma(R(v_, v_.t[:, :, 0:128]),
                  R(vB_a[h_ // 4][l], vB_a[h_ // 4][l].t[:, (h_ % 4) * 128:(h_ % 4 + 1) * 128].rearrange(
                      "(k p) c -> p k c", p=128)), q="sp")

        def S_B(blk, kt, m):
            h_, qb = blk
            kt_, q_, qr_ = KT[h_ % 2], QT[h_ % 2], QTr[h_ % 2]
            par = kt % 4
            P.mm(ps[par][:], R(kt_, kt_.t[:, kt * 128:(kt + 1) * 128]),
                 R(q_, q_.t[:, qb * 512:(qb + 1) * 512]), start=True, stop=False)
            P.mm(ps[par][:], R(Kr, Kr.t[0:64, kt * 128:(kt + 1) * 128]),
                 R(qr_, qr_.t[0:64, qb * 512:(qb + 1) * 512]), start=False, stop=True)
            P.act(Pt[par][:], ps[par][:], AF.Exp, scale=sB)

        def PV_B(blk, kt, started, m):
            h_, qb = blk
            v_ = Vt[h_ % 2]
            par = kt % 4
            for qs in range(4):
                bank, off = oslot(qs)
                st_ = kt == 0 and bank.key not in started
                started.add(bank.key)
                P.mm(R(bank, bank.t[:, off:off + 129]), R(Pt[par], Pt[par].t[:, qs * 128:(qs + 1) * 128]),
                     R(v_, v_.t[:, kt, 0:129]), start=st_, stop=(kt == 31))

        def fin_B(blk):
            h_, qb = blk
            for bnk in range(2):
                P.copy(Oc[:, bnk, :], R(ps[4 + bnk], ps[4 + bnk].t[:, 0:396]))
            yb = yblk[nblk[0] % 2]
            nblk[0] += 1
            for qs in range(4):
                P.op("dve", lambda h, qs=qs: h.reciprocal(fr.t[:, 0:1], ocslot(qs, 128, 129).ap), [Oc[:]], [fr[:]])
                P.ts(R(yb, yb.t[:, qs * 128:(qs + 1) * 128]), ocslot(qs, 0, 128), fr[:, 0:1], ALU.mult)
            defer_transpose(yb, 128, 8 + h_, 1, qb * 512, 512)

        run_dense(blocks_hq, loads_B, S_B, PV_B, fin_B, 1, 1)

        Kext = P.sbuf("Kext", [64, 10 * 128], BF16)
        Vext = P.sbuf("Vext", [128, 10, 68], BF16)
        Kc = P.sbuf("Kc", [64, 6, 128], BF16)
        Vc = P.sbuf("Vc", [128, 6, 64], BF16)
        Qc = P.sbuf("Qc", [64, 4, TOK], BF16)
        hkf = P.sbuf("hkf", [128, 128], F32)
        den4 = P.sbuf("den4", [128, 8], F32)
        P.memset(R(Vext, Vext.t[:, :, 64:65]), 1.0, eng="pool")

        def S_C(g, n, kk):
            pb = (n * 3 + kk) % 4
            P.mm(ps[pb][:], R(Kext, Kext.t[0:64, (n + kk) * 128:(n + kk + 1) * 128]),
                 R(Qc, Qc.t[0:64, :, n * 128:(n + 1) * 128]))
            pt_ = Pt[pb]
            P.act(pt_[:], ps[pb][:], AF.Exp, scale=0.125)
            pv = R(pt_, pt_.t[:].rearrange("p (r q) -> p r q", r=4))
            if kk == 0:
                sc_ = validp[:, 0:1] if n == 0 else 1.0
                P.stt(pv, pv, sc_, R(maskP, maskP.t[:].unsqueeze(1).broadcast_to([128, 4, 128])),
                      ALU.mult, ALU.mult)
            elif kk == 2:
                sc_ = validn[:, 0:1] if n == NT - 1 else 1.0
                P.stt(pv, pv, sc_, R(maskN, maskN.t[:].unsqueeze(1).broadcast_to([128, 4, 128])),
                      ALU.mult, ALU.mult)

        def PV_C(g, n, kk):
            pt_ = Pt[(n * 3 + kk) % 4]
            for r in range(4):
                P.mm(R(ps[4], ps[4].t[:, r * 68:r * 68 + 65]), R(pt_, pt_.t[:, r * 128:(r + 1) * 128]),
                     R(Vext, Vext.t[:, n + kk, 0:65]), start=(kk == 0 and r == 0), stop=(kk == 2))

        for g in range(4):
            kview = kRC_a[l].t[:].rearrange("(r f) n -> f r n", r=4)[64 + g * 64:64 + (g + 1) * 64, :, :]
            P.dma(Kc[:, 0:3, :], R(kRC_a[l], kview[:, 0:3, 896:1024]), q="sp")
            P.dma(Kc[:, 3:6, :], R(kRC_a[l], kview[:, 1:4, 0:128]), q="sp")
            vview = vC_a[l].t[:, g * 64:(g + 1) * 64].rearrange("(r t) c -> t r c", r=4)
            P.dma(Vc[:, 0:3, :], R(vC_a[l], vview[896:1024, 0:3, :]), q="sp")
            P.dma(Vc[:, 3:6, :], R(vC_a[l], vview[0:128, 1:4, :]), q="sp")
            P.dma(R(Kext, Kext.t[:, 128:1152]), kRC_s[l][64 + g * 64:64 + (g + 1) * 64, :], q="sp")
            P.dma(R(Vext, Vext.t[:, 1:9, 0:64]),
                  R(vC_s[l], vC_s[l].t[:, g * 64:(g + 1) * 64].rearrange("(t p) c -> p t c", p=128)), q="sp")
            P.dma(Qc[:], R(qTC_d, qTC_d.t[g * 256:(g + 1) * 256, :].rearrange("(r d) n -> d r n", d=64)), q="sp")
            for side, c0, s0 in ((0, 0, 0), (9, 3, 4)):
                P.ts(R(hkf, hkf.t[0:64, :]), R(Kc, Kc.t[:, c0, :]), selb[0:64, s0:s0 + 1], ALU.mult)
                P.stt(R(hkf, hkf.t[0:64, :]), R(Kc, Kc.t[:, c0 + 1, :]), selb[0:64, s0 + 1:s0 + 2],
                      R(hkf, hkf.t[0:64, :]), ALU.mult, ALU.add)
                P.stt(R(Kext, Kext.t[:, side * 128:(side + 1) * 128]), R(Kc, Kc.t[:, c0 + 2, :]),
                      selb[0:64, s0 + 2:s0 + 3], R(hkf, hkf.t[0:64, :]), ALU.mult, ALU.add)
                P.ts(R(hkf, hkf.t[:, 0:64]), R(Vc, Vc.t[:, c0, :]), selb[:, s0:s0 + 1], ALU.mult)
                P.stt(R(hkf, hkf.t[:, 0:64]), R(Vc, Vc.t[:, c0 + 1, :]), selb[:, s0 + 1:s0 + 2],
                      R(hkf, hkf.t[:, 0:64]), ALU.mult, ALU.add)
                P.stt(R(Vext, Vext.t[:, side, 0:64]), R(Vc, Vc.t[:, c0 + 2, :]), selb[:, s0 + 2:s0 + 3],
                      R(hkf, hkf.t[:, 0:64]), ALU.mult, ALU.add)
            steps = [(n, kk) for n in range(NT) for kk in range(3)]
            S_C(g, 0, 0)
            for si, (n, kk) in enumerate(steps):
                if si + 1 < len(steps):
                    S_C(g, *steps[si + 1])
                PV_C(g, n, kk)
                if kk == 0:
                    flush_pending()
                if kk == 2:
                    P.copy(R(Oc, Oc.t[:, 0, 0:272]), R(ps[4], ps[4].t[:, 0:272]))
                    ov = Oc.t[:, 0, 0:272].rearrange("p (r c) -> p r c", c=68)
                    P.tt(den4[:, 0:4], R(Oc, ov[:, :, 64]), esink[:, g * 4:(g + 1) * 4], ALU.add)
                    P.op("dve", lambda h: h.reciprocal(den4.t[:, 4:8], den4.t[:, 0:4]), [den4[:]], [den4[:]])
                    yb = yblk[nblk[0] % 2]
                    nblk[0] += 1
                    P.tt(R(yb, yb.t[:, 0:256].rearrange("p (r c) -> p r c", c=64)), R(Oc, ov[:, :, 0:64]),
                         R(den4, den4.t[:, 4:8].unsqueeze(2).broadcast_to([128, 4, 64])), ALU.mult)
                    defer_transpose(yb, 256, 16 + g * 2, 2, n * 128, 128)
            flush_pending()
        while ada_pending:
            ada_pending.pop(0)()
        ada_flush()
        P.pop()
        ymT = P.sbuf("ymT", [128, KC, TOK], BF16)

        if dbg in ("p3", "p4", "l1") and l == dbg_layer:
            o = P.dram("dbg_yT", [24 * 128, TOK], BF16, kind="ExternalOutput")
            P.dma(R(o, o.t[:].rearrange("(c p) n -> p c n", p=128)), yT[:], q="sp")
            if dbg == "p3":
                om = P.dram("dbg_mod", [DEPTH, 1, 6 * D], F32, kind="ExternalOutput")
                P.push()
                t_ = P.sbuf("dbg_t", [1, 6 * D], F32)
                for l_ in range(nlayers):
                    P.dma(t_[:], mod_d[l_, :, :], q="sp")
                    P.dma(om[l_, :, :], t_[:], q="sp")
                P.pop()
                P.emit()
                return nc

        P.push()
        wg = [P.sbuf("wg%d" % i, [128, KC, 512], BF16) for i in range(2)]
        wbr = [P.sbuf("wbr%d" % i, [128, 8, 512], BF16) for i in range(2)]
        ycb = P.sbuf("ycb", [128, NT, 512], F32)
        sig = [P.sbuf("sig%d" % i, [128, 512], BF16) for i in range(2)]
        gtmp = [P.sbuf("gtmp%d" % i, [128, 512], F32) for i in range(2)]
        ybf = [P.sbuf("ybf%d" % i, [128, 512], BF16) for i in range(2)]
        cnt = 0
        pend_tail = []

        def merge_tail(t, cb):
            P.copy(ybf[t % 2][:], ycb[:, t, :], eng="act")
            bank = 4 + t % 2
            for c in range(4):
                P.tr(R(ps[bank], psbf(bank)[:, c * 128:(c + 1) * 128]), R(ybf[t % 2], ybf[t % 2].t[:, c * 128:(c + 1) * 128]),
                     ident[:])
            P.copy(R(ymT, ymT.t[:, cb * 4:(cb + 1) * 4, t * 128:(t + 1) * 128]),
                   R(ps[bank], psbf(bank)[:, 0:512].rearrange("p (c n) -> p c n", n=128)),
                   eng="act" if t % 2 == 0 else "dve")

        for cb in range(4):
            for br, wsrc in enumerate((wa_in, wb_in, wc_in)):
                g_, w_ = wg[cnt % 2], wbr[cnt % 2]
                cnt += 1
                gc0 = O_G + br * D + cb * 512
                P.dma(g_[:], R(w_in, w_in.t[l, :, gc0:gc0 + 512].rearrange("(c p) n -> p c n", p=128)), q="pool")
                P.dma(w_[:], R(wsrc, wsrc.t[l, :, cb * 512:(cb + 1) * 512].rearrange("(c p) n -> p c n", p=128)),
                      q="pool")
                for t in range(NT):
                    if br == 0 and pend_tail:
                        pend_tail.pop(0)()
                    pg, pp = ps[(t % 2) * 2], ps[(t % 2) * 2 + 1]
                    for kc in range(KC):
                        P.mm(pg[:], hT[:, kc, t * 128:(t + 1) * 128], g_[:, kc, :], start=(kc == 0), stop=(kc == KC - 1))
                    for kc in range(8):
                        P.mm(pp[:], R(yT, yT.t[:, br * 8 + kc, t * 128:(t + 1) * 128]), w_[:, kc, :],
                             start=(kc == 0), stop=(kc == 7))
                    P.act(sig[t % 2][:], pg[:], AF.Sigmoid)
                    if br == 0:
                        P.tt(ycb[:, t, :], pp[:], sig[t % 2][:], ALU.mult)
                    else:
                        P.tt(gtmp[t % 2][:], pp[:], sig[t % 2][:], ALU.mult)
                        P.tt(ycb[:, t, :], ycb[:, t, :], gtmp[t % 2][:], ALU.add)
            for t in range(NT):
                pend_tail.append(lambda t=t, cb=cb: merge_tail(t, cb))
        while pend_tail:
            pend_tail.pop(0)()
        P.pop()

        P.push()
        wo = [P.sbuf("wo%d" % i, [128, KC, 512], BF16) for i in range(2)]
        g1b = P.sbuf("g1b", [128, D], F32)
        xq = [P.sbuf("xq%d" % i, [128, 512], F32) for i in range(2)]
        zq = [P.sbuf("zq%d" % i, [128, 512], F32) for i in range(2)]
        P.dma(g1b[:], bc_rows(mod_d, l * 6 * D + 2 * D, D), q="sp")
        for ob in range(4):
            w_ = wo[ob % 2]
            P.dma(w_[:], R(wo_in, wo_in.t[l, :, ob * 512:(ob + 1) * 512].rearrange("(c p) n -> p c n", p=128)), q="pool")
            for t in range(NT):
                pst = ps[t % 2]
                for kc in range(KC):
                    P.mm(pst[:], R(ymT, ymT.t[:, kc, t * 128:(t + 1) * 128]), w_[:, kc, :],
                         start=(kc == 0), stop=(kc == KC - 1))
                P.dma(xq[t % 2][:], x_src[t * 128:(t + 1) * 128, ob * 512:(ob + 1) * 512], q="sp")
                P.tt(zq[t % 2][:], pst[:], g1b[:, ob * 512:(ob + 1) * 512], ALU.mult)
                P.stt(zq[t % 2][:], xq[t % 2][:], ALPHA, zq[t % 2][:], ALU.mult, ALU.add)
                P.dma(x1p_d[t * 128:(t + 1) * 128, ob * 512:(ob + 1) * 512], zq[t % 2][:], q="sp")
        P.pop()
        P.pop()

        P.push()
        xt2 = [P.sbuf("xt2_%d" % i, [128, D], F32) for i in range(2)]
        x1t = [P.sbuf("x1t%d" % i, [128, D], F32) for i in range(2)]
        lng = P.sbuf("lng", [128, D], F32)
        lnb = P.sbuf("lnb", [128, D], F32)
        modA2 = P.sbuf("modA2", [128, D], F32)
        modB2 = P.sbuf("modB2", [128, D], F32)
        junk = [P.sbuf("junk_c%d" % i, [128, D], BF16) for i in range(2)]
        xn = [P.sbuf("xn_c%d" % i, [128, D], F32) for i in range(2)]
        h2f_ = [P.sbuf("h2f%d" % i, [128, D], F32) for i in range(2)]
        hb2 = [P.sbuf("hb2_%d" % i, [128, D], BF16) for i in range(2)]
        h2Tf = P.sbuf("h2Tf", [128, KC, 128], F32)
        rwf = P.sbuf("rwf", [128, KC, NEXP], F32)
        rbb = P.sbuf("rbb", [128, NEXP], F32)
        rsc = P.sbuf("rsc", [128, NT, NEXP], F32)
        rbi = P.sbuf("rbi", [128, NT, NEXP], F32)
        req = P.sbuf("req", [128, NT, NEXP], F32)
        rg2 = P.sbuf("rg2", [128, NT, NEXP], F32)
        rm1 = P.sbuf("rm1", [128, NT * 4], F32)
        rm2 = P.sbuf("rm2", [128, NT * 4], F32)
        rgs = P.sbuf("rgs", [128, NT * 4], F32)
        rgm = P.sbuf("rgm", [128, NT], F32)
        P.dma(lng[:], bc_rows(lnmg_in, l * D, D), q="sp")
        P.dma(lnb[:], bc_rows(lnmb_in, l * D, D), q="sp")
        P.dma(modB2[:], bc_rows(mod_d, l * 6 * D + 3 * D, D), q="sp")
        P.dma(modA2[:], bc_rows(mod_d, l * 6 * D + 4 * D, D), q="sp")
        P.ts(modA2[:], modA2[:], 1.0, ALU.add)
        P.dma(rwf[:], R(rw_in, rw_in.t[:].rearrange("(c p) e -> p c e", p=128)), q="sp")
        P.dma(rbb[:], bc_rows(rb_in, 0, NEXP), q="sp")
        for t in range(NT):
            P.dma(xt2[t % 2][:], x1p_d[t * 128:(t + 1) * 128, :], q="sp")
            layer_norm(xt2[t % 2], x1t[t % 2][:], lng, lnb, junk, xn)
            P.dma(xs_d[t * 128:(t + 1) * 128, :], x1t[t % 2][:], q="sp")
            h2f = h2f_[t % 2]
            layer_norm(x1t[t % 2], h2f[:], modA2, modB2, junk, xn)
            P.copy(hb2[t % 2][:], h2f[:], eng="act")
            transpose_to_hT(hb2[t % 2], t, t)
            for c4 in range(4):
                bank = 4 + c4
                for c in range(4):
                    kc = c4 * 4 + c
                    P.tr(R(ps[bank], ps[bank].t[:, c * 128:(c + 1) * 128]), h2f[:, kc * 128:(kc + 1) * 128], identf[:])
                P.copy(R(h2Tf, h2Tf.t[:, c4 * 4:(c4 + 1) * 4, :]),
                       R(ps[bank], ps[bank].t[:].rearrange("p (c n) -> p c n", n=128)),
                       eng="act")
            for kc in range(KC):
                P.mm(R(ps[1], ps[1].t[:, 0:NEXP]), h2Tf[:, kc, :], rwf[:, kc, :], start=(kc == 0), stop=(kc == KC - 1))
            P.act(R(rsc, rsc.t[:, t, :]), R(ps[1], ps[1].t[:, 0:NEXP]), AF.Sigmoid)
        NG = NT * 4
        g3v = lambda tl: R(tl, tl.t[:].rearrange("p t (g e) -> p (t g) e", e=4))
        bcg = lambda tl: R(tl, tl.t[:].unsqueeze(2).broadcast_to([128, NG, 4]))
        P.tt(rbi[:], rsc[:], R(rbb, rbb.t[:].unsqueeze(1).broadcast_to([128, NT, NEXP])), ALU.add)
        P.reduce(rm1[:], g3v(rbi), ALU.max)
        P.tt(g3v(req), g3v(rbi), bcg(rm1), ALU.is_equal)
        P.stt(g3v(rg2), g3v(req), -1e30, g3v(rbi), ALU.mult, ALU.add)
        P.reduce(rm2[:], g3v(rg2), ALU.max)
        P.tt(rgs[:], rm1[:], rm2[:], ALU.add)
        P.reduce(rgm[:], R(rgs, rgs.t[:].rearrange("p (t g) -> p t g", g=4)), ALU.max)
        P.tt(R(rgs, rgs.t[:].rearrange("p (t g) -> p t g", g=4)), R(rgs, rgs.t[:].rearrange("p (t g) -> p t g", g=4)),
             R(rgm, rgm.t[:].unsqueeze(2).broadcast_to([128, NT, 4])), ALU.is_equal)
        P.tt(g3v(req), g3v(rbi), bcg(rm2), ALU.is_ge)
        P.tt(g3v(req), g3v(req), bcg(rgs), ALU.mult)
        P.tt(rg2[:], rsc[:], req[:], ALU.mult)
        P.reduce(rgm[:], rg2[:], ALU.add)
        P.op("dve", lambda h: h.reciprocal(rgm.t[:], rgm.t[:]), [rgm[:]], [rgm[:]])
        P.tt(gates[:], rg2[:], R(rgm, rgm.t[:].unsqueeze(2).broadcast_to([128, NT, NEXP])), ALU.mult)
        P.pop()

        if dbg == "p4" and l == dbg_layer:
            o = P.dram("dbg_x1", [TOK, D], F32, kind="ExternalOutput")
            P.push()
            for t in range(NT):
                tt_ = P.sbuf("dbgx%d" % t, [128, D], F32)
                P.dma(tt_[:], xs_d[t * 128:(t + 1) * 128, :], q="sp")
                P.dma(o[t * 128:(t + 1) * 128, :], tt_[:], q="sp")
            o2 = P.dram("dbg_h2T", [D, TOK], BF16, kind="ExternalOutput")
            P.dma(R(o2, o2.t[:].rearrange("(c p) n -> p c n", p=128)), hT[:], q="sp")
            om = P.dram("dbg_mod", [DEPTH, 1, 6 * D], F32, kind="ExternalOutput")
            t_ = P.sbuf("dbg_tm", [1, 6 * D], F32)
            for l_ in range(nlayers):
                P.dma(t_[:], mod_d[l_, :, :], q="sp")
                P.dma(om[l_, :, :], t_[:], q="sp")
            o3 = P.dram("dbg_gates", [128, NT * NEXP], F32, kind="ExternalOutput")
            P.dma(o3[:], R(gates, gates.t[:].rearrange("p t e -> p (t e)")), q="sp")
            P.emit()
            return nc

        P.push()
        y_acc = P.sbuf("y_acc", [128, NT, D], F32)
        P.push()
        w13 = [P.sbuf("w13_%d" % i, [128, 2, KC, 256], BF16) for i in range(2)]
        hid = [P.sbuf("hid%d" % i, [128, 4, TOK], BF16) for i in range(2)]
        w2e = P.sbuf("w2e", [128, 4, D], BF16)
        sA = [P.sbuf("sA%d" % i, [128, 512], BF16) for i in range(2)]
        nh = 0
        for e in range(NEXP):
            hd = hid[e % 2]
            for fh in range(2):
                wb_ = w13[nh % 2]
                nh += 1
                for wi, wsrc in enumerate((w1_in, w3_in)):
                    P.dma(R(wb_, wb_.t[:, wi, :, :]),
                          R(wsrc, wsrc.t[l, e, :, fh * 256:(fh + 1) * 256].rearrange("(c p) f -> p c f", p=128)), q="pool")
                for tb in range(2):
                    for fc in range(2):
                        i_ = (tb * 2 + fc) % 2
                        pa, pb_ = ps[i_ * 2], ps[i_ * 2 + 1]
                        for kc in range(KC):
                            P.mm(pa[:], R(wb_, wb_.t[:, 0, kc, fc * 128:(fc + 1) * 128]),
                                 hT[:, kc, tb * 512:(tb + 1) * 512], start=(kc == 0), stop=(kc == KC - 1))
                        for kc in range(KC):
                            P.mm(pb_[:], R(wb_, wb_.t[:, 1, kc, fc * 128:(fc + 1) * 128]),
                                 hT[:, kc, tb * 512:(tb + 1) * 512], start=(kc == 0), stop=(kc == KC - 1))
                        P.act(sA[i_][:], pa[:], AF.Silu)
                        P.tt(R(hd, hd.t[:, fh * 2 + fc, tb * 512:(tb + 1) * 512]), pb_[:], sA[i_][:], ALU.mult)
            P.dma(w2e[:], R(w2_in, w2_in.t[l, e].rearrange("(c p) n -> p c n", p=128)), q="pool")
            for t in range(NT):
                for cb in range(4):
                    po = ps[4 + (t * 4 + cb) % 4]
                    for fc in range(4):
                        P.mm(po[:], R(hd, hd.t[:, fc, t * 128:(t + 1) * 128]), w2e[:, fc, cb * 512:(cb + 1) * 512],
                             start=(fc == 0), stop=(fc == 3))
                    ya_ = y_acc[:, t, cb * 512:(cb + 1) * 512]
                    if e == 0:
                        P.ts(ya_, po[:], gates[:, t, e:e + 1], ALU.mult)
                    else:
                        P.stt(ya_, po[:], gates[:, t, e:e + 1], ya_, ALU.mult, ALU.add)
        P.pop()
        g2b = P.sbuf("g2b", [128, D], F32)
        lng2 = P.sbuf("lng2", [128, D], F32)
        lnb2 = P.sbuf("lnb2", [128, D], F32)
        junk = [P.sbuf("junk_e%d" % i, [128, D], BF16) for i in range(2)]
        xn = [P.sbuf("xn_e%d" % i, [128, D], F32) for i in range(2)]
        x1l = [P.sbuf("x1l%d" % i, [128, D], F32) for i in range(2)]
        x2t = [P.sbuf("x2t%d" % i, [128, D], F32) for i in range(2)]
        P.dma(g2b[:], bc_rows(mod_d, l * 6 * D + 5 * D, D), q="sp")
        P.dma(lng2[:], bc_rows(lnfg_in, l * D, D), q="sp")
        P.dma(lnb2[:], bc_rows(lnfb_in, l * D, D), q="sp")
        for t in range(NT):
            P.dma(x1l[t % 2][:], xs_d[t * 128:(t + 1) * 128, :], q="sp")
            P.tt(y_acc[:, t, :], y_acc[:, t, :], g2b[:], ALU.mult)
            P.stt(x1l[t % 2][:], x1l[t % 2][:], ALPHA, y_acc[:, t, :], ALU.mult, ALU.add)
            layer_norm(x1l[t % 2], x2t[t % 2][:], lng2, lnb2, junk, xn)
            dst = out_d if l == nlayers - 1 else xs_d
            P.dma(dst[t * 128:(t + 1) * 128, :], x2t[t % 2][:], q="sp")
        P.pop()

    P.emit()
    return nc


_NC_CACHE = {}

_VEC3 = ("diff_lambda_q", "diff_lambda_k", "diff_subln", "mla_q_norm", "mla_kv_norm", "swa_sink",
         "b_ada", "ln_mix_g", "ln_mix_b", "ln_ffn_g", "ln_ffn_b")


def make_in_maps(inputs, nlayers=DEPTH):
    f = lambda k: np.ascontiguousarray(np.asarray(inputs[k]))
    shared = {}
    for k in inputs:
        if k in ("x", "c", "positions"):
            continue
        a = f(k)
        if k in _VEC3:
            a = a.reshape(DEPTH, 1, -1)
        elif k == "router_bias":
            a = a.reshape(1, NEXP)
        if a.ndim >= 3 and a.shape[0] == DEPTH and nlayers < DEPTH:
            a = np.ascontiguousarray(a[:nlayers])
        shared[k] = a
    x = f("x")
    c = f("c")
    pos = f("positions").astype(np.int32)
    maps = []
    for core in range(8):
        b, j = core // 4, core % 4
        sel = np.zeros((1, 8), np.float32)
        if j >= 1:
            sel[0, j - 1] = 1.0
        if j <= 2:
            sel[0, 4 + j] = 1.0
        m = dict(shared)
        m["x"] = np.ascontiguousarray(x[b, j * TOK:(j + 1) * TOK, :])
        m["c"] = np.ascontiguousarray(c[b:b + 1, :])
        m["positions"] = np.ascontiguousarray(pos[b, j * TOK:(j + 1) * TOK].reshape(TOK, 1))
        m["sel"] = sel
        maps.append(m)
    return maps


def kernel(**inputs):
    if "nc" not in _NC_CACHE:
        _NC_CACHE["nc"] = build()
    nc = _NC_CACHE["nc"]
    maps = make_in_maps(inputs)
    res = run_bass_kernel_spmd(nc, maps, core_ids=list(range(8)))
    out = np.empty((NB, S, D), np.float32)
    for core in range(8):
        b, j = core // 4, core % 4
        out[b, j * TOK:(j + 1) * TOK, :] = res.results[core]["out"]
    return out
```

```python
import math
import numpy as np
import concourse.bass as bass
import concourse.mybir as mybir
from concourse.bass_utils import run_bass_kernel_spmd

F32 = mybir.dt.float32
BF16 = mybir.dt.bfloat16
I32 = mybir.dt.int32
AF = mybir.ActivationFunctionType
ALU = mybir.AluOpType
AX = mybir.AxisListType

ENGS = ("pe", "act", "dve", "pool", "sp")
SB_BASE = 16512
SB_TOP = 229344


class Ref:
    __slots__ = ("ap", "keys")

    def __init__(self, ap, keys):
        self.ap = ap
        self.keys = tuple(keys)


class T:
    def __init__(self, t, key):
        self.t = t
        self.key = key

    def __getitem__(self, idx):
        return Ref(self.t[idx], (self.key,))

    def ref(self, ap):
        return Ref(ap, (self.key,))


class Op:
    __slots__ = ("eng", "fn", "deps", "flag", "kind", "ev", "idx")


class Prog:
    def __init__(self, nc, dma_k=None, same_engine_sync=True):
        self.nc = nc
        self.ops = {e: [] for e in ENGS}
        self.wstate = {}
        self.rstate = {}
        self.dma_k = dma_k or {"sp": 8, "pool": 8, "act": 4}
        self.ndma = {e: 0 for e in ENGS}
        self.ncc = 0
        self.same_engine_sync = same_engine_sync
        self.excl = set()
        self.sb_ptr = SB_BASE
        self.sb_stack = []
        self.sb_ranges = {}
        self.overlaps = {}
        self.sb_peak = SB_BASE
        self.nalloc = 0

    def push(self):
        self.sb_stack.append(self.sb_ptr)

    def pop(self):
        self.sb_ptr = self.sb_stack.pop()

    def sbuf(self, name, shape, dtype):
        esz = {F32: 4, BF16: 2, I32: 4}[dtype]
        n = 1
        for d_ in shape[1:]:
            n *= d_
        size = (n * esz + 31) // 32 * 32
        off = self.sb_ptr
        self.sb_ptr += size
        assert self.sb_ptr <= SB_TOP, "SBUF overflow at %s: %d" % (name, self.sb_ptr)
        self.sb_peak = max(self.sb_peak, self.sb_ptr)
        self.nalloc += 1
        key = "%s#%d" % (name, self.nalloc)
        t = self.nc.alloc_sbuf_tensor_at(key, list(shape), dtype, offset=off)
        ov = [key]
        for k2, (o2, e2) in self.sb_ranges.items():
            if o2 < off + size and off < e2:
                ov.append(k2)
                self.overlaps[k2].append(key)
        self.sb_ranges[key] = (off, off + size)
        self.overlaps[key] = ov
        return T(t, key)

    def psum(self, name, shape, dtype=F32):
        self.excl.add(name)
        return T(self.nc.alloc_psum_tensor(name, list(shape), dtype), name)

    def dram(self, name, shape, dtype, kind="Internal", **kw):
        return T(self.nc.dram_tensor(name, list(shape), dtype, kind=kind, **kw), name)

    def op(self, eng, fn, reads=(), writes=(), kind="c"):
        o = Op()
        o.eng = eng
        o.fn = fn
        o.kind = kind
        o.flag = False
        o.idx = len(self.ops[eng])
        deps = []
        rk = [k for r in reads for k in r.keys]
        wk = [k for w in writes for k in w.keys]
        for k0 in rk:
            for k in self.overlaps.get(k0, (k0,)):
                ev = self.wstate.get(k)
                if ev is not None:
                    deps.append(ev)
                if k in self.excl:
                    for ev in self.rstate.get(k, {}).values():
                        if not (ev[0] == "c" and ev[1] == eng):
                            deps.append(ev)
        for k0 in wk:
            for k in self.overlaps.get(k0, (k0,)):
                ev = self.wstate.get(k)
                if ev is not None:
                    deps.append(ev)
                for ev in self.rstate.get(k, {}).values():
                    deps.append(ev)
        if kind == "d":
            q = eng
            i = self.ndma[q]
            self.ndma[q] += 1
            K = self.dma_k[q]
            o.ev = ("d", q, i % K, 16 * (i // K + 1))
            if i >= K:
                deps.append(("d", q, i % K, 16 * (i // K)))
        elif kind == "cc":
            o.ev = ("cc", self.ncc)
            self.ncc += 1
        else:
            o.ev = ("c", eng, o.idx)
        fd = []
        for ev in deps:
            if ev[0] == "c":
                if ev[1] == eng:
                    if eng == "pe" or eng == "sp" or not self.same_engine_sync:
                        continue
                self.ops[ev[1]][ev[2]].flag = True
            fd.append(ev)
        o.deps = fd
        self.ops[eng].append(o)
        for k in wk:
            self.wstate[k] = o.ev
            self.rstate[k] = {}
        for k in rk:
            d = self.rstate.setdefault(k, {})
            if o.ev[0] == "c":
                d[eng] = o.ev
            elif o.ev[0] == "d":
                d[o.ev[:3]] = o.ev
            else:
                d[o.ev] = o.ev
        return o

    def emit(self, final_wait_eng="sp"):
        nc = self.nc
        sems = {e: nc.alloc_semaphore("s_" + e) for e in ENGS if e != "sp"}
        dsems = {
            q: [nc.alloc_semaphore("d_%s%d" % (q, j)) for j in range(self.dma_k[q])]
            for q in self.dma_k
        }
        ccsems = [nc.alloc_semaphore("cc%d" % i) for i in range(self.ncc)]
        semval = {}
        for e in ENGS:
            c = 0
            for o in self.ops[e]:
                if o.flag and o.kind == "c":
                    c += 1
                    semval[(e, o.idx)] = c
        final = []
        for q in self.dma_k:
            n = self.ndma[q]
            K = self.dma_k[q]
            for j in range(K):
                cnt = (n - j + K - 1) // K if n > j else 0
                if cnt > 0:
                    final.append((q, j, 16 * cnt))

        def run(e, h):
            seen = {}
            for o in self.ops[e]:
                for ev in o.deps:
                    if ev[0] == "c":
                        key = ("c", ev[1])
                        val = semval[(ev[1], ev[2])]
                        s = sems[ev[1]]
                    elif ev[0] == "d":
                        key = ("d", ev[1], ev[2])
                        val = ev[3]
                        s = dsems[ev[1]][ev[2]]
                    else:
                        key = ev
                        val = 1
                        s = ccsems[ev[1]]
                    if seen.get(key, 0) >= val:
                        continue
                    seen[key] = val
                    h.wait_ge(s, val)
                ins = o.fn(h)
                if o.kind == "d":
                    ins.then_inc(dsems[o.ev[1]][o.ev[2]], 16)
                elif o.kind == "cc":
                    ins.then_inc(ccsems[o.ev[1]], 1)
                elif o.flag:
                    ins.then_inc(sems[e], 1)
            if e == final_wait_eng:
                for q, j, v in final:
                    if seen.get(("d", q, j), 0) < v:
                        h.wait_ge(dsems[q][j], v)

        with nc.Block() as block:

            @block.tensor
            def _(h):
                run("pe", h)

            @block.scalar
            def _(h):
                run("act", h)

            @block.vector
            def _(h):
                run("dve", h)

            @block.gpsimd
            def _(h):
                run("pool", h)

            @block.sync
            def _(h):
                run("sp", h)

    def dma(self, out, in_, q="sp", **kw):
        return self.op(q, lambda h: h.dma_start(out.ap, in_.ap, **kw), [in_], [out], kind="d")

    def mm(self, out, lhsT, rhs, start=True, stop=True, **kw):
        return self.op(
            "pe",
            lambda h: h.matmul(out.ap, lhsT.ap, rhs.ap, start=start, stop=stop, **kw),
            [lhsT, rhs],
            [out],
        )

    def tr(self, out, in_, ident):
        return self.op("pe", lambda h: h.transpose(out.ap, in_.ap, ident.ap), [in_, ident], [out])

    def act(self, out, in_, func, bias=None, scale=None, accum_out=None):
        kw = {}
        rd = [in_]
        wr = [out]
        if bias is not None:
            if isinstance(bias, Ref):
                kw["bias"] = bias.ap
                rd.append(bias)
            else:
                kw["bias"] = bias
        if scale is not None:
            if isinstance(scale, Ref):
                kw["scale"] = scale.ap
                rd.append(scale)
            else:
                kw["scale"] = scale
        if accum_out is not None:
            kw["accum_out"] = accum_out.ap
            wr.append(accum_out)
        return self.op("act", lambda h: h.activation(out.ap, in_.ap, func, **kw), rd, wr)

    def copy(self, out, in_, eng="dve"):
        if eng == "act":
            return self.op(eng, lambda h: h.copy(out.ap, in_.ap), [in_], [out])
        return self.op(eng, lambda h: h.tensor_copy(out.ap, in_.ap), [in_], [out])

    def tt(self, out, in0, in1, op, eng="dve"):
        return self.op(eng, lambda h: h.tensor_tensor(out.ap, in0.ap, in1.ap, op), [in0, in1], [out])

    def ts(self, out, in0, s1, op0, s2=None, op1=None, accum_out=None, eng="dve"):
        rd = [in0]
        wr = [out]
        a1 = s1
        a2 = s2
        if isinstance(s1, Ref):
            rd.append(s1)
            a1 = s1.ap
        if isinstance(s2, Ref):
            rd.append(s2)
            a2 = s2.ap
        kw = {}
        if op1 is not None:
            kw["op1"] = op1
        if accum_out is not None:
            kw["accum_out"] = accum_out.ap
            wr.append(accum_out)
        return self.op(eng, lambda h: h.tensor_scalar(out.ap, in0.ap, a1, a2, op0, **kw), rd, wr)

    def stt(self, out, in0, scalar, in1, op0, op1, eng="dve"):
        rd = [in0, in1]
        a = scalar
        if isinstance(scalar, Ref):
            rd.append(scalar)
            a = scalar.ap
        return self.op(
            eng, lambda h: h.scalar_tensor_tensor(out.ap, in0.ap, a, in1.ap, op0, op1), rd, [out]
        )

    def memset(self, out, val, eng="dve"):
        return self.op(eng, lambda h: h.memset(out.ap, val), [], [out])

    def reduce(self, out, in_, op, axis=AX.X, eng="dve"):
        return self.op(eng, lambda h: h.tensor_reduce(out.ap, in_.ap, axis, op), [in_], [out])


D = 2048
S = 4096
NB = 2
DEPTH = 2
TOK = 1024
NT = TOK // 128
KC = D // 128
IN_W = 11584
O_AQ, O_AK, O_AV, O_CQ, O_CKV, O_KR, O_SQ, O_SK, O_SV, O_G = (
    0, 1024, 2048, 3072, 3584, 3840, 3904, 4928, 5184, 5440)
KR_A, KR_BN, KR_BR, KR_C, FK = 0, 1024, 2048, 2112, 2368
VC_A, VC_B, VC_C, FV = 0, 1024, 2048, 2304
ALPHA = (2 * DEPTH) ** 0.25
LN_EPS = 1e-5
RMS_EPS = 1e-6
NEXP = 16
EH = 512
TWO_PI = 2.0 * math.pi


def build(dbg=None, nlayers=DEPTH, moe=True, dbg_layer=0):
    nc = bass.Bass("TRN2", target_bir_lowering=False)
    P = Prog(nc)
    LW = nlayers
    din = lambda n, s, dt=F32: P.dram(n, [LW] + list(s[1:]) if (len(s) >= 3 and s[0] == DEPTH) else s, dt,
                                      kind="ExternalInput")
    x_in = din("x", [TOK, D])
    c_in = din("c", [1, D])
    pos_in = din("positions", [TOK, 1], I32)
    sel_in = din("sel", [1, 8])
    w_in = din("w_in", [DEPTH, D, IN_W])
    lamq_in = din("diff_lambda_q", [DEPTH, 1, 128])
    lamk_in = din("diff_lambda_k", [DEPTH, 1, 128])
    subln_in = din("diff_subln", [DEPTH, 1, 128])
    qn_in = din("mla_q_norm", [DEPTH, 1, 512])
    kvn_in = din("mla_kv_norm", [DEPTH, 1, 256])
    wuq_in = din("mla_w_uq", [DEPTH, 512, 1536])
    wukv_in = din("mla_w_ukv", [DEPTH, 256, 2048])
    sink_in = din("swa_sink", [DEPTH, 1, 16])
    wa_in = din("w_branch_a", [DEPTH, 1024, D])
    wb_in = din("w_branch_b", [DEPTH, 1024, D])
    wc_in = din("w_branch_c", [DEPTH, 1024, D])
    wo_in = din("w_out", [DEPTH, D, D])
    wada_in = din("w_ada", [DEPTH, D, 6 * D])
    bada_in = din("b_ada", [DEPTH, 1, 6 * D])
    lnmg_in = din("ln_mix_g", [DEPTH, 1, D])
    lnmb_in = din("ln_mix_b", [DEPTH, 1, D])
    lnfg_in = din("ln_ffn_g", [DEPTH, 1, D])
    lnfb_in = din("ln_ffn_b", [DEPTH, 1, D])
    rw_in = din("router_w", [D, NEXP])
    rb_in = din("router_bias", [1, NEXP])
    if moe:
        w1_in = din("expert_w1", [DEPTH, NEXP, D, EH])
        w3_in = din("expert_w3", [DEPTH, NEXP, D, EH])
        w2_in = din("expert_w2", [DEPTH, NEXP, EH, D])
    out_d = P.dram("out", [TOK, D], F32, kind="ExternalOutput")

    mod_d = P.dram("mod_d", [DEPTH, 1, 6 * D], F32)
    xs_d = P.dram("xs_d", [TOK, D], F32)
    x1p_d = P.dram("x1p_d", [TOK, D], F32)
    qTA_d = P.dram("qTA_d", [1024, TOK], BF16)
    qTBn_d = P.dram("qTBn_d", [1024, TOK], BF16)
    qTBr_d = P.dram("qTBr_d", [512, TOK], BF16)
    qTC_d = P.dram("qTC_d", [1024, TOK], BF16)
    def xch(name, shape):
        src = [P.dram("%s_s%d" % (name, l), shape, BF16) for l in range(DEPTH)]
        dst = [P.dram("%s_a%d" % (name, l), [4 * shape[0], shape[1]], BF16) for l in range(DEPTH)]
        return src, dst
    kA_s, kA_a = zip(*[xch("kA%d" % m, [512, TOK]) for m in range(2)])
    kB_s, kB_a = zip(*[xch("kB%d" % g, [512, TOK]) for g in range(2)])
    kRC_s, kRC_a = xch("kRC", [320, TOK])
    vA_s, vA_a = zip(*[xch("vA%d" % g, [TOK, 512]) for g in range(2)])
    vB_s, vB_a = zip(*[xch("vB%d" % g, [TOK, 512]) for g in range(2)])
    vC_s, vC_a = xch("vC", [TOK, 256])

    def bc_rows(t, row_ap_offset, n):
        return t.ref(bass.AP(t.t, row_ap_offset, [[0, 128], [1, n]]))

    identf = P.sbuf("identf", [128, 128], F32)
    ident = P.sbuf("ident", [128, 128], BF16)
    onesf = P.sbuf("onesf", [128, 128], F32)
    P.memset(identf[:], 1.0, eng="pool")
    P.op("pool", lambda h: h.affine_select(identf.t[:], identf.t[:], [[-1, 128]], ALU.is_equal, 0.0,
                                           base=0, channel_multiplier=1), [identf[:]], [identf[:]])
    P.copy(ident[:], identf[:], eng="pool")
    P.memset(onesf[:], 1.0, eng="pool")

    ps = [P.psum("ps%d" % i, [128, 512], F32) for i in range(8)]

    def psbf(i):
        return ps[i].t[:].bitcast(BF16)

    hT = P.sbuf("hT", [128, KC, TOK], BF16)
    stats = P.sbuf("stats", [128, 4, 6], F32)
    mv = P.sbuf("mv", [128, 8], F32)
    cosT = P.sbuf("cosT", [128, NT, 32], F32)
    sinT = P.sbuf("sinT", [128, NT, 32], F32)

    caT = P.sbuf("caT", [128, KC, 1], F32)
    P.push()
    cT = P.sbuf("cT", [128, KC, 1], F32)
    P.dma(cT[:], c_in.ref(bass.AP(c_in.t, 0, [[1, 128], [128, KC], [1, 1]])), q="sp", allow_slow_non_contiguous=True)
    P.act(caT[:], cT[:], AF.Silu)
    def ada_ln(l, eng, q, CW, defer=None):
        wada_b = [P.sbuf("wada_b%d" % i, [128, CW], F32) for i in range(3)]
        accs = [P.sbuf("ada_acc%d" % i, [128, CW], F32) for i in range(2)]
        modrow = P.sbuf("modrow", [128, CW], F32)
        badab = P.sbuf("badab", [128, CW], F32)
        ncb = 6 * D // CW
        seq = [(cb, kc) for cb in range(ncb) for kc in range(KC)]

        def load(i):
            cb, kc = seq[i]
            P.dma(wada_b[i % 3][:], wada_in[l, kc * 128:(kc + 1) * 128, cb * CW:(cb + 1) * CW], q=q)

        def colsum(cb, acc):
            P.dma(badab[:], bc_rows(bada_in, l * 6 * D + cb * CW, CW), q="sp")
            for n4 in range(CW // 512):
                P.mm(ps[7][:], onesf[:], acc[:, n4 * 512:(n4 + 1) * 512])
                P.tt(modrow[:, n4 * 512:(n4 + 1) * 512], ps[7][:], badab[:, n4 * 512:(n4 + 1) * 512], ALU.add)
            P.dma(mod_d[l, :, cb * CW:(cb + 1) * CW], modrow[0:1, :], q="sp")

        def fma_block(cb):
            for kc in range(KC):
                i = cb * KC + kc
                if i == 0:
                    load(0)
                    load(1)
                if i + 2 < len(seq):
                    load(i + 2)
                wb = wada_b[i % 3]
                acc = accs[cb % 2]
                if kc == 0:
                    P.ts(acc[:], wb[:], caT[:, kc, :], ALU.mult, eng=eng)
                elif eng == "dve":
                    P.stt(acc[:], wb[:], caT[:, kc, :], acc[:], ALU.mult, ALU.add, eng=eng)
                else:
                    P.ts(wb[:], wb[:], caT[:, kc, :], ALU.mult, eng=eng)
                    P.tt(acc[:], acc[:], wb[:], ALU.add, eng=eng)

        if defer is None:
            for cb in range(ncb):
                fma_block(cb)
                colsum(cb, accs[cb % 2])
        else:
            fma_block(0)
            fma_block(1)
            for cb in range(ncb):
                def step(cb=cb):
                    colsum(cb, accs[cb % 2])
                    if cb + 2 < ncb:
                        fma_block(cb + 2)
                defer.append(step)

    posi = P.sbuf("posi", [128, NT, 1], I32)
    posf = P.sbuf("posf", [128, NT, 1], F32)
    P.dma(posi[:], pos_in.ref(bass.AP(pos_in.t, 0, [[1, 128], [128, NT], [1, 1]])), q="sp", allow_slow_non_contiguous=True)
    P.copy(posf[:], posi[:])
    ii = P.sbuf("iota_i", [128, 32], I32)
    iif = P.sbuf("iota_f", [128, 32], F32)
    invf = P.sbuf("invf", [128, 32], F32)
    for i_ in range(32):
        P.memset(invf[:, i_:i_ + 1], float(np.float32(10000.0) ** np.float32(-(2.0 * i_) / 64.0)), eng="pool")
    ang = P.sbuf("ang", [128, NT, 32], F32)
    ang2 = P.sbuf("ang2", [128, NT, 32], F32)
    P.tt(ang[:], Ref(posf.t[:].broadcast_to([128, NT, 32]), posf[:].keys),
         Ref(invf.t[:].unsqueeze(1).broadcast_to([128, NT, 32]), invf[:].keys), ALU.mult)
    ki = P.sbuf("ki", [128, NT, 32], I32)
    kf = P.sbuf("kf", [128, NT, 32], F32)
    msk = P.sbuf("msk", [128, NT, 32], F32)

    def sin_of(out, a_in):
        P.ts(kf[:], a_in, 1.0 / TWO_PI, ALU.mult)
        P.copy(ki[:], kf[:])
        P.copy(kf[:], ki[:])
        P.stt(ang2[:], kf[:], -TWO_PI, a_in, ALU.mult, ALU.add)
        P.ts(msk[:], ang2[:], math.pi, ALU.is_gt)
        P.stt(ang2[:], msk[:], -TWO_PI, ang2[:], ALU.mult, ALU.add)
        P.ts(msk[:], ang2[:], -math.pi, ALU.is_lt)
        P.stt(ang2[:], msk[:], TWO_PI, ang2[:], ALU.mult, ALU.add)
        P.ts(ang2[:], ang2[:], math.pi, ALU.min, -math.pi, ALU.max)
        P.act(out, ang2[:], AF.Sin)

    sin_of(sinT[:], ang[:])
    P.ts(ang[:], ang[:], math.pi / 2, ALU.add)
    sin_of(cosT[:], ang[:])
    P.pop()

    if dbg == "p0":
        P.push()
        o = P.dram("dbg_mod", [DEPTH, 1, 6 * D], F32, kind="ExternalOutput")
        for l in range(DEPTH):
            t_ = P.sbuf("dbg_t%d" % l, [1, 6 * D], F32)
            P.dma(t_[:], mod_d[l, :, :], q="sp")
            P.dma(o[l, :, :], t_[:], q="sp")
        o2 = P.dram("dbg_cs", [128, 2 * NT * 32], F32, kind="ExternalOutput")
        P.dma(o2[:, 0:NT * 32], cosT.ref(cosT.t[:].rearrange("p a b -> p (a b)")), q="sp")
        P.dma(o2[:, NT * 32:], sinT.ref(sinT.t[:].rearrange("p a b -> p (a b)")), q="sp")
        P.emit()
        return nc


    ADA_CW = 512
    ADA_NCB = 6 * D // ADA_CW
    ada_w = [P.sbuf("ada_w%d" % i, [128, ADA_CW], F32) for i in range(4)]
    ada_acc = [P.sbuf("ada_acc%d" % i, [128, ADA_CW], F32) for i in range(2)]
    ada_row = P.sbuf("ada_row", [128, ADA_CW], F32)
    ada_b = P.sbuf("ada_b", [128, ADA_CW], F32)
    ada_seq = [(l_, cb, kc) for l_ in range(nlayers) for cb in range(ADA_NCB) for kc in range(KC)]
    ada_pos = [0]

    def ada_load(i):
        l_, cb, kc = ada_seq[i]
        P.dma(ada_w[i % 4][:], wada_in[l_, kc * 128:(kc + 1) * 128, cb * ADA_CW:(cb + 1) * ADA_CW], q="sp")

    for i_ in range(3):
        ada_load(i_)

    def ada_tick(n=1):
        for _ in range(n):
            i = ada_pos[0]
            if i >= len(ada_seq):
                return
            ada_pos[0] += 1
            if i + 3 < len(ada_seq):
                ada_load(i + 3)
            l_, cb, kc = ada_seq[i]
            acc = ada_acc[(l_ * ADA_NCB + cb) % 2]
            wb = ada_w[i % 4]
            if kc == 0:
                P.ts(acc[:], wb[:], caT[:, kc, :], ALU.mult)
            else:
                P.stt(acc[:], wb[:], caT[:, kc, :], acc[:], ALU.mult, ALU.add)
            if kc == KC - 1:
                P.dma(ada_b[:], bc_rows(bada_in, l_ * 6 * D + cb * ADA_CW, ADA_CW), q="sp")
                P.mm(ps[7][:, 0:ADA_CW], onesf[:], acc[:])
                P.tt(ada_row[:], ps[7][:, 0:ADA_CW], ada_b[:], ALU.add)
                P.dma(mod_d[l_, :, cb * ADA_CW:(cb + 1) * ADA_CW], ada_row[0:1, :], q="sp")

    def ada_flush(upto=None):
        upto = len(ada_seq) if upto is None else min(upto, len(ada_seq))
        while ada_pos[0] < upto:
            ada_tick()

    ada_tick((2 * D // ADA_CW) * KC)

    def R(tobj, ap):
        return Ref(ap, (tobj.key,))

    gates = P.sbuf("gates", [128, NT, NEXP], F32)
    selb = P.sbuf("selb", [128, 8], F32)
    P.dma(selb[:], bc_rows(sel_in, 0, 8), q="sp")
    validp = P.sbuf("validp", [128, 1], F32)
    validn = P.sbuf("validn", [128, 1], F32)
    P.reduce(validp[:], selb[:, 0:3], ALU.add)
    P.reduce(validn[:], selb[:, 4:7], ALU.add)
    maskP = P.sbuf("maskP", [128, 128], BF16)
    maskN = P.sbuf("maskN", [128, 128], BF16)
    mtmp = P.sbuf("mtmp", [128, 128], F32)
    for mk, sg in ((maskP, 1), (maskN, -1)):
        P.memset(mtmp[:], 1.0, eng="pool")
        P.op("pool", lambda h, sg=sg: h.affine_select(mtmp.t[:], mtmp.t[:], [[-sg, 128]], ALU.is_ge, 0.0,
                                                      base=0, channel_multiplier=sg), [mtmp[:]], [mtmp[:]])
        P.copy(mk[:], mtmp[:], eng="pool")
    s12_ = [P.sbuf("s12_%d" % i, [128, 4], F32) for i in range(2)]
    sm_ = [P.sbuf("sm_%d" % i, [128, 16], F32) for i in range(2)]
    lnst_ = [P.sbuf("lnst_%d" % i, [128, 4, 6], F32) for i in range(2)]
    lncnt = [0]

    def rstd_from(out, var_ref, eps):
        P.ts(out, var_ref, eps, ALU.add)
        P.act(out, out, AF.Ln)
        P.act(out, out, AF.Exp, scale=-0.5)

    def layer_norm(xt_, out_ref, A, B, junk, xn):
        i_ = lncnt[0] % 2
        lncnt[0] += 1
        sm, xn, st = sm_[i_], xn[i_], lnst_[i_]
        for c4 in range(4):
            P.op("dve", lambda h, c4=c4: h.bn_stats(st.t[:, c4, :], xt_.t[:, c4 * 512:(c4 + 1) * 512]),
                 [xt_[:]], [st[:]])
        P.op("dve", lambda h: h.bn_aggr(sm.t[:, 0:2], st.t[:]), [st[:]], [sm[:]])
        rstd_from(sm[:, 3:4], sm[:, 1:2], LN_EPS)
        P.stt(xn[:], xt_[:], sm[:, 0:1], A[:], ALU.subtract, ALU.mult)
        P.stt(out_ref, xn[:], sm[:, 3:4], B[:], ALU.mult, ALU.add)

    def transpose_to_hT(hb, t, evac_i):
        for g8 in range(2):
            bank = 2 + g8
            for c in range(8):
                kc = g8 * 8 + c
                P.tr(R(ps[bank], psbf(bank)[:, c * 128:(c + 1) * 128]), hb[:, kc * 128:(kc + 1) * 128], ident[:])
            src = R(ps[bank], psbf(bank)[:, 0:1024].rearrange("p (c n) -> p c n", n=128))
            dst = R(hT, hT.t[:, g8 * 8:(g8 + 1) * 8, t * 128:(t + 1) * 128])
            P.copy(dst, src, eng="act")

    def rope(src_ap, src_t, dst_ap, dst_t, t, nh, tmps):
        X = src_ap.rearrange("p (h two d) -> p h two d", two=2, d=32)
        Y = dst_ap.rearrange("p (h two d) -> p h two d", two=2, d=32)
        x1, x2 = R(src_t, X[:, :, 0, :]), R(src_t, X[:, :, 1, :])
        y1, y2 = R(dst_t, Y[:, :, 0, :]), R(dst_t, Y[:, :, 1, :])
        cb_ = R(cosT, cosT.t[:, t, :].unsqueeze(1).broadcast_to([128, nh, 32]))
        sb_ = R(sinT, sinT.t[:, t, :].unsqueeze(1).broadcast_to([128, nh, 32]))
        ta, tb_, tc_, td_ = tmps
        a_ = R(ta, ta.t[:, 0:nh, :])
        b_ = R(tb_, tb_.t[:, 0:nh, :])
        c_ = R(tc_, tc_.t[:, 0:nh, :])
        d_ = R(td_, td_.t[:, 0:nh, :])
        P.tt(a_, x1, cb_, ALU.mult)
        P.tt(b_, x2, sb_, ALU.mult)
        P.tt(c_, x2, cb_, ALU.mult)
        P.tt(d_, x1, sb_, ALU.mult)
        P.tt(y1, a_, b_, ALU.subtract)
        P.tt(y2, c_, d_, ALU.add)

    def rms_norm_tile(src, n, gain, out, junk):
        i_ = lncnt[0] % 2
        lncnt[0] += 1
        s12, sm = s12_[i_], sm_[i_]
        P.memset(s12[:, 2:3], 0.0)
        P.act(junk, src, AF.Square, accum_out=s12[:, 2:3])
        P.ts(sm[:, 8:9], s12[:, 2:3], 1.0 / n, ALU.mult)
        rstd_from(sm[:, 9:10], sm[:, 8:9], RMS_EPS)
        P.stt(out, src, sm[:, 9:10], gain, ALU.mult, ALU.mult)

    for l in range(nlayers):
        lam_init = 0.8 - 0.6 * math.exp(-0.3 * l)
        x_src = x_in if l == 0 else xs_d
        P.push()
        wblk = [P.sbuf("wblk%d" % i, [128, KC, 512], BF16) for i in range(2)]
        xt = [P.sbuf("xt%d" % i, [128, D], F32) for i in range(2)]
        modA = P.sbuf("modA", [128, D], F32)
        modB = P.sbuf("modB", [128, D], F32)
        junk = [P.sbuf("junk%d" % i, [128, D], BF16) for i in range(2)]
        xn = [P.sbuf("xn%d" % i, [128, D], F32) for i in range(2)]
        hb = [P.sbuf("hb%d" % i, [128, D], BF16) for i in range(2)]
        P.dma(modB[:], bc_rows(mod_d, l * 6 * D + 0 * D, D), q="sp")
        P.dma(modA[:], bc_rows(mod_d, l * 6 * D + 1 * D, D), q="sp")
        P.ts(modA[:], modA[:], 1.0, ALU.add)
        for t in range(NT):
            P.dma(xt[t % 2][:], x_src[t * 128:(t + 1) * 128, :], q="sp")
            layer_norm(xt[t % 2], hb[t % 2][:], modA, modB, junk, xn)
            transpose_to_hT(hb[t % 2], t, t)
        P.pop()
        if dbg == "ln1" and l == 0:
            o = P.dram("dbg_hT", [D, TOK], BF16, kind="ExternalOutput")
            P.dma(R(o, o.t[:].rearrange("(c p) n -> p c n", p=128)), hT[:], q="sp")
            P.emit()
            return nc

        P.push()
        wblk = [P.sbuf("wblk%d" % i, [128, KC, 512], BF16) for i in range(2)]
        stageT = [P.sbuf("stageT%d" % i, [128, 4, TOK], BF16) for i in range(2)]
        stageV = [P.sbuf("stageV%d" % i, [128, NT, 512], BF16) for i in range(2)]
        cqT = P.sbuf("cqT", [128, 4, TOK], BF16)
        ckvT = P.sbuf("ckvT", [128, 2, TOK], BF16)
        wuq = P.sbuf("wuq", [128, 4, 1536], BF16)
        wukv = P.sbuf("wukv", [128, 2, 2048], BF16)
        tok = [P.sbuf("tok%d" % i, [128, 512], BF16) for i in range(2)]
        rt = [P.sbuf("rt%d" % i, [128, 8, 32], F32) for i in range(4)]
        qnb = P.sbuf("qnb", [128, 512], F32)
        kvnb = P.sbuf("kvnb", [128, 256], F32)
        junk2_ = [P.sbuf("junk2_%d" % i, [128, 512], BF16) for i in range(2)]
        P.dma(wuq[:], R(wuq_in, wuq_in.t[l].rearrange("(c p) n -> p c n", p=128)), q="pool")
        P.dma(wukv[:], R(wukv_in, wukv_in.t[l].rearrange("(c p) n -> p c n", p=128)), q="pool")
        P.dma(qnb[:], bc_rows(qn_in, l * 512, 512), q="sp")
        P.dma(kvnb[:], bc_rows(kvn_in, l * 256, 256), q="sp")

        nst = [0, 0]

        def flushT(st, nchunk, dst_t, row0, parts=128):
            if parts == 128:
                P.dma(R(dst_t, dst_t.t[row0:row0 + nchunk * 128, :].rearrange("(c p) n -> p c n", p=128)),
                      R(st, st.t[:, 0:nchunk, :]), q="sp")
            else:
                P.dma(R(dst_t, dst_t.t[row0:row0 + parts, :]), R(st, st.t[0:parts, 0, :]), q="sp")

        def flushV(sv, ncol, dst_t):
            P.dma(R(dst_t, dst_t.t[:, 0:ncol].rearrange("(t p) c -> p t c", p=128)),
                  R(sv, sv.t[:, :, 0:ncol]), q="sp")

        def tok_transpose(tk, ncol, st, t, bank, c_off=0, eng="act"):
            nchunk = (ncol + 127) // 128
            for c in range(nchunk):
                w_ = min(128, ncol - c * 128)
                P.tr(R(ps[bank], psbf(bank)[0:w_, c * 128:(c + 1) * 128]), R(tk, tk.t[:, c * 128:c * 128 + w_]),
                     ident[:])
            if ncol % 128 == 0:
                src = R(ps[bank], psbf(bank)[:, 0:nchunk * 128].rearrange("p (c n) -> p c n", n=128))
                dst = R(st, st.t[:, c_off:c_off + nchunk, t * 128:(t + 1) * 128])
            else:
                src = R(ps[bank], psbf(bank)[0:ncol, 0:128])
                dst = R(st, st.t[0:ncol, c_off, t * 128:(t + 1) * 128])
            P.copy(dst, src, eng=eng)

        def tok_transpose_view(tk, c0_, ncol, st, t, bank):
            P.tr(R(ps[bank], psbf(bank)[0:ncol, 0:128]), R(tk, tk.t[:, c0_:c0_ + ncol]), ident[:])
            P.copy(R(st, st.t[0:ncol, 0, t * 128:(t + 1) * 128]), R(ps[bank], psbf(bank)[0:ncol, 0:128]), eng="act")

        blocks = [
            (O_AQ, 512, "ropeT", (qTA_d, 0)), (O_AQ + 512, 512, "ropeT", (qTA_d, 512)),
            (O_AK, 512, "ropeT", (kA_s[0][l], 0)), (O_AK + 512, 512, "ropeT", (kA_s[1][l], 0)),
            (O_AV, 512, "v", vA_s[0][l]), (O_AV + 512, 512, "v", vA_s[1][l]),
            (O_CQ, 512, "cq", None), (O_CKV, 320, "ckvkr", None),
            (O_SQ, 512, "ropeT", (qTC_d, 0)), (O_SQ + 512, 512, "ropeT", (qTC_d, 512)),
            (O_SK, 512, "ckcv", None),
        ]
        rg = [[0, 1, 2, 3], [4, 5, 6, 7]]

        def gather(src_t, dst_t):
            P.op("pool", lambda h, src_t=src_t, dst_t=dst_t: h.collective_compute(
                "AllGather", ALU.bypass, replica_groups=rg, ins=[src_t.t[:]], outs=[dst_t.t[:]]),
                [src_t[:]], [dst_t[:]], kind="cc")

        cc_after = {4: [(kA_s[0][l], kA_a[0][l])], 5: [(kA_s[1][l], kA_a[1][l])],
                    6: [(vA_s[0][l], vA_a[0][l])], 7: [(vA_s[1][l], vA_a[1][l])]}
        for bi, (c0, ncol, kind, dest) in enumerate(blocks):
            for pr in cc_after.get(bi, ()):
                gather(*pr)
            wb = wblk[bi % 2]
            P.dma(R(wb, wb.t[:, :, 0:ncol]),
                  R(w_in, w_in.t[l, :, c0:c0 + ncol].rearrange("(c p) n -> p c n", p=128)), q="pool")
            if kind in ("ropeT", "ckvkr", "ckcv"):
                st = stageT[nst[0] % 2]
                nst[0] += 1
            if kind in ("v", "ckcv"):
                sv = stageV[nst[1] % 2]
                nst[1] += 1
            pend_tr = []

            def flush_tr():
                while pend_tr:
                    pend_tr.pop(0)()

            for t in range(NT):
                pb = t % 2
                pst = ps[pb]
                for kc in range(KC):
                    P.mm(pst[:, 0:ncol], hT[:, kc, t * 128:(t + 1) * 128], wb[:, kc, 0:ncol],
                         start=(kc == 0), stop=(kc == KC - 1))
                flush_tr()
                tk = tok[t % 2]
                if kind == "ropeT":
                    rope(pst.t[:, 0:512], pst, tk.t[:, 0:512], tk, t, 8, rt)
                    pend_tr.append(lambda tk=tk, st=st, t=t, pb=pb: tok_transpose(tk, 512, st, t, 2 + pb))
                elif kind == "v":
                    P.copy(R(sv, sv.t[:, t, 0:ncol]), pst[:, 0:ncol], eng="act")
                elif kind == "cq":
                    rms_norm_tile(pst[:, 0:512], 512, qnb[:], tk[:, 0:512], junk2_[t % 2][:, 0:512])
                    pend_tr.append(lambda tk=tk, t=t, pb=pb: tok_transpose(tk, 512, cqT, t, 2 + pb))
                elif kind == "ckvkr":
                    rms_norm_tile(pst[:, 0:256], 256, kvnb[:], tk[:, 0:256], junk2_[t % 2][:, 0:256])
                    rope(pst.t[:, 256:320], pst, tk.t[:, 256:320], tk, t, 1, rt)
                    pend_tr.append(lambda tk=tk, t=t, pb=pb: tok_transpose(tk, 256, ckvT, t, 2 + pb))
                    pend_tr.append(lambda tk=tk, st=st, t=t, pb=pb: tok_transpose_view(tk, 256, 64, st, t, 4 + pb))
                elif kind == "ckcv":
                    rope(pst.t[:, 0:256], pst, tk.t[:, 0:256], tk, t, 4, rt)
                    P.copy(R(sv, sv.t[:, t, 0:256]), pst[:, 256:512], eng="act")
                    pend_tr.append(lambda tk=tk, st=st, t=t, pb=pb: tok_transpose(tk, 256, st, t, 2 + pb))
                ada_tick(1)
            flush_tr()
            if kind == "ropeT":
                flushT(st, 4, dest[0], dest[1])
            elif kind == "v":
                flushV(sv, ncol, dest)
            elif kind == "ckvkr":
                flushT(st, 1, kRC_s[l], 0, parts=64)
            elif kind == "ckcv":
                flushT(st, 2, kRC_s[l], 64)
                flushV(sv, 256, vC_s[l])

        for (wsrc, nkc, cstride, srcT, dsts) in ((wuq, 4, 192, cqT, None),
                                                   (wukv, 2, 256, ckvT, (kB_s[0][l], kB_s[1][l]))):
            for hg in range(2):
                st = stageT[nst[0] % 2]
                nst[0] += 1
                for hh in range(4):
                    h_ = hg * 4 + hh
                    for tb in range(2):
                        pst = ps[(hh * 2 + tb) % 2]
                        for kc in range(nkc):
                            P.mm(pst[:], R(wsrc, wsrc.t[:, kc, h_ * cstride:h_ * cstride + 128]),
                                 R(srcT, srcT.t[:, kc, tb * 512:(tb + 1) * 512]),
                                 start=(kc == 0), stop=(kc == nkc - 1))
                        P.copy(R(st, st.t[:, hh, tb * 512:(tb + 1) * 512]), pst[:],
                               eng="act" if tb == 0 else "dve")
                        ada_tick(1)
                if dsts is None:
                    flushT(st, 4, qTBn_d, hg * 512)
                else:
                    flushT(st, 4, dsts[hg], 0)
        st = stageT[nst[0] % 2]
        nst[0] += 1
        for t in range(NT):
            pb = t % 2
            pst = ps[pb]
            for kc in range(4):
                rhs = R(wuq, wuq.t[:, kc, :].rearrange("p (h c) -> p h c", c=192)[:, :, 128:192])
                P.mm(pst[:], R(cqT, cqT.t[:, kc, t * 128:(t + 1) * 128]), rhs, start=(kc == 0), stop=(kc == 3))
            flush_tr()
            tk = tok[t % 2]
            rope(pst.t[:, 0:512], pst, tk.t[:, 0:512], tk, t, 8, rt)
            pend_tr.append(lambda tk=tk, st=st, t=t, pb=pb: tok_transpose(tk, 512, st, t, 2 + pb))
        flush_tr()
        flushT(st, 4, qTBr_d, 0)
        for hg in range(2):
            sv = stageV[nst[1] % 2]
            nst[1] += 1
            for t in range(NT):
                pst = ps[t % 2]
                for kc in range(2):
                    rhs = R(wukv, wukv.t[:, kc, :].rearrange("p (h c) -> p h c", c=256)[:, hg * 4:(hg + 1) * 4, 128:256])
                    P.mm(pst[:], R(ckvT, ckvT.t[:, kc, t * 128:(t + 1) * 128]), rhs, start=(kc == 0), stop=(kc == 1))
                P.copy(R(sv, sv.t[:, t, :]), pst[:], eng="act" if t % 2 == 0 else "dve")
            flushV(sv, 512, vB_s[hg][l])
        P.pop()

        for pr in [(kB_s[0][l], kB_a[0][l]), (kB_s[1][l], kB_a[1][l]),
                   (kRC_s[l], kRC_a[l]), (vB_s[0][l], vB_a[0][l]), (vB_s[1][l], vB_a[1][l]),
                   (vC_s[l], vC_a[l])]:
            gather(*pr)

        if dbg == "p1" and l == 0:
            for nm, tt_ in (("qTA", qTA_d), ("qTBn", qTBn_d), ("qTBr", qTBr_d), ("qTC", qTC_d),
                            ("kA0", kA_a[0][l]), ("kA1", kA_a[1][l]), ("kB0", kB_a[0][l]), ("kB1", kB_a[1][l]),
                            ("kRC", kRC_a[l]), ("vA0", vA_a[0][l]), ("vA1", vA_a[1][l]), ("vB0", vB_a[0][l]),
                            ("vB1", vB_a[1][l]), ("vC", vC_a[l])):
                shp = list(tt_.t.shape)
                o = P.dram("dbg_" + nm, shp, BF16, kind="ExternalOutput")
                P.dma(o[:], tt_[:], q="sp")
            P.emit()
            return nc


        P.push()
        yT = P.sbuf("yT", [128, 24, TOK], BF16)
        P.push()
        KT = [P.sbuf("KT%d" % i, [128, S], BF16) for i in range(2)]
        Vt = [P.sbuf("Vt%d" % i, [128, 32, 132], BF16) for i in range(2)]
        QT = [P.sbuf("QT%d" % i, [128, TOK], BF16) for i in range(2)]
        Kr = P.sbuf("Kr", [64, S], BF16)
        QTr = [P.sbuf("QTr%d" % i, [64, TOK], BF16) for i in range(2)]
        Pt = [P.sbuf("Pt%d" % i, [128, 512], BF16) for i in range(4)]
        for v_ in Vt:
            P.memset(R(v_, v_.t[:, :, 128:129]), 1.0, eng="pool")
        lq = P.sbuf("lq", [128, 128], F32)
        lk = P.sbuf("lk", [128, 128], F32)
        lam2 = P.sbuf("lam2", [128, 2], F32)
        neglam = P.sbuf("neglam", [128, 1], F32)
        sublnS = P.sbuf("sublnS", [128, 128], F32)
        esink = P.sbuf("esink", [128, 16], F32)
        P.dma(lq[:], bc_rows(lamq_in, l * 128, 128), q="sp")
        P.dma(lk[:], bc_rows(lamk_in, l * 128, 128), q="sp")
        P.dma(sublnS[:], bc_rows(subln_in, l * 128, 128), q="sp")
        P.dma(esink[:], bc_rows(sink_in, l * 16, 16), q="sp")
        P.tt(lq[:], lq[:], lk[:], ALU.mult)
        P.reduce(lam2[:], R(lq, lq.t[:].rearrange("p (m d) -> p m d", m=2)), ALU.add)
        P.act(lam2[:], lam2[:], AF.Exp)
        P.act(esink[:], esink[:], AF.Exp)
        P.tt(neglam[:], lam2[:, 1:2], lam2[:, 0:1], ALU.subtract)
        P.ts(neglam[:], neglam[:], -lam_init, ALU.add)
        P.ts(sublnS[:], sublnS[:], 1.0 - lam_init, ALU.mult)
        fo = P.sbuf("fo", [128, 128], F32)
        fsq = P.sbuf("fsq", [128, 128], F32)
        fr = P.sbuf("fr", [128, 8], F32)
        Oc = P.sbuf("Oc", [128, 3, 396], F32)
        yblk = [P.sbuf("yblk%d" % i, [128, 512], BF16) for i in range(2)]
        nblk = [0]
        pending = []
        ada_pending = []

        def flush_pending():
            while pending:
                pending.pop(0)()

        def oslot(a):
            return ps[4 + a // 3], (a % 3) * 132

        def ocslot(a, c0, c1):
            return R(Oc, Oc.t[:, a // 3, (a % 3) * 132 + c0:(a % 3) * 132 + c1])

        def defer_transpose(yb, ncol, chunk0, nchunk_out, tok0, ntok):
            def f():
                nsub = ntok // 128
                nch = ncol // 128
                for sub in range(nsub):
                    for c in range(nch):
                        P.tr(R(ps[7], psbf(7)[:, (c * nsub + sub) * 128:(c * nsub + sub + 1) * 128]),
                             R(yb, yb.t[:, sub * ncol + c * 128:sub * ncol + (c + 1) * 128]), ident[:])
                P.copy(R(yT, yT.t[:, chunk0:chunk0 + nch, tok0:tok0 + ntok]),
                       R(ps[7], psbf(7)[:, 0:nch * ntok].rearrange("p (c n) -> p c n", n=ntok)))
            pending.append(f)

        def loads_A(h_):
            kt_, v_, q_ = KT[h_ % 2], Vt[h_ % 2], QT[h_ % 2]
            for m in range(2):
                P.dma(R(kt_, kt_.t[m * 64:(m + 1) * 64, :].rearrange("p (r n) -> p r n", r=4)),
                      R(kA_a[m][l], kA_a[m][l].t[:].rearrange("(r f) n -> f r n", r=4)[h_ * 64:(h_ + 1) * 64, :, :]),
                      q="sp")
                P.dma(R(q_, q_.t[m * 64:(m + 1) * 64, :]), qTA_d[m * 512 + h_ * 64:m * 512 + (h_ + 1) * 64, :], q="sp")
            P.dma(R(v_, v_.t[:, :, 0:128]),
                  R(vA_a[h_ // 4][l], vA_a[h_ // 4][l].t[:, (h_ % 4) * 128:(h_ % 4 + 1) * 128].rearrange(
                      "(k p) c -> p k c", p=128)), q="sp")

        def S_A(blk, kt, m):
            h_, qb = blk
            kt_, q_ = KT[h_ % 2], QT[h_ % 2]
            par = kt % 2
            P.mm(ps[par * 2 + m][:], R(kt_, kt_.t[m * 64:(m + 1) * 64, kt * 128:(kt + 1) * 128]),
                 R(q_, q_.t[m * 64:(m + 1) * 64, qb * 512:(qb + 1) * 512]))
            P.act(Pt[par * 2 + m][:], ps[par * 2 + m][:], AF.Exp, scale=0.125)

        def PV_A(blk, kt, started, m):
            h_, qb = blk
            v_ = Vt[h_ % 2]
            par = kt % 2
            for qs in range(4):
                if True:
                    bank, off = oslot(m * 4 + qs)
                    st_ = kt == 0 and bank.key not in started
                    started.add(bank.key)
                    P.mm(R(bank, bank.t[:, off:off + 129]),
                         R(Pt[par * 2 + m], Pt[par * 2 + m].t[:, qs * 128:(qs + 1) * 128]),
                         R(v_, v_.t[:, kt, 0:129]), start=st_, stop=(kt == 31))

        def fin_A(blk):
            h_, qb = blk
            for bnk in range(3):
                P.copy(Oc[:, bnk, :], R(ps[4 + bnk], ps[4 + bnk].t[:, 0:396]))
            yb = yblk[nblk[0] % 2]
            nblk[0] += 1
            for qs in range(4):
                P.op("dve", lambda h, qs=qs: h.reciprocal(fr.t[:, 0:1], ocslot(qs, 128, 129).ap), [Oc[:]], [fr[:]])
                P.op("dve", lambda h, qs=qs: h.reciprocal(fr.t[:, 1:2], ocslot(4 + qs, 128, 129).ap), [Oc[:]], [fr[:]])
                P.tt(fr[:, 2:3], fr[:, 1:2], neglam[:], ALU.mult)
                P.ts(fo[:], ocslot(qs, 0, 128), fr[:, 0:1], ALU.mult)
                P.stt(fo[:], ocslot(4 + qs, 0, 128), fr[:, 2:3], fo[:], ALU.mult, ALU.add)
                P.tt(fsq[:], fo[:], fo[:], ALU.mult)
                P.reduce(fr[:, 3:4], fsq[:], ALU.add)
                P.ts(fr[:, 3:4], fr[:, 3:4], 1.0 / 128, ALU.mult)
                rstd_from(fr[:, 4:5], fr[:, 3:4], RMS_EPS)
                P.stt(R(yb, yb.t[:, qs * 128:(qs + 1) * 128]), fo[:], fr[:, 4:5], sublnS[:], ALU.mult, ALU.mult)
            defer_transpose(yb, 128, h_, 1, qb * 512, 512)

        def run_dense(blocks_, loads, S_, PV_, fin_, nm, ticks):
            loads(blocks_[0][0])
            for m in range(nm):
                S_(blocks_[0], 0, m)
            for bi, blk in enumerate(blocks_):
                if blk[1] == 0 and blk[0] + 1 < 8:
                    loads(blk[0] + 1)
                started = set()
                for kt in range(32):
                    nxt = (blk, kt + 1) if kt + 1 < 32 else ((blocks_[bi + 1], 0) if bi + 1 < len(blocks_) else None)
                    if nxt is not None:
                        for m in range(nm):
                            S_(nxt[0], nxt[1], m)
                    for m in range(nm):
                        PV_(blk, kt, started, m)
                    if ticks:
                        ada_tick(ticks)
                    if kt == 6:
                        flush_pending()
                    if kt == 12 and ada_pending and (bi % 2 == 1):
                        ada_pending.pop(0)()
                fin_(blk)
            flush_pending()

        blocks_hq = [(h_, qb) for h_ in range(8) for qb in range(2)]
        run_dense(blocks_hq, loads_A, S_A, PV_A, fin_A, 2, 0)

        P.dma(R(Kr, Kr.t[:, :].rearrange("p (r n) -> p r n", r=4)),
              R(kRC_a[l], kRC_a[l].t[:].rearrange("(r f) n -> f r n", r=4)[0:64, :, :]), q="sp")
        sB = 192.0 ** -0.5

        def loads_B(h_):
            kt_, v_, q_, qr_ = KT[h_ % 2], Vt[h_ % 2], QT[h_ % 2], QTr[h_ % 2]
            P.dma(R(kt_, kt_.t[:, :].rearrange("p (r n) -> p r n", r=4)),
                  R(kB_a[h_ // 4][l], kB_a[h_ // 4][l].t[:].rearrange("(r f) n -> f r n", r=4)[
                      (h_ % 4) * 128:(h_ % 4 + 1) * 128, :, :]), q="sp")
            P.dma(q_[:], qTBn_d[h_ * 128:(h_ + 1) * 128, :], q="sp")
            P.dma(qr_[:], qTBr_d[h_ * 64:(h_ + 1) * 64, :], q="sp")
            P.dma(R(v_, v_.t[:, :, 0:128]),
                  R(vB_a[h_ // 4][l], vB_a[h_ // 4][l].t[:, (h_ % 4) * 128:(h_ % 4 + 1) * 128].rearrange(
                      "(k p) c -> p k c", p=128)), q="sp")

        def S_B(blk, kt, m):
            h_, qb = blk
            kt_, q_, qr_ = KT[h_ % 2], QT[h_ % 2], QTr[h_ % 2]
            par = kt % 4
            P.mm(ps[par][:], R(kt_, kt_.t[:, kt * 128:(kt + 1) * 128]),
                 R(q_, q_.t[:, qb * 512:(qb + 1) * 512]), start=True, stop=False)
            P.mm(ps[par][:], R(Kr, Kr.t[0:64, kt * 128:(kt + 1) * 128]),
                 R(qr_, qr_.t[0:64, qb * 512:(qb + 1) * 512]), start=False, stop=True)
            P.act(Pt[par][:], ps[par][:], AF.Exp, scale=sB)

        def PV_B(blk, kt, started, m):
            h_, qb = blk
            v_ = Vt[h_ % 2]
            par = kt % 4
            for qs in range(4):
                bank, off = oslot(qs)
                st_ = kt == 0 and bank.key not in started
                started.add(bank.key)
                P.mm(R(bank, bank.t[:, off:off + 129]), R(Pt[par], Pt[par].t[:, qs * 128:(qs + 1) * 128]),
                     R(v_, v_.t[:, kt, 0:129]), start=st_, stop=(kt == 31))

        def fin_B(blk):
            h_, qb = blk
            for bnk in range(2):
                P.copy(Oc[:, bnk, :], R(ps[4 + bnk], ps[4 + bnk].t[:, 0:396]))
            yb = yblk[nblk[0] % 2]
            nblk[0] += 1
            for qs in range(4):
                P.op("dve", lambda h, qs=qs: h.reciprocal(fr.t[:, 0:1], ocslot(qs, 128, 129).ap), [Oc[:]], [fr[:]])
                P.ts(R(yb, yb.t[:, qs * 128:(qs + 1) * 128]), ocslot(qs, 0, 128), fr[:, 0:1], ALU.mult)
            defer_transpose(yb, 128, 8 + h_, 1, qb * 512, 512)

        run_dense(blocks_hq, loads_B, S_B, PV_B, fin_B, 1, 1)

        Kext = P.sbuf("Kext", [64, 10 * 128], BF16)
        Vext = P.sbuf("Vext", [128, 10, 68], BF16)
        Kc = P.sbuf("Kc", [64, 6, 128], BF16)
        Vc = P.sbuf("Vc", [128, 6, 64], BF16)
        Qc = P.sbuf("Qc", [64, 4, TOK], BF16)
        hkf = P.sbuf("hkf", [128, 128], F32)
        den4 = P.sbuf("den4", [128, 8], F32)
        P.memset(R(Vext, Vext.t[:, :, 64:65]), 1.0, eng="pool")

        def S_C(g, n, kk):
            pb = (n * 3 + kk) % 4
            P.mm(ps[pb][:], R(Kext, Kext.t[0:64, (n + kk) * 128:(n + kk + 1) * 128]),
                 R(Qc, Qc.t[0:64, :, n * 128:(n + 1) * 128]))
            pt_ = Pt[pb]
            P.act(pt_[:], ps[pb][:], AF.Exp, scale=0.125)
            pv = R(pt_, pt_.t[:].rearrange("p (r q) -> p r q", r=4))
            if kk == 0:
                sc_ = validp[:, 0:1] if n == 0 else 1.0
                P.stt(pv, pv, sc_, R(maskP, maskP.t[:].unsqueeze(1).broadcast_to([128, 4, 128])),
                      ALU.mult, ALU.mult)
            elif kk == 2:
                sc_ = validn[:, 0:1] if n == NT - 1 else 1.0
                P.stt(pv, pv, sc_, R(maskN, maskN.t[:].unsqueeze(1).broadcast_to([128, 4, 128])),
                      ALU.mult, ALU.mult)

        def PV_C(g, n, kk):
            pt_ = Pt[(n * 3 + kk) % 4]
            for r in range(4):
                P.mm(R(ps[4], ps[4].t[:, r * 68:r * 68 + 65]), R(pt_, pt_.t[:, r * 128:(r + 1) * 128]),
                     R(Vext, Vext.t[:, n + kk, 0:65]), start=(kk == 0 and r == 0), stop=(kk == 2))

        for g in range(4):
            kview = kRC_a[l].t[:].rearrange("(r f) n -> f r n", r=4)[64 + g * 64:64 + (g + 1) * 64, :, :]
            P.dma(Kc[:, 0:3, :], R(kRC_a[l], kview[:, 0:3, 896:1024]), q="sp")
            P.dma(Kc[:, 3:6, :], R(kRC_a[l], kview[:, 1:4, 0:128]), q="sp")
            vview = vC_a[l].t[:, g * 64:(g + 1) * 64].rearrange("(r t) c -> t r c", r=4)
            P.dma(Vc[:, 0:3, :], R(vC_a[l], vview[896:1024, 0:3, :]), q="sp")
            P.dma(Vc[:, 3:6, :], R(vC_a[l], vview[0:128, 1:4, :]), q="sp")
            P.dma(R(Kext, Kext.t[:, 128:1152]), kRC_s[l][64 + g * 64:64 + (g + 1) * 64, :], q="sp")
            P.dma(R(Vext, Vext.t[:, 1:9, 0:64]),
                  R(vC_s[l], vC_s[l].t[:, g * 64:(g + 1) * 64].rearrange("(t p) c -> p t c", p=128)), q="sp")
            P.dma(Qc[:], R(qTC_d, qTC_d.t[g * 256:(g + 1) * 256, :].rearrange("(r d) n -> d r n", d=64)), q="sp")
            for side, c0, s0 in ((0, 0, 0), (9, 3, 4)):
                P.ts(R(hkf, hkf.t[0:64, :]), R(Kc, Kc.t[:, c0, :]), selb[0:64, s0:s0 + 1], ALU.mult)
                P.stt(R(hkf, hkf.t[0:64, :]), R(Kc, Kc.t[:, c0 + 1, :]), selb[0:64, s0 + 1:s0 + 2],
                      R(hkf, hkf.t[0:64, :]), ALU.mult, ALU.add)
                P.stt(R(Kext, Kext.t[:, side * 128:(side + 1) * 128]), R(Kc, Kc.t[:, c0 + 2, :]),
                      selb[0:64, s0 + 2:s0 + 3], R(hkf, hkf.t[0:64, :]), ALU.mult, ALU.add)
                P.ts(R(hkf, hkf.t[:, 0:64]), R(Vc, Vc.t[:, c0, :]), selb[:, s0:s0 + 1], ALU.mult)
                P.stt(R(hkf, hkf.t[:, 0:64]), R(Vc, Vc.t[:, c0 + 1, :]), selb[:, s0 + 1:s0 + 2],
                      R(hkf, hkf.t[:, 0:64]), ALU.mult, ALU.add)
                P.stt(R(Vext, Vext.t[:, side, 0:64]), R(Vc, Vc.t[:, c0 + 2, :]), selb[:, s0 + 2:s0 + 3],
                      R(hkf, hkf.t[:, 0:64]), ALU.mult, ALU.add)
            steps = [(n, kk) for n in range(NT) for kk in range(3)]
            S_C(g, 0, 0)
            for si, (n, kk) in enumerate(steps):
                if si + 1 < len(steps):
                    S_C(g, *steps[si + 1])
                PV_C(g, n, kk)
                if kk == 0:
                    flush_pending()
                if kk == 2:
                    P.copy(R(Oc, Oc.t[:, 0, 0:272]), R(ps[4], ps[4].t[:, 0:272]))
                    ov = Oc.t[:, 0, 0:272].rearrange("p (r c) -> p r c", c=68)
                    P.tt(den4[:, 0:4], R(Oc, ov[:, :, 64]), esink[:, g * 4:(g + 1) * 4], ALU.add)
                    P.op("dve", lambda h: h.reciprocal(den4.t[:, 4:8], den4.t[:, 0:4]), [den4[:]], [den4[:]])
                    yb = yblk[nblk[0] % 2]
                    nblk[0] += 1
                    P.tt(R(yb, yb.t[:, 0:256].rearrange("p (r c) -> p r c", c=64)), R(Oc, ov[:, :, 0:64]),
                         R(den4, den4.t[:, 4:8].unsqueeze(2).broadcast_to([128, 4, 64])), ALU.mult)
                    defer_transpose(yb, 256, 16 + g * 2, 2, n * 128, 128)
            flush_pending()
        while ada_pending:
            ada_pending.pop(0)()
        ada_flush()
        P.pop()
        ymT = P.sbuf("ymT", [128, KC, TOK], BF16)

        if dbg in ("p3", "p4", "l1") and l == dbg_layer:
            o = P.dram("dbg_yT", [24 * 128, TOK], BF16, kind="ExternalOutput")
            P.dma(R(o, o.t[:].rearrange("(c p) n -> p c n", p=128)), yT[:], q="sp")
            if dbg == "p3":
                om = P.dram("dbg_mod", [DEPTH, 1, 6 * D], F32, kind="ExternalOutput")
                P.push()
                t_ = P.sbuf("dbg_t", [1, 6 * D], F32)
                for l_ in range(nlayers):
                    P.dma(t_[:], mod_d[l_, :, :], q="sp")
                    P.dma(om[l_, :, :], t_[:], q="sp")
                P.pop()
                P.emit()
                return nc

        P.push()
        wg = [P.sbuf("wg%d" % i, [128, KC, 512], BF16) for i in range(2)]
        wbr = [P.sbuf("wbr%d" % i, [128, 8, 512], BF16) for i in range(2)]
        ycb = P.sbuf("ycb", [128, NT, 512], F32)
        sig = [P.sbuf("sig%d" % i, [128, 512], BF16) for i in range(2)]
        gtmp = [P.sbuf("gtmp%d" % i, [128, 512], F32) for i in range(2)]
        ybf = [P.sbuf("ybf%d" % i, [128, 512], BF16) for i in range(2)]
        cnt = 0
        pend_tail = []

        def merge_tail(t, cb):
            P.copy(ybf[t % 2][:], ycb[:, t, :], eng="act")
            bank = 4 + t % 2
            for c in range(4):
                P.tr(R(ps[bank], psbf(bank)[:, c * 128:(c + 1) * 128]), R(ybf[t % 2], ybf[t % 2].t[:, c * 128:(c + 1) * 128]),
                     ident[:])
            P.copy(R(ymT, ymT.t[:, cb * 4:(cb + 1) * 4, t * 128:(t + 1) * 128]),
                   R(ps[bank], psbf(bank)[:, 0:512].rearrange("p (c n) -> p c n", n=128)),
                   eng="act" if t % 2 == 0 else "dve")

        for cb in range(4):
            for br, wsrc in enumerate((wa_in, wb_in, wc_in)):
                g_, w_ = wg[cnt % 2], wbr[cnt % 2]
                cnt += 1
                gc0 = O_G + br * D + cb * 512
                P.dma(g_[:], R(w_in, w_in.t[l, :, gc0:gc0 + 512].rearrange("(c p) n -> p c n", p=128)), q="pool")
                P.dma(w_[:], R(wsrc, wsrc.t[l, :, cb * 512:(cb + 1) * 512].rearrange("(c p) n -> p c n", p=128)),
                      q="pool")
                for t in range(NT):
                    if br == 0 and pend_tail:
                        pend_tail.pop(0)()
                    pg, pp = ps[(t % 2) * 2], ps[(t % 2) * 2 + 1]
                    for kc in range(KC):
                        P.mm(pg[:], hT[:, kc, t * 128:(t + 1) * 128], g_[:, kc, :], start=(kc == 0), stop=(kc == KC - 1))
                    for kc in range(8):
                        P.mm(pp[:], R(yT, yT.t[:, br * 8 + kc, t * 128:(t + 1) * 128]), w_[:, kc, :],
                             start=(kc == 0), stop=(kc == 7))
                    P.act(sig[t % 2][:], pg[:], AF.Sigmoid)
                    if br == 0:
                        P.tt(ycb[:, t, :], pp[:], sig[t % 2][:], ALU.mult)
                    else:
                        P.tt(gtmp[t % 2][:], pp[:], sig[t % 2][:], ALU.mult)
                        P.tt(ycb[:, t, :], ycb[:, t, :], gtmp[t % 2][:], ALU.add)
            for t in range(NT):
                pend_tail.append(lambda t=t, cb=cb: merge_tail(t, cb))
        while pend_tail:
            pend_tail.pop(0)()
        P.pop()

        P.push()
        wo = [P.sbuf("wo%d" % i, [128, KC, 512], BF16) for i in range(2)]
        g1b = P.sbuf("g1b", [128, D], F32)
        xq = [P.sbuf("xq%d" % i, [128, 512], F32) for i in range(2)]
        zq = [P.sbuf("zq%d" % i, [128, 512], F32) for i in range(2)]
        P.dma(g1b[:], bc_rows(mod_d, l * 6 * D + 2 * D, D), q="sp")
        for ob in range(4):
            w_ = wo[ob % 2]
            P.dma(w_[:], R(wo_in, wo_in.t[l, :, ob * 512:(ob + 1) * 512].rearrange("(c p) n -> p c n", p=128)), q="pool")
            for t in range(NT):
                pst = ps[t % 2]
                for kc in range(KC):
                    P.mm(pst[:], R(ymT, ymT.t[:, kc, t * 128:(t + 1) * 128]), w_[:, kc, :],
                         start=(kc == 0), stop=(kc == KC - 1))
                P.dma(xq[t % 2][:], x_src[t * 128:(t + 1) * 128, ob * 512:(ob + 1) * 512], q="sp")
                P.tt(zq[t % 2][:], pst[:], g1b[:, ob * 512:(ob + 1) * 512], ALU.mult)
                P.stt(zq[t % 2][:], xq[t % 2][:], ALPHA, zq[t % 2][:], ALU.mult, ALU.add)
                P.dma(x1p_d[t * 128:(t + 1) * 128, ob * 512:(ob + 1) * 512], zq[t % 2][:], q="sp")
        P.pop()
        P.pop()

        P.push()
        xt2 = [P.sbuf("xt2_%d" % i, [128, D], F32) for i in range(2)]
        x1t = [P.sbuf("x1t%d" % i, [128, D], F32) for i in range(2)]
        lng = P.sbuf("lng", [128, D], F32)
        lnb = P.sbuf("lnb", [128, D], F32)
        modA2 = P.sbuf("modA2", [128, D], F32)
        modB2 = P.sbuf("modB2", [128, D], F32)
        junk = [P.sbuf("junk_c%d" % i, [128, D], BF16) for i in range(2)]
        xn = [P.sbuf("xn_c%d" % i, [128, D], F32) for i in range(2)]
        h2f_ = [P.sbuf("h2f%d" % i, [128, D], F32) for i in range(2)]
        hb2 = [P.sbuf("hb2_%d" % i, [128, D], BF16) for i in range(2)]
        h2Tf = P.sbuf("h2Tf", [128, KC, 128], F32)
        rwf = P.sbuf("rwf", [128, KC, NEXP], F32)
        rbb = P.sbuf("rbb", [128, NEXP], F32)
        rsc = P.sbuf("rsc", [128, NT, NEXP], F32)
        rbi = P.sbuf("rbi", [128, NT, NEXP], F32)
        req = P.sbuf("req", [128, NT, NEXP], F32)
        rg2 = P.sbuf("rg2", [128, NT, NEXP], F32)
        rm1 = P.sbuf("rm1", [128, NT * 4], F32)
        rm2 = P.sbuf("rm2", [128, NT * 4], F32)
        rgs = P.sbuf("rgs", [128, NT * 4], F32)
        rgm = P.sbuf("rgm", [128, NT], F32)
        P.dma(lng[:], bc_rows(lnmg_in, l * D, D), q="sp")
        P.dma(lnb[:], bc_rows(lnmb_in, l * D, D), q="sp")
        P.dma(modB2[:], bc_rows(mod_d, l * 6 * D + 3 * D, D), q="sp")
        P.dma(modA2[:], bc_rows(mod_d, l * 6 * D + 4 * D, D), q="sp")
        P.ts(modA2[:], modA2[:], 1.0, ALU.add)
        P.dma(rwf[:], R(rw_in, rw_in.t[:].rearrange("(c p) e -> p c e", p=128)), q="sp")
        P.dma(rbb[:], bc_rows(rb_in, 0, NEXP), q="sp")
        for t in range(NT):
            P.dma(xt2[t % 2][:], x1p_d[t * 128:(t + 1) * 128, :], q="sp")
            layer_norm(xt2[t % 2], x1t[t % 2][:], lng, lnb, junk, xn)
            P.dma(xs_d[t * 128:(t + 1) * 128, :], x1t[t % 2][:], q="sp")
            h2f = h2f_[t % 2]
            layer_norm(x1t[t % 2], h2f[:], modA2, modB2, junk, xn)
            P.copy(hb2[t % 2][:], h2f[:], eng="act")
            transpose_to_hT(hb2[t % 2], t, t)
            for c4 in range(4):
                bank = 4 + c4
                for c in range(4):
                    kc = c4 * 4 + c
                    P.tr(R(ps[bank], ps[bank].t[:, c * 128:(c + 1) * 128]), h2f[:, kc * 128:(kc + 1) * 128], identf[:])
                P.copy(R(h2Tf, h2Tf.t[:, c4 * 4:(c4 + 1) * 4, :]),
                       R(ps[bank], ps[bank].t[:].rearrange("p (c n) -> p c n", n=128)),
                       eng="act")
            for kc in range(KC):
                P.mm(R(ps[1], ps[1].t[:, 0:NEXP]), h2Tf[:, kc, :], rwf[:, kc, :], start=(kc == 0), stop=(kc == KC - 1))
            P.act(R(rsc, rsc.t[:, t, :]), R(ps[1], ps[1].t[:, 0:NEXP]), AF.Sigmoid)
        NG = NT * 4
        g3v = lambda tl: R(tl, tl.t[:].rearrange("p t (g e) -> p (t g) e", e=4))
        bcg = lambda tl: R(tl, tl.t[:].unsqueeze(2).broadcast_to([128, NG, 4]))
        P.tt(rbi[:], rsc[:], R(rbb, rbb.t[:].unsqueeze(1).broadcast_to([128, NT, NEXP])), ALU.add)
        P.reduce(rm1[:], g3v(rbi), ALU.max)
        P.tt(g3v(req), g3v(rbi), bcg(rm1), ALU.is_equal)
        P.stt(g3v(rg2), g3v(req), -1e30, g3v(rbi), ALU.mult, ALU.add)
        P.reduce(rm2[:], g3v(rg2), ALU.max)
        P.tt(rgs[:], rm1[:], rm2[:], ALU.add)
        P.reduce(rgm[:], R(rgs, rgs.t[:].rearrange("p (t g) -> p t g", g=4)), ALU.max)
        P.tt(R(rgs, rgs.t[:].rearrange("p (t g) -> p t g", g=4)), R(rgs, rgs.t[:].rearrange("p (t g) -> p t g", g=4)),
             R(rgm, rgm.t[:].unsqueeze(2).broadcast_to([128, NT, 4])), ALU.is_equal)
        P.tt(g3v(req), g3v(rbi), bcg(rm2), ALU.is_ge)
        P.tt(g3v(req), g3v(req), bcg(rgs), ALU.mult)
        P.tt(rg2[:], rsc[:], req[:], ALU.mult)
        P.reduce(rgm[:], rg2[:], ALU.add)
        P.op("dve", lambda h: h.reciprocal(rgm.t[:], rgm.t[:]), [rgm[:]], [rgm[:]])
        P.tt(gates[:], rg2[:], R(rgm, rgm.t[:].unsqueeze(2).broadcast_to([128, NT, NEXP])), ALU.mult)
        P.pop()

        if dbg == "p4" and l == dbg_layer:
            o = P.dram("dbg_x1", [TOK, D], F32, kind="ExternalOutput")
            P.push()
            for t in range(NT):
                tt_ = P.sbuf("dbgx%d" % t, [128, D], F32)
                P.dma(tt_[:], xs_d[t * 128:(t + 1) * 128, :], q="sp")
                P.dma(o[t * 128:(t + 1) * 128, :], tt_[:], q="sp")
            o2 = P.dram("dbg_h2T", [D, TOK], BF16, kind="ExternalOutput")
            P.dma(R(o2, o2.t[:].rearrange("(c p) n -> p c n", p=128)), hT[:], q="sp")
            om = P.dram("dbg_mod", [DEPTH, 1, 6 * D], F32, kind="ExternalOutput")
            t_ = P.sbuf("dbg_tm", [1, 6 * D], F32)
            for l_ in range(nlayers):
                P.dma(t_[:], mod_d[l_, :, :], q="sp")
                P.dma(om[l_, :, :], t_[:], q="sp")
            o3 = P.dram("dbg_gates", [128, NT * NEXP], F32, kind="ExternalOutput")
            P.dma(o3[:], R(gates, gates.t[:].rearrange("p t e -> p (t e)")), q="sp")
            P.emit()
            return nc

        P.push()
        y_acc = P.sbuf("y_acc", [128, NT, D], F32)
        P.push()
        w13 = [P.sbuf("w13_%d" % i, [128, 2, KC, 256], BF16) for i in range(2)]
        hid = [P.sbuf("hid%d" % i, [128, 4, TOK], BF16) for i in range(2)]
        w2e = P.sbuf("w2e", [128, 4, D], BF16)
        sA = [P.sbuf("sA%d" % i, [128, 512], BF16) for i in range(2)]
        nh = 0
        for e in range(NEXP):
            hd = hid[e % 2]
            for fh in range(2):
                wb_ = w13[nh % 2]
                nh += 1
                for wi, wsrc in enumerate((w1_in, w3_in)):
                    P.dma(R(wb_, wb_.t[:, wi, :, :]),
                          R(wsrc, wsrc.t[l, e, :, fh * 256:(fh + 1) * 256].rearrange("(c p) f -> p c f", p=128)), q="pool")
                for tb in range(2):
                    for fc in range(2):
                        i_ = (tb * 2 + fc) % 2
                        pa, pb_ = ps[i_ * 2], ps[i_ * 2 + 1]
                        for kc in range(KC):
                            P.mm(pa[:], R(wb_, wb_.t[:, 0, kc, fc * 128:(fc + 1) * 128]),
                                 hT[:, kc, tb * 512:(tb + 1) * 512], start=(kc == 0), stop=(kc == KC - 1))
                        for kc in range(KC):
                            P.mm(pb_[:], R(wb_, wb_.t[:, 1, kc, fc * 128:(fc + 1) * 128]),
                                 hT[:, kc, tb * 512:(tb + 1) * 512], start=(kc == 0), stop=(kc == KC - 1))
                        P.act(sA[i_][:], pa[:], AF.Silu)
                        P.tt(R(hd, hd.t[:, fh * 2 + fc, tb * 512:(tb + 1) * 512]), pb_[:], sA[i_][:], ALU.mult)
            P.dma(w2e[:], R(w2_in, w2_in.t[l, e].rearrange("(c p) n -> p c n", p=128)), q="pool")
            for t in range(NT):
                for cb in range(4):
                    po = ps[4 + (t * 4 + cb) % 4]
                    for fc in range(4):
                        P.mm(po[:], R(hd, hd.t[:, fc, t * 128:(t + 1) * 128]), w2e[:, fc, cb * 512:(cb + 1) * 512],
                             start=(fc == 0), stop=(fc == 3))
                    ya_ = y_acc[:, t, cb * 512:(cb + 1) * 512]
                    if e == 0:
                        P.ts(ya_, po[:], gates[:, t, e:e + 1], ALU.mult)
                    else:
                        P.stt(ya_, po[:], gates[:, t, e:e + 1], ya_, ALU.mult, ALU.add)
        P.pop()
        g2b = P.sbuf("g2b", [128, D], F32)
        lng2 = P.sbuf("lng2", [128, D], F32)
        lnb2 = P.sbuf("lnb2", [128, D], F32)
        junk = [P.sbuf("junk_e%d" % i, [128, D], BF16) for i in range(2)]
        xn = [P.sbuf("xn_e%d" % i, [128, D], F32) for i in range(2)]
        x1l = [P.sbuf("x1l%d" % i, [128, D], F32) for i in range(2)]
        x2t = [P.sbuf("x2t%d" % i, [128, D], F32) for i in range(2)]
        P.dma(g2b[:], bc_rows(mod_d, l * 6 * D + 5 * D, D), q="sp")
        P.dma(lng2[:], bc_rows(lnfg_in, l * D, D), q="sp")
        P.dma(lnb2[:], bc_rows(lnfb_in, l * D, D), q="sp")
        for t in range(NT):
            P.dma(x1l[t % 2][:], xs_d[t * 128:(t + 1) * 128, :], q="sp")
            P.tt(y_acc[:, t, :], y_acc[:, t, :], g2b[:], ALU.mult)
            P.stt(x1l[t % 2][:], x1l[t % 2][:], ALPHA, y_acc[:, t, :], ALU.mult, ALU.add)
            layer_norm(x1l[t % 2], x2t[t % 2][:], lng2, lnb2, junk, xn)
            dst = out_d if l == nlayers - 1 else xs_d
            P.dma(dst[t * 128:(t + 1) * 128, :], x2t[t % 2][:], q="sp")
        P.pop()

    P.emit()
    return nc


_NC_CACHE = {}

_VEC3 = ("diff_lambda_q", "diff_lambda_k", "diff_subln", "mla_q_norm", "mla_kv_norm", "swa_sink",
         "b_ada", "ln_mix_g", "ln_mix_b", "ln_ffn_g", "ln_ffn_b")


def make_in_maps(inputs, nlayers=DEPTH):
    f = lambda k: np.ascontiguousarray(np.asarray(inputs[k]))
    shared = {}
    for k in inputs:
        if k in ("x", "c", "positions"):
            continue
        a = f(k)
        if k in _VEC3:
            a = a.reshape(DEPTH, 1, -1)
        elif k == "router_bias":
            a = a.reshape(1, NEXP)
        if a.ndim >= 3 and a.shape[0] == DEPTH and nlayers < DEPTH:
            a = np.ascontiguousarray(a[:nlayers])
        shared[k] = a
    x = f("x")
    c = f("c")
    pos = f("positions").astype(np.int32)
    maps = []
    for core in range(8):
        b, j = core // 4, core % 4
        sel = np.zeros((1, 8), np.float32)
        if j >= 1:
            sel[0, j - 1] = 1.0
        if j <= 2:
            sel[0, 4 + j] = 1.0
        m = dict(shared)
        m["x"] = np.ascontiguousarray(x[b, j * TOK:(j + 1) * TOK, :])
        m["c"] = np.ascontiguousarray(c[b:b + 1, :])
        m["positions"] = np.ascontiguousarray(pos[b, j * TOK:(j + 1) * TOK].reshape(TOK, 1))
        m["sel"] = sel
        maps.append(m)
    return maps


def kernel(**inputs):
    if "nc" not in _NC_CACHE:
        _NC_CACHE["nc"] = build()
    nc = _NC_CACHE["nc"]
    maps = make_in_maps(inputs)
    res = run_bass_kernel_spmd(nc, maps, core_ids=list(range(8)))
    out = np.empty((NB, S, D), np.float32)
    for core in range(8):
        b, j = core // 4, core % 4
        out[b, j * TOK:(j + 1) * TOK, :] = res.results[core]["out"]
    return out
```

```python
import math
import numpy as np
import concourse.bass as bass
import concourse.mybir as mybir
from concourse.bass_utils import run_bass_kernel_spmd

F32 = mybir.dt.float32
BF16 = mybir.dt.bfloat16
I32 = mybir.dt.int32
AF = mybir.ActivationFunctionType
ALU = mybir.AluOpType
AX = mybir.AxisListType

ENGS = ("pe", "act", "dve", "pool", "sp")
SB_BASE = 16512
SB_TOP = 229344


class Ref:
    __slots__ = ("ap", "keys")

    def __init__(self, ap, keys):
        self.ap = ap
        self.keys = tuple(keys)


class T:
    def __init__(self, t, key):
        self.t = t
        self.key = key

    def __getitem__(self, idx):
        return Ref(self.t[idx], (self.key,))

    def ref(self, ap):
        return Ref(ap, (self.key,))


class Op:
    __slots__ = ("eng", "fn", "deps", "flag", "kind", "ev", "idx")


class Prog:
    def __init__(self, nc, dma_k=None, same_engine_sync=True):
        self.nc = nc
        self.ops = {e: [] for e in ENGS}
        self.wstate = {}
        self.rstate = {}
        self.dma_k = dma_k or {"sp": 8, "pool": 8, "act": 4}
        self.ndma = {e: 0 for e in ENGS}
        self.ncc = 0
        self.same_engine_sync = same_engine_sync
        self.excl = set()
        self.sb_ptr = SB_BASE
        self.sb_stack = []
        self.sb_ranges = {}
        self.overlaps = {}
        self.sb_peak = SB_BASE
        self.nalloc = 0

    def push(self):
        self.sb_stack.append(self.sb_ptr)

    def pop(self):
        self.sb_ptr = self.sb_stack.pop()

    def sbuf(self, name, shape, dtype):
        esz = {F32: 4, BF16: 2, I32: 4}[dtype]
        n = 1
        for d_ in shape[1:]:
            n *= d_
        size = (n * esz + 31) // 32 * 32
        off = self.sb_ptr
        self.sb_ptr += size
        assert self.sb_ptr <= SB_TOP, "SBUF overflow at %s: %d" % (name, self.sb_ptr)
        self.sb_peak = max(self.sb_peak, self.sb_ptr)
        self.nalloc += 1
        key = "%s#%d" % (name, self.nalloc)
        t = self.nc.alloc_sbuf_tensor_at(key, list(shape), dtype, offset=off)
        ov = [key]
        for k2, (o2, e2) in self.sb_ranges.items():
            if o2 < off + size and off < e2:
                ov.append(k2)
                self.overlaps[k2].append(key)
        self.sb_ranges[key] = (off, off + size)
        self.overlaps[key] = ov
        return T(t, key)

    def psum(self, name, shape, dtype=F32):
        self.excl.add(name)
        return T(self.nc.alloc_psum_tensor(name, list(shape), dtype), name)

    def dram(self, name, shape, dtype, kind="Internal", **kw):
        return T(self.nc.dram_tensor(name, list(shape), dtype, kind=kind, **kw), name)

    def op(self, eng, fn, reads=(), writes=(), kind="c"):
        o = Op()
        o.eng = eng
        o.fn = fn
        o.kind = kind
        o.flag = False
        o.idx = len(self.ops[eng])
        deps = []
        rk = [k for r in reads for k in r.keys]
        wk = [k for w in writes for k in w.keys]
        for k0 in rk:
            for k in self.overlaps.get(k0, (k0,)):
                ev = self.wstate.get(k)
                if ev is not None:
                    deps.append(ev)
                if k in self.excl:
                    for ev in self.rstate.get(k, {}).values():
                        if not (ev[0] == "c" and ev[1] == eng):
                            deps.append(ev)
        for k0 in wk:
            for k in self.overlaps.get(k0, (k0,)):
                ev = self.wstate.get(k)
                if ev is not None:
                    deps.append(ev)
                for ev in self.rstate.get(k, {}).values():
                    deps.append(ev)
        if kind == "d":
            q = eng
            i = self.ndma[q]
            self.ndma[q] += 1
            K = self.dma_k[q]
            o.ev = ("d", q, i % K, 16 * (i // K + 1))
            if i >= K:
                deps.append(("d", q, i % K, 16 * (i // K)))
        elif kind == "cc":
            o.ev = ("cc", self.ncc)
            self.ncc += 1
        else:
            o.ev = ("c", eng, o.idx)
        fd = []
        for ev in deps:
            if ev[0] == "c":
                if ev[1] == eng:
                    if eng == "pe" or eng == "sp" or not self.same_engine_sync:
                        continue
                self.ops[ev[1]][ev[2]].flag = True
            fd.append(ev)
        o.deps = fd
        self.ops[eng].append(o)
        for k in wk:
            self.wstate[k] = o.ev
            self.rstate[k] = {}
        for k in rk:
            d = self.rstate.setdefault(k, {})
            if o.ev[0] == "c":
                d[eng] = o.ev
            elif o.ev[0] == "d":
                d[o.ev[:3]] = o.ev
            else:
                d[o.ev] = o.ev
        return o

    def emit(self, final_wait_eng="sp"):
        nc = self.nc
        sems = {e: nc.alloc_semaphore("s_" + e) for e in ENGS if e != "sp"}
        dsems = {
            q: [nc.alloc_semaphore("d_%s%d" % (q, j)) for j in range(self.dma_k[q])]
            for q in self.dma_k
        }
        ccsems = [nc.alloc_semaphore("cc%d" % i) for i in range(self.ncc)]
        semval = {}
        for e in ENGS:
            c = 0
            for o in self.ops[e]:
                if o.flag and o.kind == "c":
                    c += 1
                    semval[(e, o.idx)] = c
        final = []
        for q in self.dma_k:
            n = self.ndma[q]
            K = self.dma_k[q]
            for j in range(K):
                cnt = (n - j + K - 1) // K if n > j else 0
                if cnt > 0:
                    final.append((q, j, 16 * cnt))

        def run(e, h):
            seen = {}
            for o in self.ops[e]:
                for ev in o.deps:
                    if ev[0] == "c":
                        key = ("c", ev[1])
                        val = semval[(ev[1], ev[2])]
                        s = sems[ev[1]]
                    elif ev[0] == "d":
                        key = ("d", ev[1], ev[2])
                        val = ev[3]
                        s = dsems[ev[1]][ev[2]]
                    else:
                        key = ev
                        val = 1
                        s = ccsems[ev[1]]
                    if seen.get(key, 0) >= val:
                        continue
                    seen[key] = val
                    h.wait_ge(s, val)
                ins = o.fn(h)
                if o.kind == "d":
                    ins.then_inc(dsems[o.ev[1]][o.ev[2]], 16)
                elif o.kind == "cc":
                    ins.then_inc(ccsems[o.ev[1]], 1)
                elif o.flag:
                    ins.then_inc(sems[e], 1)
            if e == final_wait_eng:
                for q, j, v in final:
                    if seen.get(("d", q, j), 0) < v:
                        h.wait_ge(dsems[q][j], v)

        with nc.Block() as block:

            @block.tensor
            def _(h):
                run("pe", h)

            @block.scalar
            def _(h):
                run("act", h)

            @block.vector
            def _(h):
                run("dve", h)

            @block.gpsimd
            def _(h):
                run("pool", h)

            @block.sync
            def _(h):
                run("sp", h)

    def dma(self, out, in_, q="sp", **kw):
        return self.op(q, lambda h: h.dma_start(out.ap, in_.ap, **kw), [in_], [out], kind="d")

    def mm(self, out, lhsT, rhs, start=True, stop=True, **kw):
        return self.op(
            "pe",
            lambda h: h.matmul(out.ap, lhsT.ap, rhs.ap, start=start, stop=stop, **kw),
            [lhsT, rhs],
            [out],
        )

    def tr(self, out, in_, ident):
        return self.op("pe", lambda h: h.transpose(out.ap, in_.ap, ident.ap), [in_, ident], [out])

    def act(self, out, in_, func, bias=None, scale=None, accum_out=None):
        kw = {}
        rd = [in_]
        wr = [out]
        if bias is not None:
            if isinstance(bias, Ref):
                kw["bias"] = bias.ap
                rd.append(bias)
            else:
                kw["bias"] = bias
        if scale is not None:
            if isinstance(scale, Ref):
                kw["scale"] = scale.ap
                rd.append(scale)
            else:
                kw["scale"] = scale
        if accum_out is not None:
            kw["accum_out"] = accum_out.ap
            wr.append(accum_out)
        return self.op("act", lambda h: h.activation(out.ap, in_.ap, func, **kw), rd, wr)

    def copy(self, out, in_, eng="dve"):
        if eng == "act":
            return self.op(eng, lambda h: h.copy(out.ap, in_.ap), [in_], [out])
        return self.op(eng, lambda h: h.tensor_copy(out.ap, in_.ap), [in_], [out])

    def tt(self, out, in0, in1, op, eng="dve"):
        return self.op(eng, lambda h: h.tensor_tensor(out.ap, in0.ap, in1.ap, op), [in0, in1], [out])

    def ts(self, out, in0, s1, op0, s2=None, op1=None, accum_out=None, eng="dve"):
        rd = [in0]
        wr = [out]
        a1 = s1
        a2 = s2
        if isinstance(s1, Ref):
            rd.append(s1)
            a1 = s1.ap
        if isinstance(s2, Ref):
            rd.append(s2)
            a2 = s2.ap
        kw = {}
        if op1 is not None:
            kw["op1"] = op1
        if accum_out is not None:
            kw["accum_out"] = accum_out.ap
            wr.append(accum_out)
        return self.op(eng, lambda h: h.tensor_scalar(out.ap, in0.ap, a1, a2, op0, **kw), rd, wr)

    def stt(self, out, in0, scalar, in1, op0, op1, eng="dve"):
        rd = [in0, in1]
        a = scalar
        if isinstance(scalar, Ref):
            rd.append(scalar)
            a = scalar.ap
        return self.op(
            eng, lambda h: h.scalar_tensor_tensor(out.ap, in0.ap, a, in1.ap, op0, op1), rd, [out]
        )

    def memset(self, out, val, eng="dve"):
        return self.op(eng, lambda h: h.memset(out.ap, val), [], [out])

    def reduce(self, out, in_, op, axis=AX.X, eng="dve"):
        return self.op(eng, lambda h: h.tensor_reduce(out.ap, in_.ap, axis, op), [in_], [out])


D = 2048
S = 4096
NB = 2
DEPTH = 2
TOK = 1024
NT = TOK // 128
KC = D // 128
IN_W = 11584
O_AQ, O_AK, O_AV, O_CQ, O_CKV, O_KR, O_SQ, O_SK, O_SV, O_G = (
    0, 1024, 2048, 3072, 3584, 3840, 3904, 4928, 5184, 5440)
KR_A, KR_BN, KR_BR, KR_C, FK = 0, 1024, 2048, 2112, 2368
VC_A, VC_B, VC_C, FV = 0, 1024, 2048, 2304
ALPHA = (2 * DEPTH) ** 0.25
LN_EPS = 1e-5
RMS_EPS = 1e-6
NEXP = 16
EH = 512
TWO_PI = 2.0 * math.pi


def build(dbg=None, nlayers=DEPTH, moe=True, dbg_layer=0):
    nc = bass.Bass("TRN2", target_bir_lowering=False)
    P = Prog(nc)
    LW = nlayers
    din = lambda n, s, dt=F32: P.dram(n, [LW] + list(s[1:]) if (len(s) >= 3 and s[0] == DEPTH) else s, dt,
                                      kind="ExternalInput")
    x_in = din("x", [TOK, D])
    c_in = din("c", [1, D])
    pos_in = din("positions", [TOK, 1], I32)
    sel_in = din("sel", [1, 8])
    w_in = din("w_in", [DEPTH, D, IN_W])
    lamq_in = din("diff_lambda_q", [DEPTH, 1, 128])
    lamk_in = din("diff_lambda_k", [DEPTH, 1, 128])
    subln_in = din("diff_subln", [DEPTH, 1, 128])
    qn_in = din("mla_q_norm", [DEPTH, 1, 512])
    kvn_in = din("mla_kv_norm", [DEPTH, 1, 256])
    wuq_in = din("mla_w_uq", [DEPTH, 512, 1536])
    wukv_in = din("mla_w_ukv", [DEPTH, 256, 2048])
    sink_in = din("swa_sink", [DEPTH, 1, 16])
    wa_in = din("w_branch_a", [DEPTH, 1024, D])
    wb_in = din("w_branch_b", [DEPTH, 1024, D])
    wc_in = din("w_branch_c", [DEPTH, 1024, D])
    wo_in = din("w_out", [DEPTH, D, D])
    wada_in = din("w_ada", [DEPTH, D, 6 * D])
    bada_in = din("b_ada", [DEPTH, 1, 6 * D])
    lnmg_in = din("ln_mix_g", [DEPTH, 1, D])
    lnmb_in = din("ln_mix_b", [DEPTH, 1, D])
    lnfg_in = din("ln_ffn_g", [DEPTH, 1, D])
    lnfb_in = din("ln_ffn_b", [DEPTH, 1, D])
    rw_in = din("router_w", [D, NEXP])
    rb_in = din("router_bias", [1, NEXP])
    if moe:
        w1_in = din("expert_w1", [DEPTH, NEXP, D, EH])
        w3_in = din("expert_w3", [DEPTH, NEXP, D, EH])
        w2_in = din("expert_w2", [DEPTH, NEXP, EH, D])
    out_d = P.dram("out", [TOK, D], F32, kind="ExternalOutput")

    mod_d = P.dram("mod_d", [DEPTH, 1, 6 * D], F32)
    xs_d = P.dram("xs_d", [TOK, D], F32)
    x1p_d = P.dram("x1p_d", [TOK, D], F32)
    qTA_d = P.dram("qTA_d", [1024, TOK], BF16)
    qTBn_d = P.dram("qTBn_d", [1024, TOK], BF16)
    qTBr_d = P.dram("qTBr_d", [512, TOK], BF16)
    qTC_d = P.dram("qTC_d", [1024, TOK], BF16)
    def xch(name, shape):
        src = [P.dram("%s_s%d" % (name, l), shape, BF16) for l in range(DEPTH)]
        dst = [P.dram("%s_a%d" % (name, l), [4 * shape[0], shape[1]], BF16) for l in range(DEPTH)]
        return src, dst
    kA_s, kA_a = zip(*[xch("kA%d" % m, [512, TOK]) for m in range(2)])
    kB_s, kB_a = zip(*[xch("kB%d" % g, [512, TOK]) for g in range(2)])
    kRC_s, kRC_a = xch("kRC", [320, TOK])
    vA_s, vA_a = zip(*[xch("vA%d" % g, [TOK, 512]) for g in range(2)])
    vB_s, vB_a = zip(*[xch("vB%d" % g, [TOK, 512]) for g in range(2)])
    vC_s, vC_a = xch("vC", [TOK, 256])

    def bc_rows(t, row_ap_offset, n):
        return t.ref(bass.AP(t.t, row_ap_offset, [[0, 128], [1, n]]))

    identf = P.sbuf("identf", [128, 128], F32)
    ident = P.sbuf("ident", [128, 128], BF16)
    onesf = P.sbuf("onesf", [128, 128], F32)
    P.memset(identf[:], 1.0, eng="pool")
    P.op("pool", lambda h: h.affine_select(identf.t[:], identf.t[:], [[-1, 128]], ALU.is_equal, 0.0,
                                           base=0, channel_multiplier=1), [identf[:]], [identf[:]])
    P.copy(ident[:], identf[:], eng="pool")
    P.memset(onesf[:], 1.0, eng="pool")

    ps = [P.psum("ps%d" % i, [128, 512], F32) for i in range(8)]

    def psbf(i):
        return ps[i].t[:].bitcast(BF16)

    hT = P.sbuf("hT", [128, KC, TOK], BF16)
    stats = P.sbuf("stats", [128, 4, 6], F32)
    mv = P.sbuf("mv", [128, 8], F32)
    cosT = P.sbuf("cosT", [128, NT, 32], F32)
    sinT = P.sbuf("sinT", [128, NT, 32], F32)

    caT = P.sbuf("caT", [128, KC, 1], F32)
    P.push()
    cT = P.sbuf("cT", [128, KC, 1], F32)
    P.dma(cT[:], c_in.ref(bass.AP(c_in.t, 0, [[1, 128], [128, KC], [1, 1]])), q="sp", allow_slow_non_contiguous=True)
    P.act(caT[:], cT[:], AF.Silu)
    def ada_ln(l, eng, q, CW, defer=None):
        wada_b = [P.sbuf("wada_b%d" % i, [128, CW], F32) for i in range(3)]
        accs = [P.sbuf("ada_acc%d" % i, [128, CW], F32) for i in range(2)]
        modrow = P.sbuf("modrow", [128, CW], F32)
        badab = P.sbuf("badab", [128, CW], F32)
        ncb = 6 * D // CW
        seq = [(cb, kc) for cb in range(ncb) for kc in range(KC)]

        def load(i):
            cb, kc = seq[i]
            P.dma(wada_b[i % 3][:], wada_in[l, kc * 128:(kc + 1) * 128, cb * CW:(cb + 1) * CW], q=q)

        def colsum(cb, acc):
            P.dma(badab[:], bc_rows(bada_in, l * 6 * D + cb * CW, CW), q="sp")
            for n4 in range(CW // 512):
                P.mm(ps[7][:], onesf[:], acc[:, n4 * 512:(n4 + 1) * 512])
                P.tt(modrow[:, n4 * 512:(n4 + 1) * 512], ps[7][:], badab[:, n4 * 512:(n4 + 1) * 512], ALU.add)
            P.dma(mod_d[l, :, cb * CW:(cb + 1) * CW], modrow[0:1, :], q="sp")

        def fma_block(cb):
            for kc in range(KC):
                i = cb * KC + kc
                if i == 0:
                    load(0)
                    load(1)
                if i + 2 < len(seq):
                    load(i + 2)
                wb = wada_b[i % 3]
                acc = accs[cb % 2]
                if kc == 0:
                    P.ts(acc[:], wb[:], caT[:, kc, :], ALU.mult, eng=eng)
                elif eng == "dve":
                    P.stt(acc[:], wb[:], caT[:, kc, :], acc[:], ALU.mult, ALU.add, eng=eng)
                else:
                    P.ts(wb[:], wb[:], caT[:, kc, :], ALU.mult, eng=eng)
                    P.tt(acc[:], acc[:], wb[:], ALU.add, eng=eng)

        if defer is None:
            for cb in range(ncb):
                fma_block(cb)
                colsum(cb, accs[cb % 2])
        else:
            fma_block(0)
            fma_block(1)
            for cb in range(ncb):
                def step(cb=cb):
                    colsum(cb, accs[cb % 2])
                    if cb + 2 < ncb:
                        fma_block(cb + 2)
                defer.append(step)

    posi = P.sbuf("posi", [128, NT, 1], I32)
    posf = P.sbuf("posf", [128, NT, 1], F32)
    P.dma(posi[:], pos_in.ref(bass.AP(pos_in.t, 0, [[1, 128], [128, NT], [1, 1]])), q="sp", allow_slow_non_contiguous=True)
    P.copy(posf[:], posi[:])
    ii = P.sbuf("iota_i", [128, 32], I32)
    iif = P.sbuf("iota_f", [128, 32], F32)
    invf = P.sbuf("invf", [128, 32], F32)
    for i_ in range(32):
        P.memset(invf[:, i_:i_ + 1], float(np.float32(10000.0) ** np.float32(-(2.0 * i_) / 64.0)), eng="pool")
    ang = P.sbuf("ang", [128, NT, 32], F32)
    ang2 = P.sbuf("ang2", [128, NT, 32], F32)
    P.tt(ang[:], Ref(posf.t[:].broadcast_to([128, NT, 32]), posf[:].keys),
         Ref(invf.t[:].unsqueeze(1).broadcast_to([128, NT, 32]), invf[:].keys), ALU.mult)
    ki = P.sbuf("ki", [128, NT, 32], I32)
    kf = P.sbuf("kf", [128, NT, 32], F32)
    msk = P.sbuf("msk", [128, NT, 32], F32)

    def sin_of(out, a_in):
        P.ts(kf[:], a_in, 1.0 / TWO_PI, ALU.mult)
        P.copy(ki[:], kf[:])
        P.copy(kf[:], ki[:])
        P.stt(ang2[:], kf[:], -TWO_PI, a_in, ALU.mult, ALU.add)
        P.ts(msk[:], ang2[:], math.pi, ALU.is_gt)
        P.stt(ang2[:], msk[:], -TWO_PI, ang2[:], ALU.mult, ALU.add)
        P.ts(msk[:], ang2[:], -math.pi, ALU.is_lt)
        P.stt(ang2[:], msk[:], TWO_PI, ang2[:], ALU.mult, ALU.add)
        P.ts(ang2[:], ang2[:], math.pi, ALU.min, -math.pi, ALU.max)
        P.act(out, ang2[:], AF.Sin)

    sin_of(sinT[:], ang[:])
    P.ts(ang[:], ang[:], math.pi / 2, ALU.add)
    sin_of(cosT[:], ang[:])
    P.pop()

    if dbg == "p0":
        P.push()
        o = P.dram("dbg_mod", [DEPTH, 1, 6 * D], F32, kind="ExternalOutput")
        for l in range(DEPTH):
            t_ = P.sbuf("dbg_t%d" % l, [1, 6 * D], F32)
            P.dma(t_[:], mod_d[l, :, :], q="sp")
            P.dma(o[l, :, :], t_[:], q="sp")
        o2 = P.dram("dbg_cs", [128, 2 * NT * 32], F32, kind="ExternalOutput")
        P.dma(o2[:, 0:NT * 32], cosT.ref(cosT.t[:].rearrange("p a b -> p (a b)")), q="sp")
        P.dma(o2[:, NT * 32:], sinT.ref(sinT.t[:].rearrange("p a b -> p (a b)")), q="sp")
        P.emit()
        return nc


    ADA_CW = 512
    ADA_NCB = 6 * D // ADA_CW
    ada_w = [P.sbuf("ada_w%d" % i, [128, ADA_CW], F32) for i in range(4)]
    ada_acc = [P.sbuf("ada_acc%d" % i, [128, ADA_CW], F32) for i in range(2)]
    ada_row = P.sbuf("ada_row", [128, ADA_CW], F32)
    ada_b = P.sbuf("ada_b", [128, ADA_CW], F32)
    ada_seq = [(l_, cb, kc) for l_ in range(nlayers) for cb in range(ADA_NCB) for kc in range(KC)]
    ada_pos = [0]

    def ada_load(i):
        l_, cb, kc = ada_seq[i]
        P.dma(ada_w[i % 4][:], wada_in[l_, kc * 128:(kc + 1) * 128, cb * ADA_CW:(cb + 1) * ADA_CW], q="sp")

    for i_ in range(3):
        ada_load(i_)

    def ada_tick(n=1):
        for _ in range(n):
            i = ada_pos[0]
            if i >= len(ada_seq):
                return
            ada_pos[0] += 1
            if i + 3 < len(ada_seq):
                ada_load(i + 3)
            l_, cb, kc = ada_seq[i]
            acc = ada_acc[(l_ * ADA_NCB + cb) % 2]
            wb = ada_w[i % 4]
            if kc == 0:
                P.ts(acc[:], wb[:], caT[:, kc, :], ALU.mult)
            else:
                P.stt(acc[:], wb[:], caT[:, kc, :], acc[:], ALU.mult, ALU.add)
            if kc == KC - 1:
                P.dma(ada_b[:], bc_rows(bada_in, l_ * 6 * D + cb * ADA_CW, ADA_CW), q="sp")
                P.mm(ps[7][:, 0:ADA_CW], onesf[:], acc[:])
                P.tt(ada_row[:], ps[7][:, 0:ADA_CW], ada_b[:], ALU.add)
                P.dma(mod_d[l_, :, cb * ADA_CW:(cb + 1) * ADA_CW], ada_row[0:1, :], q="sp")

    def ada_flush(upto=None):
        upto = len(ada_seq) if upto is None else min(upto, len(ada_seq))
        while ada_pos[0] < upto:
            ada_tick()

    ada_tick((2 * D // ADA_CW) * KC)

    def R(tobj, ap):
        return Ref(ap, (tobj.key,))

    gates = P.sbuf("gates", [128, NT, NEXP], F32)
    selb = P.sbuf("selb", [128, 8], F32)
    P.dma(selb[:], bc_rows(sel_in, 0, 8), q="sp")
    validp = P.sbuf("validp", [128, 1], F32)
    validn = P.sbuf("validn", [128, 1], F32)
    P.reduce(validp[:], selb[:, 0:3], ALU.add)
    P.reduce(validn[:], selb[:, 4:7], ALU.add)
    maskP = P.sbuf("maskP", [128, 128], BF16)
    maskN = P.sbuf("maskN", [128, 128], BF16)
    mtmp = P.sbuf("mtmp", [128, 128], F32)
    for mk, sg in ((maskP, 1), (maskN, -1)):
        P.memset(mtmp[:], 1.0, eng="pool")
        P.op("pool", lambda h, sg=sg: h.affine_select(mtmp.t[:], mtmp.t[:], [[-sg, 128]], ALU.is_ge, 0.0,
                                                      base=0, channel_multiplier=sg), [mtmp[:]], [mtmp[:]])
        P.copy(mk[:], mtmp[:], eng="pool")
    s12_ = [P.sbuf("s12_%d" % i, [128, 4], F32) for i in range(2)]
    sm_ = [P.sbuf("sm_%d" % i, [128, 16], F32) for i in range(2)]
    lnst_ = [P.sbuf("lnst_%d" % i, [128, 4, 6], F32) for i in range(2)]
    lncnt = [0]

    def rstd_from(out, var_ref, eps):
        P.ts(out, var_ref, eps, ALU.add)
        P.act(out, out, AF.Ln)
        P.act(out, out, AF.Exp, scale=-0.5)

    def layer_norm(xt_, out_ref, A, B, junk, xn):
        i_ = lncnt[0] % 2
        lncnt[0] += 1
        sm, xn, st = sm_[i_], xn[i_], lnst_[i_]
        for c4 in range(4):
            P.op("dve", lambda h, c4=c4: h.bn_stats(st.t[:, c4, :], xt_.t[:, c4 * 512:(c4 + 1) * 512]),
                 [xt_[:]], [st[:]])
        P.op("dve", lambda h: h.bn_aggr(sm.t[:, 0:2], st.t[:]), [st[:]], [sm[:]])
        rstd_from(sm[:, 3:4], sm[:, 1:2], LN_EPS)
        P.stt(xn[:], xt_[:], sm[:, 0:1], A[:], ALU.subtract, ALU.mult)
        P.stt(out_ref, xn[:], sm[:, 3:4], B[:], ALU.mult, ALU.add)

    def transpose_to_hT(hb, t, evac_i):
        for g8 in range(2):
            bank = 2 + g8
            for c in range(8):
                kc = g8 * 8 + c
                P.tr(R(ps[bank], psbf(bank)[:, c * 128:(c + 1) * 128]), hb[:, kc * 128:(kc + 1) * 128], ident[:])
            src = R(ps[bank], psbf(bank)[:, 0:1024].rearrange("p (c n) -> p c n", n=128))
            dst = R(hT, hT.t[:, g8 * 8:(g8 + 1) * 8, t * 128:(t + 1) * 128])
            P.copy(dst, src, eng="act")

    def rope(src_ap, src_t, dst_ap, dst_t, t, nh, tmps):
        X = src_ap.rearrange("p (h two d) -> p h two d", two=2, d=32)
        Y = dst_ap.rearrange("p (h two d) -> p h two d", two=2, d=32)
        x1, x2 = R(src_t, X[:, :, 0, :]), R(src_t, X[:, :, 1, :])
        y1, y2 = R(dst_t, Y[:, :, 0, :]), R(dst_t, Y[:, :, 1, :])
        cb_ = R(cosT, cosT.t[:, t, :].unsqueeze(1).broadcast_to([128, nh, 32]))
        sb_ = R(sinT, sinT.t[:, t, :].unsqueeze(1).broadcast_to([128, nh, 32]))
        ta, tb_, tc_, td_ = tmps
        a_ = R(ta, ta.t[:, 0:nh, :])
        b_ = R(tb_, tb_.t[:, 0:nh, :])
        c_ = R(tc_, tc_.t[:, 0:nh, :])
        d_ = R(td_, td_.t[:, 0:nh, :])
        P.tt(a_, x1, cb_, ALU.mult)
        P.tt(b_, x2, sb_, ALU.mult)
        P.tt(c_, x2, cb_, ALU.mult)
        P.tt(d_, x1, sb_, ALU.mult)
        P.tt(y1, a_, b_, ALU.subtract)
        P.tt(y2, c_, d_, ALU.add)

    def rms_norm_tile(src, n, gain, out, junk):
        i_ = lncnt[0] % 2
        lncnt[0] += 1
        s12, sm = s12_[i_], sm_[i_]
        P.memset(s12[:, 2:3], 0.0)
        P.act(junk, src, AF.Square, accum_out=s12[:, 2:3])
        P.ts(sm[:, 8:9], s12[:, 2:3], 1.0 / n, ALU.mult)
        rstd_from(sm[:, 9:10], sm[:, 8:9], RMS_EPS)
        P.stt(out, src, sm[:, 9:10], gain, ALU.mult, ALU.mult)

    for l in range(nlayers):
        lam_init = 0.8 - 0.6 * math.exp(-0.3 * l)
        x_src = x_in if l == 0 else xs_d
        P.push()
        wblk = [P.sbuf("wblk%d" % i, [128, KC, 512], BF16) for i in range(2)]
        xt = [P.sbuf("xt%d" % i, [128, D], F32) for i in range(2)]
        modA = P.sbuf("modA", [128, D], F32)
        modB = P.sbuf("modB", [128, D], F32)
        junk = [P.sbuf("junk%d" % i, [128, D], BF16) for i in range(2)]
        xn = [P.sbuf("xn%d" % i, [128, D], F32) for i in range(2)]
        hb = [P.sbuf("hb%d" % i, [128, D], BF16) for i in range(2)]
        P.dma(modB[:], bc_rows(mod_d, l * 6 * D + 0 * D, D), q="sp")
        P.dma(modA[:], bc_rows(mod_d, l * 6 * D + 1 * D, D), q="sp")
        P.ts(modA[:], modA[:], 1.0, ALU.add)
        for t in range(NT):
            P.dma(xt[t % 2][:], x_src[t * 128:(t + 1) * 128, :], q="sp")
            layer_norm(xt[t % 2], hb[t % 2][:], modA, modB, junk, xn)
            transpose_to_hT(hb[t % 2], t, t)
        P.pop()
        if dbg == "ln1" and l == 0:
            o = P.dram("dbg_hT", [D, TOK], BF16, kind="ExternalOutput")
            P.dma(R(o, o.t[:].rearrange("(c p) n -> p c n", p=128)), hT[:], q="sp")
            P.emit()
            return nc

        P.push()
        wblk = [P.sbuf("wblk%d" % i, [128, KC, 512], BF16) for i in range(2)]
        stageT = [P.sbuf("stageT%d" % i, [128, 4, TOK], BF16) for i in range(2)]
        stageV = [P.sbuf("stageV%d" % i, [128, NT, 512], BF16) for i in range(2)]
        cqT = P.sbuf("cqT", [128, 4, TOK], BF16)
        ckvT = P.sbuf("ckvT", [128, 2, TOK], BF16)
        wuq = P.sbuf("wuq", [128, 4, 1536], BF16)
        wukv = P.sbuf("wukv", [128, 2, 2048], BF16)
        tok = [P.sbuf("tok%d" % i, [128, 512], BF16) for i in range(2)]
        rt = [P.sbuf("rt%d" % i, [128, 8, 32], F32) for i in range(4)]
        qnb = P.sbuf("qnb", [128, 512], F32)
        kvnb = P.sbuf("kvnb", [128, 256], F32)
        junk2_ = [P.sbuf("junk2_%d" % i, [128, 512], BF16) for i in range(2)]
        P.dma(wuq[:], R(wuq_in, wuq_in.t[l].rearrange("(c p) n -> p c n", p=128)), q="pool")
        P.dma(wukv[:], R(wukv_in, wukv_in.t[l].rearrange("(c p) n -> p c n", p=128)), q="pool")
        P.dma(qnb[:], bc_rows(qn_in, l * 512, 512), q="sp")
        P.dma(kvnb[:], bc_rows(kvn_in, l * 256, 256), q="sp")

        nst = [0, 0]

        def flushT(st, nchunk, dst_t, row0, parts=128):
            if parts == 128:
                P.dma(R(dst_t, dst_t.t[row0:row0 + nchunk * 128, :].rearrange("(c p) n -> p c n", p=128)),
                      R(st, st.t[:, 0:nchunk, :]), q="sp")
            else:
                P.dma(R(dst_t, dst_t.t[row0:row0 + parts, :]), R(st, st.t[0:parts, 0, :]), q="sp")

        def flushV(sv, ncol, dst_t):
            P.dma(R(dst_t, dst_t.t[:, 0:ncol].rearrange("(t p) c -> p t c", p=128)),
                  R(sv, sv.t[:, :, 0:ncol]), q="sp")

        def tok_transpose(tk, ncol, st, t, bank, c_off=0, eng="act"):
            nchunk = (ncol + 127) // 128
            for c in range(nchunk):
                w_ = min(128, ncol - c * 128)
                P.tr(R(ps[bank], psbf(bank)[0:w_, c * 128:(c + 1) * 128]), R(tk, tk.t[:, c * 128:c * 128 + w_]),
                     ident[:])
            if ncol % 128 == 0:
                src = R(ps[bank], psbf(bank)[:, 0:nchunk * 128].rearrange("p (c n) -> p c n", n=128))
                dst = R(st, st.t[:, c_off:c_off + nchunk, t * 128:(t + 1) * 128])
            else:
                src = R(ps[bank], psbf(bank)[0:ncol, 0:128])
                dst = R(st, st.t[0:ncol, c_off, t * 128:(t + 1) * 128])
            P.copy(dst, src, eng=eng)

        def tok_transpose_view(tk, c0_, ncol, st, t, bank):
            P.tr(R(ps[bank], psbf(bank)[0:ncol, 0:128]), R(tk, tk.t[:, c0_:c0_ + ncol]), ident[:])
            P.copy(R(st, st.t[0:ncol, 0, t * 128:(t + 1) * 128]), R(ps[bank], psbf(bank)[0:ncol, 0:128]), eng="act")

        blocks = [
            (O_AQ, 512, "ropeT", (qTA_d, 0)), (O_AQ + 512, 512, "ropeT", (qTA_d, 512)),
            (O_AK, 512, "ropeT", (kA_s[0][l], 0)), (O_AK + 512, 512, "ropeT", (kA_s[1][l], 0)),
            (O_AV, 512, "v", vA_s[0][l]), (O_AV + 512, 512, "v", vA_s[1][l]),
            (O_CQ, 512, "cq", None), (O_CKV, 320, "ckvkr", None),
            (O_SQ, 512, "ropeT", (qTC_d, 0)), (O_SQ + 512, 512, "ropeT", (qTC_d, 512)),
            (O_SK, 512, "ckcv", None),
        ]
        rg = [[0, 1, 2, 3], [4, 5, 6, 7]]

        def gather(src_t, dst_t):
            P.op("pool", lambda h, src_t=src_t, dst_t=dst_t: h.collective_compute(
                "AllGather", ALU.bypass, replica_groups=rg, ins=[src_t.t[:]], outs=[dst_t.t[:]]),
                [src_t[:]], [dst_t[:]], kind="cc")

        cc_after = {4: [(kA_s[0][l], kA_a[0][l])], 5: [(kA_s[1][l], kA_a[1][l])],
                    6: [(vA_s[0][l], vA_a[0][l])], 7: [(vA_s[1][l], vA_a[1][l])]}
        for bi, (c0, ncol, kind, dest) in enumerate(blocks):
            for pr in cc_after.get(bi, ()):
                gather(*pr)
            wb = wblk[bi % 2]
            P.dma(R(wb, wb.t[:, :, 0:ncol]),
                  R(w_in, w_in.t[l, :, c0:c0 + ncol].rearrange("(c p) n -> p c n", p=128)), q="pool")
            if kind in ("ropeT", "ckvkr", "ckcv"):
                st = stageT[nst[0] % 2]
                nst[0] += 1
            if kind in ("v", "ckcv"):
                sv = stageV[nst[1] % 2]
                nst[1] += 1
            pend_tr = []

            def flush_tr():
                while pend_tr:
                    pend_tr.pop(0)()

            for t in range(NT):
                pb = t % 2
                pst = ps[pb]
                for kc in range(KC):
                    P.mm(pst[:, 0:ncol], hT[:, kc, t * 128:(t + 1) * 128], wb[:, kc, 0:ncol],
                         start=(kc == 0), stop=(kc == KC - 1))
                flush_tr()
                tk = tok[t % 2]
                if kind == "ropeT":
                    rope(pst.t[:, 0:512], pst, tk.t[:, 0:512], tk, t, 8, rt)
                    pend_tr.append(lambda tk=tk, st=st, t=t, pb=pb: tok_transpose(tk, 512, st, t, 2 + pb))
                elif kind == "v":
                    P.copy(R(sv, sv.t[:, t, 0:ncol]), pst[:, 0:ncol], eng="act")
                elif kind == "cq":
                    rms_norm_tile(pst[:, 0:512], 512, qnb[:], tk[:, 0:512], junk2_[t % 2][:, 0:512])
                    pend_tr.append(lambda tk=tk, t=t, pb=pb: tok_transpose(tk, 512, cqT, t, 2 + pb))
                elif kind == "ckvkr":
                    rms_norm_tile(pst[:, 0:256], 256, kvnb[:], tk[:, 0:256], junk2_[t % 2][:, 0:256])
                    rope(pst.t[:, 256:320], pst, tk.t[:, 256:320], tk, t, 1, rt)
                    pend_tr.append(lambda tk=tk, t=t, pb=pb: tok_transpose(tk, 256, ckvT, t, 2 + pb))
                    pend_tr.append(lambda tk=tk, st=st, t=t, pb=pb: tok_transpose_view(tk, 256, 64, st, t, 4 + pb))
                elif kind == "ckcv":
                    rope(pst.t[:, 0:256], pst, tk.t[:, 0:256], tk, t, 4, rt)
                    P.copy(R(sv, sv.t[:, t, 0:256]), pst[:, 256:512], eng="act")
                    pend_tr.append(lambda tk=tk, st=st, t=t, pb=pb: tok_transpose(tk, 256, st, t, 2 + pb))
                ada_tick(1)
            flush_tr()
            if kind == "ropeT":
                flushT(st, 4, dest[0], dest[1])
            elif kind == "v":
                flushV(sv, ncol, dest)
            elif kind == "ckvkr":
                flushT(st, 1, kRC_s[l], 0, parts=64)
            elif kind == "ckcv":
                flushT(st, 2, kRC_s[l], 64)
                flushV(sv, 256, vC_s[l])

        for (wsrc, nkc, cstride, srcT, dsts) in ((wuq, 4, 192, cqT, None),
                                                   (wukv, 2, 256, ckvT, (kB_s[0][l], kB_s[1][l]))):
            for hg in range(2):
                st = stageT[nst[0] % 2]
                nst[0] += 1
                for hh in range(4):
                    h_ = hg * 4 + hh
                    for tb in range(2):
                        pst = ps[(hh * 2 + tb) % 2]
                        for kc in range(nkc):
                            P.mm(pst[:], R(wsrc, wsrc.t[:, kc, h_ * cstride:h_ * cstride + 128]),
                                 R(srcT, srcT.t[:, kc, tb * 512:(tb + 1) * 512]),
                                 start=(kc == 0), stop=(kc == nkc - 1))
                        P.copy(R(st, st.t[:, hh, tb * 512:(tb + 1) * 512]), pst[:],
                               eng="act" if tb == 0 else "dve")
                        ada_tick(1)
                if dsts is None:
                    flushT(st, 4, qTBn_d, hg * 512)
                else:
                    flushT(st, 4, dsts[hg], 0)
        st = stageT[nst[0] % 2]
        nst[0] += 1
        for t in range(NT):
            pb = t % 2
            pst = ps[pb]
            for kc in range(4):
                rhs = R(wuq, wuq.t[:, kc, :].rearrange("p (h c) -> p h c", c=192)[:, :, 128:192])
                P.mm(pst[:], R(cqT, cqT.t[:, kc, t * 128:(t + 1) * 128]), rhs, start=(kc == 0), stop=(kc == 3))
            flush_tr()
            tk = tok[t % 2]
            rope(pst.t[:, 0:512], pst, tk.t[:, 0:512], tk, t, 8, rt)
            pend_tr.append(lambda tk=tk, st=st, t=t, pb=pb: tok_transpose(tk, 512, st, t, 2 + pb))
        flush_tr()
        flushT(st, 4, qTBr_d, 0)
        for hg in range(2):
            sv = stageV[nst[1] % 2]
            nst[1] += 1
            for t in range(NT):
                pst = ps[t % 2]
                for kc in range(2):
                    rhs = R(wukv, wukv.t[:, kc, :].rearrange("p (h c) -> p h c", c=256)[:, hg * 4:(hg + 1) * 4, 128:256])
                    P.mm(pst[:], R(ckvT, ckvT.t[:, kc, t * 128:(t + 1) * 128]), rhs, start=(kc == 0), stop=(kc == 1))
                P.copy(R(sv, sv.t[:, t, :]), pst[:], eng="act" if t % 2 == 0 else "dve")
            flushV(sv, 512, vB_s[hg][l])
        P.pop()

        for pr in [(kB_s[0][l], kB_a[0][l]), (kB_s[1][l], kB_a[1][l]),
                   (kRC_s[l], kRC_a[l]), (vB_s[0][l], vB_a[0][l]), (vB_s[1][l], vB_a[1][l]),
                   (vC_s[l], vC_a[l])]:
            gather(*pr)

        if dbg == "p1" and l == 0:
            for nm, tt_ in (("qTA", qTA_d), ("qTBn", qTBn_d), ("qTBr", qTBr_d), ("qTC", qTC_d),
                            ("kA0", kA_a[0][l]), ("kA1", kA_a[1][l]), ("kB0", kB_a[0][l]), ("kB1", kB_a[1][l]),
                            ("kRC", kRC_a[l]), ("vA0", vA_a[0][l]), ("vA1", vA_a[1][l]), ("vB0", vB_a[0][l]),
                            ("vB1", vB_a[1][l]), ("vC", vC_a[l])):
                shp = list(tt_.t.shape)
                o = P.dram("dbg_" + nm, shp, BF16, kind="ExternalOutput")
                P.dma(o[:], tt_[:], q="sp")
            P.emit()
            return nc


        P.push()
        yT = P.sbuf("yT", [128, 24, TOK], BF16)
        P.push()
        KT = [P.sbuf("KT%d" % i, [128, S], BF16) for i in range(2)]
        Vt = [P.sbuf("Vt%d" % i, [128, 32, 132], BF16) for i in range(2)]
        QT = [P.sbuf("QT%d" % i, [128, TOK], BF16) for i in range(2)]
        Kr = P.sbuf("Kr", [64, S], BF16)
        QTr = [P.sbuf("QTr%d" % i, [64, TOK], BF16) for i in range(2)]
        Pt = [P.sbuf("Pt%d" % i, [128, 512], BF16) for i in range(4)]
        for v_ in Vt:
            P.memset(R(v_, v_.t[:, :, 128:129]), 1.0, eng="pool")
        lq = P.sbuf("lq", [128, 128], F32)
        lk = P.sbuf("lk", [128, 128], F32)
        lam2 = P.sbuf("lam2", [128, 2], F32)
        neglam = P.sbuf("neglam", [128, 1], F32)
        sublnS = P.sbuf("sublnS", [128, 128], F32)
        esink = P.sbuf("esink", [128, 16], F32)
        P.dma(lq[:], bc_rows(lamq_in, l * 128, 128), q="sp")
        P.dma(lk[:], bc_rows(lamk_in, l * 128, 128), q="sp")
        P.dma(sublnS[:], bc_rows(subln_in, l * 128, 128), q="sp")
        P.dma(esink[:], bc_rows(sink_in, l * 16, 16), q="sp")
        P.tt(lq[:], lq[:], lk[:], ALU.mult)
        P.reduce(lam2[:], R(lq, lq.t[:].rearrange("p (m d) -> p m d", m=2)), ALU.add)
        P.act(lam2[:], lam2[:], AF.Exp)
        P.act(esink[:], esink[:], AF.Exp)
        P.tt(neglam[:], lam2[:, 1:2], lam2[:, 0:1], ALU.subtract)
        P.ts(neglam[:], neglam[:], -lam_init, ALU.add)
        P.ts(sublnS[:], sublnS[:], 1.0 - lam_init, ALU.mult)
        fo = P.sbuf("fo", [128, 128], F32)
        fsq = P.sbuf("fsq", [128, 128], F32)
        fr = P.sbuf("fr", [128, 8], F32)
        Oc = P.sbuf("Oc", [128, 3, 396], F32)
        yblk = [P.sbuf("yblk%d" % i, [128, 512], BF16) for i in range(2)]
        nblk = [0]
        pending = []
        ada_pending = []

        def flush_pending():
            while pending:
                pending.pop(0)()

        def oslot(a):
            return ps[4 + a // 3], (a % 3) * 132

        def ocslot(a, c0, c1):
            return R(Oc, Oc.t[:, a // 3, (a % 3) * 132 + c0:(a % 3) * 132 + c1])

        def defer_transpose(yb, ncol, chunk0, nchunk_out, tok0, ntok):
            def f():
                nsub = ntok // 128
                nch = ncol // 128
                for sub in range(nsub):
                    for c in range(nch):
                        P.tr(R(ps[7], psbf(7)[:, (c * nsub + sub) * 128:(c * nsub + sub + 1) * 128]),
                             R(yb, yb.t[:, sub * ncol + c * 128:sub * ncol + (c + 1) * 128]), ident[:])
                P.copy(R(yT, yT.t[:, chunk0:chunk0 + nch, tok0:tok0 + ntok]),
                       R(ps[7], psbf(7)[:, 0:nch * ntok].rearrange("p (c n) -> p c n", n=ntok)))
            pending.append(f)

        def loads_A(h_):
            kt_, v_, q_ = KT[h_ % 2], Vt[h_ % 2], QT[h_ % 2]
            for m in range(2):
                P.dma(R(kt_, kt_.t[m * 64:(m + 1) * 64, :].rearrange("p (r n) -> p r n", r=4)),
                      R(kA_a[m][l], kA_a[m][l].t[:].rearrange("(r f) n -> f r n", r=4)[h_ * 64:(h_ + 1) * 64, :, :]),
                      q="sp")
                P.dma(R(q_, q_.t[m * 64:(m + 1) * 64, :]), qTA_d[m * 512 + h_ * 64:m * 512 + (h_ + 1) * 64, :], q="sp")
            P.dma(R(v_, v_.t[:, :, 0:128]),
                  R(vA_a[h_ // 4][l], vA_a[h_ // 4][l].t[:, (h_ % 4) * 128:(h_ % 4 + 1) * 128].rearrange(
                      "(k p) c -> p k c", p=128)), q="sp")

        def S_A(blk, kt, m):
            h_, qb = blk
            kt_, q_ = KT[h_ % 2], QT[h_ % 2]
            par = kt % 2
            P.mm(ps[par * 2 + m][:], R(kt_, kt_.t[m * 64:(m + 1) * 64, kt * 128:(kt + 1) * 128]),
                 R(q_, q_.t[m * 64:(m + 1) * 64, qb * 512:(qb + 1) * 512]))
            P.act(Pt[par * 2 + m][:], ps[par * 2 + m][:], AF.Exp, scale=0.125)

        def PV_A(blk, kt, started, m):
            h_, qb = blk
            v_ = Vt[h_ % 2]
            par = kt % 2
            for qs in range(4):
                if True:
                    bank, off = oslot(m * 4 + qs)
                    st_ = kt == 0 and bank.key not in started
                    started.add(bank.key)
                    P.mm(R(bank, bank.t[:, off:off + 129]),
                         R(Pt[par * 2 + m], Pt[par * 2 + m].t[:, qs * 128:(qs + 1) * 128]),
                         R(v_, v_.t[:, kt, 0:129]), start=st_, stop=(kt == 31), skip_group_check=True)

        def fin_A(blk):
            h_, qb = blk
            for bnk in range(3):
                P.copy(Oc[:, bnk, :], R(ps[4 + bnk], ps[4 + bnk].t[:, 0:396]))
            yb = yblk[nblk[0] % 2]
            nblk[0] += 1
            for qs in range(4):
                P.op("dve", lambda h, qs=qs: h.reciprocal(fr.t[:, 0:1], ocslot(qs, 128, 129).ap), [Oc[:]], [fr[:]])
                P.op("dve", lambda h, qs=qs: h.reciprocal(fr.t[:, 1:2], ocslot(4 + qs, 128, 129).ap), [Oc[:]], [fr[:]])
                P.tt(fr[:, 2:3], fr[:, 1:2], neglam[:], ALU.mult)
                P.ts(fo[:], ocslot(qs, 0, 128), fr[:, 0:1], ALU.mult)
                P.stt(fo[:], ocslot(4 + qs, 0, 128), fr[:, 2:3], fo[:], ALU.mult, ALU.add)
                P.tt(fsq[:], fo[:], fo[:], ALU.mult)
                P.reduce(fr[:, 3:4], fsq[:], ALU.add)
                P.ts(fr[:, 3:4], fr[:, 3:4], 1.0 / 128, ALU.mult)
                rstd_from(fr[:, 4:5], fr[:, 3:4], RMS_EPS)
                P.stt(R(yb, yb.t[:, qs * 128:(qs + 1) * 128]), fo[:], fr[:, 4:5], sublnS[:], ALU.mult, ALU.mult)
            defer_transpose(yb, 128, h_, 1, qb * 512, 512)

        def run_dense(blocks_, loads, S_, PV_, fin_, nm, ticks):
            loads(blocks_[0][0])
            for m in range(nm):
                S_(blocks_[0], 0, m)
            for bi, blk in enumerate(blocks_):
                if blk[1] == 0 and blk[0] + 1 < 8:
                    loads(blk[0] + 1)
                started = set()
                for kt in range(32):
                    nxt = (blk, kt + 1) if kt + 1 < 32 else ((blocks_[bi + 1], 0) if bi + 1 < len(blocks_) else None)
                    if nxt is not None:
                        for m in range(nm):
                            S_(nxt[0], nxt[1], m)
                    for m in range(nm):
                        PV_(blk, kt, started, m)
                    if ticks:
                        ada_tick(ticks)
                    if kt == 6:
                        flush_pending()
                    if kt == 12 and ada_pending and (bi % 2 == 1):
                        ada_pending.pop(0)()
                fin_(blk)
            flush_pending()

        blocks_hq = [(h_, qb) for h_ in range(8) for qb in range(2)]
        run_dense(blocks_hq, loads_A, S_A, PV_A, fin_A, 2, 0)

        P.dma(R(Kr, Kr.t[:, :].rearrange("p (r n) -> p r n", r=4)),
              R(kRC_a[l], kRC_a[l].t[:].rearrange("(r f) n -> f r n", r=4)[0:64, :, :]), q="sp")
        sB = 192.0 ** -0.5

        def loads_B(h_):
            kt_, v_, q_, qr_ = KT[h_ % 2], Vt[h_ % 2], QT[h_ % 2], QTr[h_ % 2]
            P.dma(R(kt_, kt_.t[:, :].rearrange("p (r n) -> p r n", r=4)),
                  R(kB_a[h_ // 4][l], kB_a[h_ // 4][l].t[:].rearrange("(r f) n -> f r n", r=4)[
                      (h_ % 4) * 128:(h_ % 4 + 1) * 128, :, :]), q="sp")
            P.dma(q_[:], qTBn_d[h_ * 128:(h_ + 1) * 128, :], q="sp")
            P.dma(qr_[:], qTBr_d[h_ * 64:(h_ + 1) * 64, :], q="sp")
            P.dma(R(v_, v_.t[:, :, 0:128]),
                  R(vB_a[h_ // 4][l], vB_a[h_ // 4][l].t[:, (h_ % 4) * 128:(h_ % 4 + 1) * 128].rearrange(
                      "(k p) c -> p k c", p=128)), q="sp")

        def S_B(blk, kt, m):
            h_, qb = blk
            kt_, q_, qr_ = KT[h_ % 2], QT[h_ % 2], QTr[h_ % 2]
            par = kt % 4
            P.mm(ps[par][:], R(kt_, kt_.t[:, kt * 128:(kt + 1) * 128]),
                 R(q_, q_.t[:, qb * 512:(qb + 1) * 512]), start=True, stop=False)
            P.mm(ps[par][:], R(Kr, Kr.t[0:64, kt * 128:(kt + 1) * 128]),
                 R(qr_, qr_.t[0:64, qb * 512:(qb + 1) * 512]), start=False, stop=True)
            P.act(Pt[par][:], ps[par][:], AF.Exp, scale=sB)

        def PV_B(blk, kt, started, m):
            h_, qb = blk
            v_ = Vt[h_ % 2]
            par = kt % 4
            for qs in range(4):
                bank, off = oslot(qs)
                st_ = kt == 0 and bank.key not in started
                started.add(bank.key)
                P.mm(R(bank, bank.t[:, off:off + 129]), R(Pt[par], Pt[par].t[:, qs * 128:(qs + 1) * 128]),
                     R(v_, v_.t[:, kt, 0:129]), start=st_, stop=(kt == 31), skip_group_check=True)

        def fin_B(blk):
            h_, qb = blk
            for bnk in range(2):
                P.copy(Oc[:, bnk, :], R(ps[4 + bnk], ps[4 + bnk].t[:, 0:396]))
            yb = yblk[nblk[0] % 2]
            nblk[0] += 1
            for qs in range(4):
                P.op("dve", lambda h, qs=qs: h.reciprocal(fr.t[:, 0:1], ocslot(qs, 128, 129).ap), [Oc[:]], [fr[:]])
                P.ts(R(yb, yb.t[:, qs * 128:(qs + 1) * 128]), ocslot(qs, 0, 128), fr[:, 0:1], ALU.mult)
            defer_transpose(yb, 128, 8 + h_, 1, qb * 512, 512)

        run_dense(blocks_hq, loads_B, S_B, PV_B, fin_B, 1, 1)

        Kext = P.sbuf("Kext", [64, 10 * 128], BF16)
        Vext = P.sbuf("Vext", [128, 10, 68], BF16)
        Kc = P.sbuf("Kc", [64, 6, 128], BF16)
        Vc = P.sbuf("Vc", [128, 6, 64], BF16)
        Qc = P.sbuf("Qc", [64, 4, TOK], BF16)
        hkf = P.sbuf("hkf", [128, 128], F32)
        den4 = P.sbuf("den4", [128, 8], F32)
        P.memset(R(Vext, Vext.t[:, :, 64:65]), 1.0, eng="pool")

        def S_C(g, n, kk):
            pb = (n * 3 + kk) % 4
            P.mm(ps[pb][:], R(Kext, Kext.t[0:64, (n + kk) * 128:(n + kk + 1) * 128]),
                 R(Qc, Qc.t[0:64, :, n * 128:(n + 1) * 128]))
            pt_ = Pt[pb]
            P.act(pt_[:], ps[pb][:], AF.Exp, scale=0.125)
            pv = R(pt_, pt_.t[:].rearrange("p (r q) -> p r q", r=4))
            if kk == 0:
                sc_ = validp[:, 0:1] if n == 0 else 1.0
                P.stt(pv, pv, sc_, R(maskP, maskP.t[:].unsqueeze(1).broadcast_to([128, 4, 128])),
                      ALU.mult, ALU.mult)
            elif kk == 2:
                sc_ = validn[:, 0:1] if n == NT - 1 else 1.0
                P.stt(pv, pv, sc_, R(maskN, maskN.t[:].unsqueeze(1).broadcast_to([128, 4, 128])),
                      ALU.mult, ALU.mult)

        def PV_C(g, n, kk):
            pt_ = Pt[(n * 3 + kk) % 4]
            for r in range(4):
                P.mm(R(ps[4], ps[4].t[:, r * 68:r * 68 + 65]), R(pt_, pt_.t[:, r * 128:(r + 1) * 128]),
                     R(Vext, Vext.t[:, n + kk, 0:65]), start=(kk == 0 and r == 0), stop=(kk == 2), skip_group_check=True)

        for g in range(4):
            kview = kRC_a[l].t[:].rearrange("(r f) n -> f r n", r=4)[64 + g * 64:64 + (g + 1) * 64, :, :]
            P.dma(Kc[:, 0:3, :], R(kRC_a[l], kview[:, 0:3, 896:1024]), q="sp")
            P.dma(Kc[:, 3:6, :], R(kRC_a[l], kview[:, 1:4, 0:128]), q="sp")
            vview = vC_a[l].t[:, g * 64:(g + 1) * 64].rearrange("(r t) c -> t r c", r=4)
            P.dma(Vc[:, 0:3, :], R(vC_a[l], vview[896:1024, 0:3, :]), q="sp")
            P.dma(Vc[:, 3:6, :], R(vC_a[l], vview[0:128, 1:4, :]), q="sp")
            P.dma(R(Kext, Kext.t[:, 128:1152]), kRC_s[l][64 + g * 64:64 + (g + 1) * 64, :], q="sp")
            P.dma(R(Vext, Vext.t[:, 1:9, 0:64]),
                  R(vC_s[l], vC_s[l].t[:, g * 64:(g + 1) * 64].rearrange("(t p) c -> p t c", p=128)), q="sp")
            P.dma(Qc[:], R(qTC_d, qTC_d.t[g * 256:(g + 1) * 256, :].rearrange("(r d) n -> d r n", d=64)), q="sp")
            for side, c0, s0 in ((0, 0, 0), (9, 3, 4)):
                P.ts(R(hkf, hkf.t[0:64, :]), R(Kc, Kc.t[:, c0, :]), selb[0:64, s0:s0 + 1], ALU.mult)
                P.stt(R(hkf, hkf.t[0:64, :]), R(Kc, Kc.t[:, c0 + 1, :]), selb[0:64, s0 + 1:s0 + 2],
                      R(hkf, hkf.t[0:64, :]), ALU.mult, ALU.add)
                P.stt(R(Kext, Kext.t[:, side * 128:(side + 1) * 128]), R(Kc, Kc.t[:, c0 + 2, :]),
                      selb[0:64, s0 + 2:s0 + 3], R(hkf, hkf.t[0:64, :]), ALU.mult, ALU.add)
                P.ts(R(hkf, hkf.t[:, 0:64]), R(Vc, Vc.t[:, c0, :]), selb[:, s0:s0 + 1], ALU.mult)
                P.stt(R(hkf, hkf.t[:, 0:64]), R(Vc, Vc.t[:, c0 + 1, :]), selb[:, s0 + 1:s0 + 2],
                      R(hkf, hkf.t[:, 0:64]), ALU.mult, ALU.add)
                P.stt(R(Vext, Vext.t[:, side, 0:64]), R(Vc, Vc.t[:, c0 + 2, :]), selb[:, s0 + 2:s0 + 3],
                      R(hkf, hkf.t[:, 0:64]), ALU.mult, ALU.add)
            steps = [(n, kk) for n in range(NT) for kk in range(3)]
            S_C(g, 0, 0)
            for si, (n, kk) in enumerate(steps):
                if si + 1 < len(steps):
                    S_C(g, *steps[si + 1])
                PV_C(g, n, kk)
                if kk == 0:
                    flush_pending()
                if kk == 2:
                    P.copy(R(Oc, Oc.t[:, 0, 0:272]), R(ps[4], ps[4].t[:, 0:272]))
                    ov = Oc.t[:, 0, 0:272].rearrange("p (r c) -> p r c", c=68)
                    P.tt(den4[:, 0:4], R(Oc, ov[:, :, 64]), esink[:, g * 4:(g + 1) * 4], ALU.add)
                    P.op("dve", lambda h: h.reciprocal(den4.t[:, 4:8], den4.t[:, 0:4]), [den4[:]], [den4[:]])
                    yb = yblk[nblk[0] % 2]
                    nblk[0] += 1
                    P.tt(R(yb, yb.t[:, 0:256].rearrange("p (r c) -> p r c", c=64)), R(Oc, ov[:, :, 0:64]),
                         R(den4, den4.t[:, 4:8].unsqueeze(2).broadcast_to([128, 4, 64])), ALU.mult)
                    defer_transpose(yb, 256, 16 + g * 2, 2, n * 128, 128)
            flush_pending()
        while ada_pending:
            ada_pending.pop(0)()
        ada_flush()
        P.pop()
        ymT = P.sbuf("ymT", [128, KC, TOK], BF16)

        if dbg in ("p3", "p4", "l1") and l == dbg_layer:
            o = P.dram("dbg_yT", [24 * 128, TOK], BF16, kind="ExternalOutput")
            P.dma(R(o, o.t[:].rearrange("(c p) n -> p c n", p=128)), yT[:], q="sp")
            if dbg == "p3":
                om = P.dram("dbg_mod", [DEPTH, 1, 6 * D], F32, kind="ExternalOutput")
                P.push()
                t_ = P.sbuf("dbg_t", [1, 6 * D], F32)
                for l_ in range(nlayers):
                    P.dma(t_[:], mod_d[l_, :, :], q="sp")
                    P.dma(om[l_, :, :], t_[:], q="sp")
                P.pop()
                P.emit()
                return nc

        P.push()
        wg = [P.sbuf("wg%d" % i, [128, KC, 512], BF16) for i in range(2)]
        wbr = [P.sbuf("wbr%d" % i, [128, 8, 512], BF16) for i in range(2)]
        ycb = P.sbuf("ycb", [128, NT, 512], F32)
        sig = [P.sbuf("sig%d" % i, [128, 512], BF16) for i in range(2)]
        gtmp = [P.sbuf("gtmp%d" % i, [128, 512], F32) for i in range(2)]
        ybf = [P.sbuf("ybf%d" % i, [128, 512], BF16) for i in range(2)]
        cnt = 0
        for cb in range(4):
            for br, wsrc in enumerate((wa_in, wb_in, wc_in)):
                g_, w_ = wg[cnt % 2], wbr[cnt % 2]
                cnt += 1
                gc0 = O_G + br * D + cb * 512
                P.dma(g_[:], R(w_in, w_in.t[l, :, gc0:gc0 + 512].rearrange("(c p) n -> p c n", p=128)), q="pool")
                P.dma(w_[:], R(wsrc, wsrc.t[l, :, cb * 512:(cb + 1) * 512].rearrange("(c p) n -> p c n", p=128)),
                      q="pool")
                for t in range(NT):
                    pg, pp = ps[(t % 2) * 2], ps[(t % 2) * 2 + 1]
                    for kc in range(KC):
                        P.mm(pg[:], hT[:, kc, t * 128:(t + 1) * 128], g_[:, kc, :], start=(kc == 0), stop=(kc == KC - 1))
                    for kc in range(8):
                        P.mm(pp[:], R(yT, yT.t[:, br * 8 + kc, t * 128:(t + 1) * 128]), w_[:, kc, :],
                             start=(kc == 0), stop=(kc == 7))
                    P.act(sig[t % 2][:], pg[:], AF.Sigmoid)
                    if br == 0:
                        P.tt(ycb[:, t, :], pp[:], sig[t % 2][:], ALU.mult)
                    else:
                        P.tt(gtmp[t % 2][:], pp[:], sig[t % 2][:], ALU.mult)
                        P.tt(ycb[:, t, :], ycb[:, t, :], gtmp[t % 2][:], ALU.add)
            for t in range(NT):
                P.copy(ybf[t % 2][:], ycb[:, t, :], eng="act")
                bank = 4 + t % 2
                for c in range(4):
                    P.tr(R(ps[bank], psbf(bank)[:, c * 128:(c + 1) * 128]), R(ybf[t % 2], ybf[t % 2].t[:, c * 128:(c + 1) * 128]),
                         ident[:])
                P.copy(R(ymT, ymT.t[:, cb * 4:(cb + 1) * 4, t * 128:(t + 1) * 128]),
                       R(ps[bank], psbf(bank)[:, 0:512].rearrange("p (c n) -> p c n", n=128)),
                       eng="act" if t % 2 == 0 else "dve")
        P.pop()

        P.push()
        wo = [P.sbuf("wo%d" % i, [128, KC, 512], BF16) for i in range(2)]
        g1b = P.sbuf("g1b", [128, D], F32)
        xq = [P.sbuf("xq%d" % i, [128, 512], F32) for i in range(2)]
        zq = [P.sbuf("zq%d" % i, [128, 512], F32) for i in range(2)]
        P.dma(g1b[:], bc_rows(mod_d, l * 6 * D + 2 * D, D), q="sp")
        for ob in range(4):
            w_ = wo[ob % 2]
            P.dma(w_[:], R(wo_in, wo_in.t[l, :, ob * 512:(ob + 1) * 512].rearrange("(c p) n -> p c n", p=128)), q="pool")
            for t in range(NT):
                pst = ps[t % 2]
                for kc in range(KC):
                    P.mm(pst[:], R(ymT, ymT.t[:, kc, t * 128:(t + 1) * 128]), w_[:, kc, :],
                         start=(kc == 0), stop=(kc == KC - 1))
                P.dma(xq[t % 2][:], x_src[t * 128:(t + 1) * 128, ob * 512:(ob + 1) * 512], q="sp")
                P.tt(zq[t % 2][:], pst[:], g1b[:, ob * 512:(ob + 1) * 512], ALU.mult)
                P.stt(zq[t % 2][:], xq[t % 2][:], ALPHA, zq[t % 2][:], ALU.mult, ALU.add)
                P.dma(x1p_d[t * 128:(t + 1) * 128, ob * 512:(ob + 1) * 512], zq[t % 2][:], q="sp")
        P.pop()
        P.pop()

        P.push()
        xt2 = [P.sbuf("xt2_%d" % i, [128, D], F32) for i in range(2)]
        x1t = [P.sbuf("x1t%d" % i, [128, D], F32) for i in range(2)]
        lng = P.sbuf("lng", [128, D], F32)
        lnb = P.sbuf("lnb", [128, D], F32)
        modA2 = P.sbuf("modA2", [128, D], F32)
        modB2 = P.sbuf("modB2", [128, D], F32)
        junk = [P.sbuf("junk_c%d" % i, [128, D], BF16) for i in range(2)]
        xn = [P.sbuf("xn_c%d" % i, [128, D], F32) for i in range(2)]
        h2f_ = [P.sbuf("h2f%d" % i, [128, D], F32) for i in range(2)]
        hb2 = [P.sbuf("hb2_%d" % i, [128, D], BF16) for i in range(2)]
        h2Tf = P.sbuf("h2Tf", [128, KC, 128], F32)
        rwf = P.sbuf("rwf", [128, KC, NEXP], F32)
        rbb = P.sbuf("rbb", [128, NEXP], F32)
        rsc = P.sbuf("rsc", [128, NT, NEXP], F32)
        rbi = P.sbuf("rbi", [128, NT, NEXP], F32)
        req = P.sbuf("req", [128, NT, NEXP], F32)
        rg2 = P.sbuf("rg2", [128, NT, NEXP], F32)
        rm1 = P.sbuf("rm1", [128, NT * 4], F32)
        rm2 = P.sbuf("rm2", [128, NT * 4], F32)
        rgs = P.sbuf("rgs", [128, NT * 4], F32)
        rgm = P.sbuf("rgm", [128, NT], F32)
        P.dma(lng[:], bc_rows(lnmg_in, l * D, D), q="sp")
        P.dma(lnb[:], bc_rows(lnmb_in, l * D, D), q="sp")
        P.dma(modB2[:], bc_rows(mod_d, l * 6 * D + 3 * D, D), q="sp")
        P.dma(modA2[:], bc_rows(mod_d, l * 6 * D + 4 * D, D), q="sp")
        P.ts(modA2[:], modA2[:], 1.0, ALU.add)
        P.dma(rwf[:], R(rw_in, rw_in.t[:].rearrange("(c p) e -> p c e", p=128)), q="sp")
        P.dma(rbb[:], bc_rows(rb_in, 0, NEXP), q="sp")
        for t in range(NT):
            P.dma(xt2[t % 2][:], x1p_d[t * 128:(t + 1) * 128, :], q="sp")
            layer_norm(xt2[t % 2], x1t[t % 2][:], lng, lnb, junk, xn)
            P.dma(xs_d[t * 128:(t + 1) * 128, :], x1t[t % 2][:], q="sp")
            h2f = h2f_[t % 2]
            layer_norm(x1t[t % 2], h2f[:], modA2, modB2, junk, xn)
            P.copy(hb2[t % 2][:], h2f[:], eng="act")
            transpose_to_hT(hb2[t % 2], t, t)
            for c4 in range(4):
                bank = 4 + c4
                for c in range(4):
                    kc = c4 * 4 + c
                    P.tr(R(ps[bank], ps[bank].t[:, c * 128:(c + 1) * 128]), h2f[:, kc * 128:(kc + 1) * 128], identf[:])
                P.copy(R(h2Tf, h2Tf.t[:, c4 * 4:(c4 + 1) * 4, :]),
                       R(ps[bank], ps[bank].t[:].rearrange("p (c n) -> p c n", n=128)),
                       eng="act")
            for kc in range(KC):
                P.mm(R(ps[1], ps[1].t[:, 0:NEXP]), h2Tf[:, kc, :], rwf[:, kc, :], start=(kc == 0), stop=(kc == KC - 1))
            P.act(R(rsc, rsc.t[:, t, :]), R(ps[1], ps[1].t[:, 0:NEXP]), AF.Sigmoid)
        NG = NT * 4
        g3v = lambda tl: R(tl, tl.t[:].rearrange("p t (g e) -> p (t g) e", e=4))
        bcg = lambda tl: R(tl, tl.t[:].unsqueeze(2).broadcast_to([128, NG, 4]))
        P.tt(rbi[:], rsc[:], R(rbb, rbb.t[:].unsqueeze(1).broadcast_to([128, NT, NEXP])), ALU.add)
        P.reduce(rm1[:], g3v(rbi), ALU.max)
        P.tt(g3v(req), g3v(rbi), bcg(rm1), ALU.is_equal)
        P.stt(g3v(rg2), g3v(req), -1e30, g3v(rbi), ALU.mult, ALU.add)
        P.reduce(rm2[:], g3v(rg2), ALU.max)
        P.tt(rgs[:], rm1[:], rm2[:], ALU.add)
        P.reduce(rgm[:], R(rgs, rgs.t[:].rearrange("p (t g) -> p t g", g=4)), ALU.max)
        P.tt(R(rgs, rgs.t[:].rearrange("p (t g) -> p t g", g=4)), R(rgs, rgs.t[:].rearrange("p (t g) -> p t g", g=4)),
             R(rgm, rgm.t[:].unsqueeze(2).broadcast_to([128, NT, 4])), ALU.is_equal)
        P.tt(g3v(req), g3v(rbi), bcg(rm2), ALU.is_ge)
        P.tt(g3v(req), g3v(req), bcg(rgs), ALU.mult)
        P.tt(rg2[:], rsc[:], req[:], ALU.mult)
        P.reduce(rgm[:], rg2[:], ALU.add)
        P.op("dve", lambda h: h.reciprocal(rgm.t[:], rgm.t[:]), [rgm[:]], [rgm[:]])
        P.tt(gates[:], rg2[:], R(rgm, rgm.t[:].unsqueeze(2).broadcast_to([128, NT, NEXP])), ALU.mult)
        P.pop()

        if dbg == "p4" and l == dbg_layer:
            o = P.dram("dbg_x1", [TOK, D], F32, kind="ExternalOutput")
            P.push()
            for t in range(NT):
                tt_ = P.sbuf("dbgx%d" % t, [128, D], F32)
                P.dma(tt_[:], xs_d[t * 128:(t + 1) * 128, :], q="sp")
                P.dma(o[t * 128:(t + 1) * 128, :], tt_[:], q="sp")
            o2 = P.dram("dbg_h2T", [D, TOK], BF16, kind="ExternalOutput")
            P.dma(R(o2, o2.t[:].rearrange("(c p) n -> p c n", p=128)), hT[:], q="sp")
            om = P.dram("dbg_mod", [DEPTH, 1, 6 * D], F32, kind="ExternalOutput")
            t_ = P.sbuf("dbg_tm", [1, 6 * D], F32)
            for l_ in range(nlayers):
                P.dma(t_[:], mod_d[l_, :, :], q="sp")
                P.dma(om[l_, :, :], t_[:], q="sp")
            o3 = P.dram("dbg_gates", [128, NT * NEXP], F32, kind="ExternalOutput")
            P.dma(o3[:], R(gates, gates.t[:].rearrange("p t e -> p (t e)")), q="sp")
            P.emit()
            return nc

        P.push()
        y_acc = P.sbuf("y_acc", [128, NT, D], F32)
        P.push()
        w13 = [P.sbuf("w13_%d" % i, [128, 2, KC, 256], BF16) for i in range(2)]
        hid = [P.sbuf("hid%d" % i, [128, 4, TOK], BF16) for i in range(2)]
        w2e = P.sbuf("w2e", [128, 4, D], BF16)
        sA = [P.sbuf("sA%d" % i, [128, 512], BF16) for i in range(2)]
        nh = 0
        for e in range(NEXP):
            hd = hid[e % 2]
            for fh in range(2):
                wb_ = w13[nh % 2]
                nh += 1
                for wi, wsrc in enumerate((w1_in, w3_in)):
                    P.dma(R(wb_, wb_.t[:, wi, :, :]),
                          R(wsrc, wsrc.t[l, e, :, fh * 256:(fh + 1) * 256].rearrange("(c p) f -> p c f", p=128)), q="pool")
                for tb in range(2):
                    for fc in range(2):
                        i_ = (tb * 2 + fc) % 2
                        pa, pb_ = ps[i_ * 2], ps[i_ * 2 + 1]
                        for kc in range(KC):
                            P.mm(pa[:], R(wb_, wb_.t[:, 0, kc, fc * 128:(fc + 1) * 128]),
                                 hT[:, kc, tb * 512:(tb + 1) * 512], start=(kc == 0), stop=(kc == KC - 1))
                        for kc in range(KC):
                            P.mm(pb_[:], R(wb_, wb_.t[:, 1, kc, fc * 128:(fc + 1) * 128]),
                                 hT[:, kc, tb * 512:(tb + 1) * 512], start=(kc == 0), stop=(kc == KC - 1))
                        P.act(sA[i_][:], pa[:], AF.Silu)
                        P.tt(R(hd, hd.t[:, fh * 2 + fc, tb * 512:(tb + 1) * 512]), pb_[:], sA[i_][:], ALU.mult)
            P.dma(w2e[:], R(w2_in, w2_in.t[l, e].rearrange("(c p) n -> p c n", p=128)), q="pool")
            for t in range(NT):
                for cb in range(4):
                    po = ps[4 + (t * 4 + cb) % 4]
                    for fc in range(4):
                        P.mm(po[:], R(hd, hd.t[:, fc, t * 128:(t + 1) * 128]), w2e[:, fc, cb * 512:(cb + 1) * 512],
                             start=(fc == 0), stop=(fc == 3))
                    ya_ = y_acc[:, t, cb * 512:(cb + 1) * 512]
                    if e == 0:
                        P.ts(ya_, po[:], gates[:, t, e:e + 1], ALU.mult)
                    else:
                        P.stt(ya_, po[:], gates[:, t, e:e + 1], ya_, ALU.mult, ALU.add)
        P.pop()
        g2b = P.sbuf("g2b", [128, D], F32)
        lng2 = P.sbuf("lng2", [128, D], F32)
        lnb2 = P.sbuf("lnb2", [128, D], F32)
        junk = [P.sbuf("junk_e%d" % i, [128, D], BF16) for i in range(2)]
        xn = [P.sbuf("xn_e%d" % i, [128, D], F32) for i in range(2)]
        x1l = [P.sbuf("x1l%d" % i, [128, D], F32) for i in range(2)]
        x2t = [P.sbuf("x2t%d" % i, [128, D], F32) for i in range(2)]
        P.dma(g2b[:], bc_rows(mod_d, l * 6 * D + 5 * D, D), q="sp")
        P.dma(lng2[:], bc_rows(lnfg_in, l * D, D), q="sp")
        P.dma(lnb2[:], bc_rows(lnfb_in, l * D, D), q="sp")
        for t in range(NT):
            P.dma(x1l[t % 2][:], xs_d[t * 128:(t + 1) * 128, :], q="sp")
            P.tt(y_acc[:, t, :], y_acc[:, t, :], g2b[:], ALU.mult)
            P.stt(x1l[t % 2][:], x1l[t % 2][:], ALPHA, y_acc[:, t, :], ALU.mult, ALU.add)
            layer_norm(x1l[t % 2], x2t[t % 2][:], lng2, lnb2, junk, xn)
            dst = out_d if l == nlayers - 1 else xs_d
            P.dma(dst[t * 128:(t + 1) * 128, :], x2t[t % 2][:], q="sp")
        P.pop()

    P.emit()
    return nc


_NC_CACHE = {}

_VEC3 = ("diff_lambda_q", "diff_lambda_k", "diff_subln", "mla_q_norm", "mla_kv_norm", "swa_sink",
         "b_ada", "ln_mix_g", "ln_mix_b", "ln_ffn_g", "ln_ffn_b")


def make_in_maps(inputs, nlayers=DEPTH):
    f = lambda k: np.ascontiguousarray(np.asarray(inputs[k]))
    shared = {}
    for k in inputs:
        if k in ("x", "c", "positions"):
            continue
        a = f(k)
        if k in _VEC3:
            a = a.reshape(DEPTH, 1, -1)
        elif k == "router_bias":
            a = a.reshape(1, NEXP)
        if a.ndim >= 3 and a.shape[0] == DEPTH and nlayers < DEPTH:
            a = np.ascontiguousarray(a[:nlayers])
        shared[k] = a
    x = f("x")
    c = f("c")
    pos = f("positions").astype(np.int32)
    maps = []
    for core in range(8):
        b, j = core // 4, core % 4
        sel = np.zeros((1, 8), np.float32)
        if j >= 1:
            sel[0, j - 1] = 1.0
        if j <= 2:
            sel[0, 4 + j] = 1.0
        m = dict(shared)
        m["x"] = np.ascontiguousarray(x[b, j * TOK:(j + 1) * TOK, :])
        m["c"] = np.ascontiguousarray(c[b:b + 1, :])
        m["positions"] = np.ascontiguousarray(pos[b, j * TOK:(j + 1) * TOK].reshape(TOK, 1))
        m["sel"] = sel
        maps.append(m)
    return maps


def kernel(**inputs):
    if "nc" not in _NC_CACHE:
        _NC_CACHE["nc"] = build()
    nc = _NC_CACHE["nc"]
    maps = make_in_maps(inputs)
    res = run_bass_kernel_spmd(nc, maps, core_ids=list(range(8)))
    out = np.empty((NB, S, D), np.float32)
    for core in range(8):
        b, j = core // 4, core % 4
        out[b, j * TOK:(j + 1) * TOK, :] = res.results[core]["out"]
    return out
```
